# Optimizing a Trainium2 kernel written in Bass

```python
import math
import jax, jax.numpy as jnp
from jax import lax
import numpy as np


D_MODEL = 1024
BATCH = 8
SEQ = 2048
DEPTH = 2
DEC_BATCH = 32
DEC_SEQ = 4
PAST_LEN = 16384
PAGE_SIZE = 128

N_EVEN = (DEPTH + 1) // 2
N_ODD = DEPTH // 2
D_CONV = D_MODEL // 2
CONV_K = 31
N_HEADS = 8
NOPE_DIM = 64
ROPE_DIM = 32
V_DIM = 64
Q_RANK = 512
KV_RANK = 256
ROPE_THETA = 10000.0
Q_BLOCK = 128
ATTN_SCALE = (NOPE_DIM + ROPE_DIM) ** -0.5
IN_COLS = 2 * D_CONV + Q_RANK + KV_RANK + ROPE_DIM
MIX_COLS = D_CONV + N_HEADS * V_DIM
POOL_WINDOWS = (2, 4, 8, 16)
POOL_GROUPS = len(POOL_WINDOWS)
POOL_GC = D_MODEL // POOL_GROUPS
POOL_CTX = max(POOL_WINDOWS) - 1
D_FF = 2816
N_EXPERTS = 8
TOP_K = 2
D_FF_EXPERT = 3584
EPS = 1e-6

kernel_name = "hybrid_conv_mla_pool_moe_decoder_step"


def rmsnorm(x, g):
    xf = x.astype(jnp.float32)
    y = xf * lax.rsqrt(jnp.mean(xf * xf, axis=-1, keepdims=True) + EPS)
    return (y * g.astype(jnp.float32)).astype(x.dtype)


def layernorm(x, g, b):
    xf = x.astype(jnp.float32)
    mu = jnp.mean(xf, axis=-1, keepdims=True)
    xc = xf - mu
    y = xc * lax.rsqrt(jnp.mean(xc * xc, axis=-1, keepdims=True) + EPS)
    return (y * g.astype(jnp.float32) + b.astype(jnp.float32)).astype(x.dtype)


def rope(x, pos):
    half = ROPE_DIM // 2
    inv_freq = jnp.power(ROPE_THETA, -jnp.arange(half, dtype=jnp.float32) / half)
    ang = pos.astype(jnp.float32)[:, None] * inv_freq[None, :]
    shape = (pos.shape[0],) + (1,) * (x.ndim - 3) + (half,)
    cos = jnp.cos(ang).reshape(shape)
    sin = jnp.sin(ang).reshape(shape)
    xf = x.astype(jnp.float32)
    x1, x2 = xf[..., :half], xf[..., half:]
    return jnp.concatenate([x1 * cos - x2 * sin, x2 * cos + x1 * sin], axis=-1).astype(x.dtype)


def swiglu(h, wg, wu, wd):
    return (jax.nn.silu(h @ wg) * (h @ wu)) @ wd


def causal_depthwise(ext, w, b):
    y = lax.conv_general_dilated(ext, w[:, None, :].astype(ext.dtype), window_strides=(1,), padding='VALID',
                                 dimension_numbers=('NWC', 'WIO', 'NWC'), feature_group_count=ext.shape[-1])
    return y + b.astype(y.dtype)


def mla_prompt(q_nope, q_pe, ckv, kpe, w_ukv):
    B, S, H, _ = q_nope.shape
    kv = jnp.einsum('bsr,rhd->bshd', ckv, w_ukv)
    k_nope, v = kv[..., :NOPE_DIM], kv[..., NOPE_DIM:]
    nb = S // Q_BLOCK
    qn = q_nope.reshape(B, nb, Q_BLOCK, H, NOPE_DIM).transpose(1, 0, 2, 3, 4)
    qp = q_pe.reshape(B, nb, Q_BLOCK, H, ROPE_DIM).transpose(1, 0, 2, 3, 4)
    key_pos = jnp.arange(S)

    def block(args):
        qn_b, qp_b, b_idx = args
        q_pos = b_idx * Q_BLOCK + jnp.arange(Q_BLOCK)
        s = (jnp.einsum('bqhd,bkhd->bhqk', qn_b, k_nope)
             + jnp.einsum('bqhp,bkp->bhqk', qp_b, kpe)).astype(jnp.float32) * ATTN_SCALE
        mask = key_pos[None, :] <= q_pos[:, None]
        p = jax.nn.softmax(jnp.where(mask[None, None], s, -jnp.inf), axis=-1).astype(v.dtype)
        return jnp.einsum('bhqk,bkhd->bqhd', p, v)

    out = lax.map(block, (qn, qp, jnp.arange(nb)))
    return out.transpose(1, 0, 2, 3, 4).reshape(B, S, H, V_DIM)


def mla_sample(q_nope, q_pe, ckv_new, kpe_new, cache_ckv, cache_kpe, i, page_table, w_ukv):
    DB, T, H, _ = q_nope.shape
    past = page_table.shape[1] * cache_ckv.shape[2]
    ckv_past = cache_ckv[i, page_table].reshape(DB, past, KV_RANK)
    kpe_past = cache_kpe[i, page_table].reshape(DB, past, ROPE_DIM)
    w_uk, w_uv = w_ukv[..., :NOPE_DIM], w_ukv[..., NOPE_DIM:]
    q_lat = jnp.einsum('bthn,rhn->bthr', q_nope, w_uk)
    s_past = (jnp.einsum('bthr,bsr->bhts', q_lat, ckv_past)
              + jnp.einsum('bthp,bsp->bhts', q_pe, kpe_past)).astype(jnp.float32) * ATTN_SCALE
    s_new = (jnp.einsum('bthr,bsr->bhts', q_lat, ckv_new)
             + jnp.einsum('bthp,bsp->bhts', q_pe, kpe_new)).astype(jnp.float32) * ATTN_SCALE
    causal = jnp.tril(jnp.ones((T, T), dtype=bool))
    s_new = jnp.where(causal[None, None], s_new, -jnp.inf)
    p = jax.nn.softmax(jnp.concatenate([s_past, s_new], axis=-1), axis=-1).astype(ckv_new.dtype)
    p_past, p_new = p[..., :past], p[..., past:]
    o_lat = (jnp.einsum('bhts,bsr->bthr', p_past, ckv_past)
             + jnp.einsum('bhts,bsr->bthr', p_new, ckv_new))
    return jnp.einsum('bthr,rhv->bthv', o_lat, w_uv)


def mixer_conv_mla(h, pos, conv_prev, attend, i, P):
    B, T, _ = h.shape
    proj = h @ P['w_in'][i]
    a = proj[..., :D_CONV]
    gate = proj[..., D_CONV:2 * D_CONV]
    q_dn = proj[..., 2 * D_CONV:2 * D_CONV + Q_RANK]
    kv_dn = proj[..., 2 * D_CONV + Q_RANK:]
    u = a * jax.nn.sigmoid(gate)
    ext = jnp.concatenate([conv_prev.astype(u.dtype), u], axis=1)
    c = causal_depthwise(ext, P['conv_w'][i], P['conv_b'][i])
    c = jax.nn.silu(layernorm(c, P['conv_ln_g'][i], P['conv_ln_b'][i]))
    q = (rmsnorm(q_dn, P['q_norm'][i]) @ P['w_uq'][i]).reshape(B, T, N_HEADS, NOPE_DIM + ROPE_DIM)
    q_nope, q_pe = q[..., :NOPE_DIM], rope(q[..., NOPE_DIM:], pos)
    ckv = rmsnorm(kv_dn[..., :KV_RANK], P['kv_norm'][i])
    kpe = rope(kv_dn[..., KV_RANK:], pos)
    w_ukv = P['w_ukv'][i].reshape(KV_RANK, N_HEADS, NOPE_DIM + V_DIM)
    o = attend(i, q_nope, q_pe, ckv, kpe, w_ukv)
    out = jnp.concatenate([c, o.reshape(B, T, N_HEADS * V_DIM).astype(c.dtype)], axis=-1) @ P['w_out'][i]
    return out, ext[:, -(CONV_K - 1):], ckv, kpe


def pool_mixer(h, prev, pos, w_pool, scale):
    B, T, D = h.shape
    ext = jnp.concatenate([prev.astype(h.dtype), h], axis=1)
    cs = jnp.cumsum(ext.astype(jnp.float32), axis=1)
    cs = jnp.concatenate([jnp.zeros((B, 1, D), jnp.float32), cs], axis=1)
    start = POOL_CTX + 1
    means = []
    for g, w in enumerate(POOL_WINDOWS):
        sl = slice(g * POOL_GC, (g + 1) * POOL_GC)
        s = cs[:, start:start + T, sl] - cs[:, start - w:start - w + T, sl]
        cnt = jnp.minimum(pos + 1, w).astype(jnp.float32)[None, :, None]
        means.append(s / cnt)
    diff = (jnp.concatenate(means, axis=-1) - h.astype(jnp.float32)).astype(h.dtype)
    y = jnp.einsum('btgc,gcd->btgd', diff.reshape(B, T, POOL_GROUPS, POOL_GC), w_pool).reshape(B, T, D)
    return y * scale.astype(y.dtype), ext[:, -POOL_CTX:]


def moe_swiglu(h, router_w, wg, wu, wd):
    B, T, D = h.shape
    t = h.reshape(B * T, D)
    logits = (t @ router_w).astype(jnp.float32)
    vals, idx = lax.top_k(logits, TOP_K)
    gk = jax.nn.softmax(vals, axis=-1)
    gate = jnp.sum(jax.nn.one_hot(idx, N_EXPERTS, dtype=jnp.float32) * gk[..., None], axis=1)
    out = jnp.zeros((B * T, D), jnp.float32)
    for e in range(N_EXPERTS):
        out = out + gate[:, e:e + 1] * swiglu(t, wg[e], wu[e], wd[e]).astype(jnp.float32)
    return out.astype(h.dtype).reshape(B, T, D)


def forward(x, pos, conv_prev, pool_prev, attend, P):
    ckvs, kpes, convs, pools = [], [], [], []
    for layer in range(DEPTH):
        i = layer // 2
        h = rmsnorm(x, P['norm_mix'][layer])
        if layer % 2 == 0:
            m, cst, ckv, kpe = mixer_conv_mla(h, pos, conv_prev[i], attend, i, P)
            ckvs.append(ckv); kpes.append(kpe); convs.append(cst)
        else:
            m, pst = pool_mixer(h, pool_prev[i], pos, P['pool_w'][i], P['pool_scale'][i])
            pools.append(pst)
        x = x + m
        h = rmsnorm(x, P['norm_ffn'][layer])
        if layer % 2 == 0:
            f = swiglu(h, P['ffn_w_gate'][i], P['ffn_w_up'][i], P['ffn_w_down'][i])
        else:
            f = moe_swiglu(h, P['router_w'][i], P['moe_w_gate'][i], P['moe_w_up'][i], P['moe_w_down'][i])
        x = x + f
    y = rmsnorm(x, P['norm_final'])
    return y, jnp.stack(ckvs), jnp.stack(kpes), jnp.stack(convs), jnp.stack(pools)


def setup_inputs(seed: int = 0) -> dict:
    key = jax.random.key(seed)
    ks = iter(jax.random.split(key, 40))
    f32 = jnp.float32

    def nrm(shape, fan_in):
        return jax.random.normal(next(ks), shape, f32) * (fan_in ** -0.5)

    def gain(shape):
        return 1.0 + 0.02 * jax.random.normal(next(ks), shape, f32)

    def small(shape):
        return 0.02 * jax.random.normal(next(ks), shape, f32)

    n_pages = PAST_LEN // PAGE_SIZE
    n_used = DEC_BATCH * n_pages
    n_pool = n_used + n_used // 4
    x_prompt = jax.random.normal(next(ks), (BATCH, SEQ, D_MODEL), f32)
    x_sample = jax.random.normal(next(ks), (DEC_BATCH, DEC_SEQ, D_MODEL), f32)
    cache_ckv = jax.random.normal(next(ks), (N_EVEN, n_pool, PAGE_SIZE, KV_RANK), f32)
    cache_kpe = jax.random.normal(next(ks), (N_EVEN, n_pool, PAGE_SIZE, ROPE_DIM), f32)
    page_table = jax.random.permutation(next(ks), n_pool)[:n_used].reshape(DEC_BATCH, n_pages).astype(jnp.int32)
    state_conv = 0.5 * jax.random.normal(next(ks), (N_EVEN, DEC_BATCH, CONV_K - 1, D_CONV), f32)
    state_pool = jax.random.normal(next(ks), (N_ODD, DEC_BATCH, POOL_CTX, D_MODEL), f32)
    return {
        'x_prompt': x_prompt,
        'x_sample': x_sample,
        'cache_ckv': cache_ckv,
        'cache_kpe': cache_kpe,
        'page_table': page_table,
        'state_conv': state_conv,
        'state_pool': state_pool,
        'norm_mix': gain((DEPTH, D_MODEL)),
        'norm_ffn': gain((DEPTH, D_MODEL)),
        'norm_final': gain((D_MODEL,)),
        'w_in': nrm((N_EVEN, D_MODEL, IN_COLS), D_MODEL),
        'conv_w': nrm((N_EVEN, CONV_K, D_CONV), CONV_K),
        'conv_b': small((N_EVEN, D_CONV)),
        'conv_ln_g': gain((N_EVEN, D_CONV)),
        'conv_ln_b': small((N_EVEN, D_CONV)),
        'q_norm': gain((N_EVEN, Q_RANK)),
        'w_uq': nrm((N_EVEN, Q_RANK, N_HEADS * (NOPE_DIM + ROPE_DIM)), Q_RANK),
        'kv_norm': gain((N_EVEN, KV_RANK)),
        'w_ukv': nrm((N_EVEN, KV_RANK, N_HEADS * (NOPE_DIM + V_DIM)), KV_RANK),
        'w_out': nrm((N_EVEN, MIX_COLS, D_MODEL), MIX_COLS),
        'ffn_w_gate': nrm((N_EVEN, D_MODEL, D_FF), D_MODEL),
        'ffn_w_up': nrm((N_EVEN, D_MODEL, D_FF), D_MODEL),
        'ffn_w_down': nrm((N_EVEN, D_FF, D_MODEL), D_FF),
        'pool_w': nrm((N_ODD, POOL_GROUPS, POOL_GC, POOL_GC), POOL_GC),
        'pool_scale': 0.5 + 0.1 * jax.random.normal(next(ks), (N_ODD, D_MODEL), f32),
        'router_w': nrm((N_ODD, D_MODEL, N_EXPERTS), D_MODEL),
        'moe_w_gate': nrm((N_ODD, N_EXPERTS, D_MODEL, D_FF_EXPERT), D_MODEL),
        'moe_w_up': nrm((N_ODD, N_EXPERTS, D_MODEL, D_FF_EXPERT), D_MODEL),
        'moe_w_down': nrm((N_ODD, N_EXPERTS, D_FF_EXPERT, D_MODEL), D_FF_EXPERT),
    }


def reference(x_prompt, x_sample, cache_ckv, cache_kpe, page_table, state_conv, state_pool,
              norm_mix, norm_ffn, norm_final, w_in, conv_w, conv_b, conv_ln_g, conv_ln_b,
              q_norm, w_uq, kv_norm, w_ukv, w_out, ffn_w_gate, ffn_w_up, ffn_w_down,
              pool_w, pool_scale, router_w, moe_w_gate, moe_w_up, moe_w_down):
    P = {'norm_mix': norm_mix, 'norm_ffn': norm_ffn, 'norm_final': norm_final, 'w_in': w_in,
         'conv_w': conv_w, 'conv_b': conv_b, 'conv_ln_g': conv_ln_g, 'conv_ln_b': conv_ln_b,
         'q_norm': q_norm, 'w_uq': w_uq, 'kv_norm': kv_norm, 'w_ukv': w_ukv, 'w_out': w_out,
         'ffn_w_gate': ffn_w_gate, 'ffn_w_up': ffn_w_up, 'ffn_w_down': ffn_w_down,
         'pool_w': pool_w, 'pool_scale': pool_scale, 'router_w': router_w,
         'moe_w_gate': moe_w_gate, 'moe_w_up': moe_w_up, 'moe_w_down': moe_w_down}

    Bp, Sp, _ = x_prompt.shape
    pos_p = jnp.arange(Sp, dtype=jnp.int32)
    conv0 = jnp.zeros((N_EVEN, Bp, CONV_K - 1, D_CONV), x_prompt.dtype)
    pool0 = jnp.zeros((N_ODD, Bp, POOL_CTX, D_MODEL), x_prompt.dtype)

    def attend_prompt(i, qn, qp, ckv, kpe, wkv):
        return mla_prompt(qn, qp, ckv, kpe, wkv)

    y_prompt, ckv_p, kpe_p, conv_p, pool_p = forward(x_prompt, pos_p, conv0, pool0, attend_prompt, P)

    Ts = x_sample.shape[1]
    pos_s = PAST_LEN + jnp.arange(Ts, dtype=jnp.int32)

    def attend_sample(i, qn, qp, ckv, kpe, wkv):
        return mla_sample(qn, qp, ckv, kpe, cache_ckv, cache_kpe, i, page_table, wkv)

    y_sample, ckv_s, kpe_s, conv_s, pool_s = forward(x_sample, pos_s, state_conv, state_pool, attend_sample, P)

    return (y_prompt, y_sample, ckv_p, kpe_p, conv_p, pool_p, ckv_s, kpe_s, conv_s, pool_s)
```

```python
from contextlib import ExitStack
import numpy as np
import concourse.bass as bass
import concourse.mybir as mybir
from concourse.bass_utils import run_bass_kernel_spmd

F32 = mybir.dt.float32
BF16 = mybir.dt.bfloat16
I32 = mybir.dt.int32
AF = mybir.ActivationFunctionType
ALU = mybir.AluOpType

NP_ = 2048
NS_ = 16
T = NP_ + NS_
TILES = [(0, 512), (512, 512), (1024, 512), (1536, 512), (2048, 16)]
EPS = 1e-6
SCALE = 96.0 ** -0.5
DFF = 2816
DFE = 3584
NE = 8
CACHE_PAGES = 5120
SMALLW = False
PV_NM0, PV_NF0, PV_NM1, PV_NF1, PV_NFIN = 0, 8, 16, 24, 32
PV_CB, PV_LG, PV_LB, PV_QN, PV_KVN, PV_PS, PV_CW = 40, 44, 48, 52, 56, 58, 66
PV_W = 66 + 124
C_ID, C_TRI, C_MN, C_INV, C_IOTA = 0, 128, 256, 288, 352
C_W = 353


class Eng:
    def __init__(self, name, h, sem):
        self.name, self.h, self.sem, self.cnt, self.seen = name, h, sem, 0, {}


class Tok:
    __slots__ = ("w", "r")

    def __init__(self):
        self.w = None
        self.r = {}


class K:
    def __init__(self, nc, es):
        self.nc = nc
        self.es = es
        mk = lambda n, h: Eng(n, h, es.enter_context(nc.semaphore("s_" + n)))
        self.pe = mk("pe", nc.tensor)
        self.act = mk("act", nc.scalar)
        self.dve = mk("dve", nc.vector)
        self.pool = mk("pool", nc.gpsimd)
        self.sp = mk("sp", nc.sync)
        self.dsems = []
        self.nds = 0
        self.n = 0
        self.limit = 10 ** 9
        self.log = []

    def dsem(self):
        self.nds += 1
        d = Eng("d%d" % self.nds, None, self.es.enter_context(self.nc.semaphore("s_d%d" % self.nds)))
        self.dsems.append(d)
        return d

    def _waits(self, eng, r, w):
        need = {}

        def add(p):
            if p is None:
                return
            e, c = p
            if e is eng and eng is self.pe:
                return
            if need.get(e, 0) < c:
                need[e] = c

        for t in r:
            add(t.w)
        for t in w:
            add(t.w)
            for e, c in t.r.items():
                add((e, c))
        for e, c in need.items():
            if eng.seen.get(e, 0) >= c:
                continue
            eng.h.wait_ge(e.sem, c)
            eng.seen[e] = c

    def op(self, eng, fn, r=(), w=()):
        self.n += 1
        if self.n > self.limit:
            return None
        self._waits(eng, r, w)
        ins = fn()
        eng.cnt += 1
        ins.then_inc(eng.sem, 1)
        for t in r:
            t.r[eng] = eng.cnt
        for t in w:
            t.w = (eng, eng.cnt)
            t.r = {}
        return ins

    def dma(self, q, ds, out, in_, r=(), w=(), **kw):
        self.n += 1
        if self.n > self.limit:
            return None
        self._waits(q, r, w)
        ins = q.h.dma_start(out=out, in_=in_, **kw)
        ds.cnt += 16
        ins.then_inc(ds.sem, 16)
        for t in r:
            t.r[ds] = ds.cnt
        for t in w:
            t.w = (ds, ds.cnt)
            t.r = {}

    def idma(self, ds, out, in_, idx_ap, r=(), w=()):
        q = self.pool
        self.n += 1
        if self.n > self.limit:
            return None
        self._waits(q, r, w)
        ins = q.h.indirect_dma_start(out=out, out_offset=None, in_=in_,
                                     in_offset=bass.IndirectOffsetOnAxis(ap=idx_ap, axis=0))
        ds.cnt += 16
        ins.then_inc(ds.sem, 16)
        for t in r:
            t.r[ds] = ds.cnt
        for t in w:
            t.w = (ds, ds.cnt)
            t.r = {}

    def barrier(self):
        engs = [self.pe, self.act, self.dve, self.pool, self.sp]
        for e in engs:
            for o in engs + self.dsems:
                if o is e or o.cnt == 0:
                    continue
                if e.seen.get(o, 0) < o.cnt:
                    e.h.wait_ge(o.sem, o.cnt)
                    e.seen[o] = o.cnt


def build(stage=99, limit=10 ** 9):
    nc = bass.Bass("TRN2", target_bir_lowering=False)

    def din(name, shape, dt=F32):
        return nc.dram_tensor(name, list(shape), dt, kind="ExternalInput").ap()

    def dout(name, shape):
        return nc.dram_tensor(name, list(shape), F32, kind="ExternalOutput").ap()

    xp = din("xp", [NP_, 1024])
    xs = din("xs", [NS_, 1024])
    ccat = din("ccat", [CACHE_PAGES * 128, 288])
    ptab = din("ptab", [128, 512], I32)
    sconv = din("sconv", [120, 512])
    spool = din("spool", [60, 1024])
    pvec_d = din("pvec", [128, PV_W])
    cst_d = din("cst", [128, C_W])
    ropec_d = din("ropec", [32, T])
    ropes_d = din("ropes", [32, T])
    w_in = din("w_in", [1024, 1824])
    w_uq = din("w_uq", [512, 768])
    w_ukv = din("w_ukv", [256, 1024])
    w_out = din("w_out", [1024, 1024])
    ffn_g = din("ffn_g", [1024, DFF])
    ffn_u = din("ffn_u", [1024, DFF])
    ffn_d = din("ffn_d", [DFF, 1024])
    pool_w = din("pool_w", [4, 256, 256])
    router_w = din("router_w", [1024, 8])
    ne_ = 1 if SMALLW else NE
    moe_g = din("moe_g", [ne_, 1024, DFE])
    moe_u = din("moe_u", [ne_, 1024, DFE])
    moe_d = din("moe_d", [ne_, DFE, 1024])

    o_yp = dout("o_yp", [NP_, 1024])
    o_ys = dout("o_ys", [NS_, 1024])
    o_ckvp = dout("o_ckvp", [NP_, 256])
    o_kpep = dout("o_kpep", [NP_, 32])
    o_convp = dout("o_convp", [30, 512])
    o_poolp = dout("o_poolp", [15, 1024])
    o_ckvs = dout("o_ckvs", [NS_, 256])
    o_kpes = dout("o_kpes", [NS_, 32])
    o_convs = dout("o_convs", [4, 30, 512])
    o_pools = dout("o_pools", [4, 15, 1024])
    xscr = nc.dram_tensor("xscr", [128, 8, T], F32, kind="Internal").ap()

    es = ExitStack()
    with es:
        k = K(nc, es)
        k.limit = limit
        pe, act, dve, pool, sp = k.pe, k.act, k.dve, k.pool, k.sp
        ctr = [0]

        def sb(stack, shape, dt=F32, name=None):
            ctr[0] += 1
            return stack.enter_context(nc.sbuf_tensor("%s_%d" % (name or "t", ctr[0]), list(shape), dt))

        banks = []
        for i in range(8):
            banks.append((es.enter_context(nc.psum_tensor("ps%d" % i, [128, 512], F32)), Tok()))
        pctr = [0]

        nrot = [6]

        def ps():
            b = banks[pctr[0] % nrot[0]]
            pctr[0] += 1
            return b
        actr = [0]

        def psacc():
            b = banks[5 + actr[0] % 2]
            actr[0] += 1
            return b

        out_ds = []
        octr = [0]

        odm = {}

        def odma(out, in_, r):
            key = id(r[0]) if r else 0
            if key not in odm:
                odm[key] = k.dsem()
                out_ds.append(odm[key])
            k.dma(sp, odm[key], out, in_, r=r)

        cst = sb(es, [128, C_W], name="cst")
        pvec = sb(es, [128, PV_W], name="pvec")
        t_c = Tok()
        d_c = k.dsem()
        k.dma(sp, d_c, cst[:], cst_d, w=[t_c])
        k.dma(sp, d_c, pvec[:], pvec_d, w=[t_c])
        ident = cst[:, C_ID:C_ID + 128]
        o1024 = sb(es, [128, 128], BF16, "o1024")
        o512 = sb(es, [128, 128], BF16, "o512")
        o256 = sb(es, [128, 128], BF16, "o256")
        tri = sb(es, [128, 128], BF16, "tri")
        mnew = sb(es, [4, 32], BF16, "mnew")
        k.op(dve, lambda: nc.vector.memset(o1024[:], 1.0 / 1024), w=[t_c])
        k.op(dve, lambda: nc.vector.memset(o512[:], 1.0 / 512), w=[t_c])
        k.op(dve, lambda: nc.vector.memset(o256[:], 1.0 / 256), w=[t_c])
        k.op(dve, lambda: nc.vector.tensor_copy(out=tri[:], in_=cst[:, C_TRI:C_TRI + 128]), r=[t_c], w=[t_c])
        k.op(dve, lambda: nc.vector.tensor_copy(out=mnew[:], in_=cst[0:4, C_MN:C_MN + 32]), r=[t_c], w=[t_c])

        def pv(col, c=0):
            return pvec[:, col + c:col + c + 1]

        def wload(ds, dst, src, w):
            k.dma(pool, ds, dst, src, w=w)
            if ds.cnt >= 48 and pool.seen.get(ds, 0) < ds.cnt - 32:
                pool.h.wait_ge(ds.sem, ds.cnt - 32)
                pool.seen[ds] = ds.cnt - 32

        epsb = sb(es, [128, 1], F32, "epsb")
        k.op(dve, lambda: nc.vector.memset(epsb[:], EPS), w=[t_c])

        def rsqrt_to(out_ap, in_ap, scale, r, t_out):
            np_ = out_ap.shape[0]
            k.op(act, lambda: nc.scalar.activation(out=out_ap, in_=in_ap, func=AF.Sqrt, scale=scale,
                                                   bias=epsb[0:np_, 0:1]), r=list(r) + [t_c], w=[t_out])
            k.op(dve, lambda: nc.vector.reciprocal(out=out_ap, in_=out_ap), r=[t_out], w=[t_out])

        def rstd_from(srcs, n, ones_t, sqbuf, t_sq, rstd_ap, t_rstd, r):
            for i, s in enumerate(srcs):
                k.op(act, (lambda s=s, i=i: nc.scalar.activation(out=sqbuf[:, i, 0:n], in_=s, func=AF.Square)),
                     r=r, w=[t_sq])
            pst, tp = ps()

            def mm():
                ins = None
                for i in range(len(srcs)):
                    ins = nc.tensor.matmul(pst[:, 0:n], ones_t[:], sqbuf[:, i, 0:n], start=(i == 0),
                                           stop=(i == len(srcs) - 1))
                return ins
            k.op(pe, mm, r=[t_sq, t_c], w=[tp])
            rsqrt_to(rstd_ap[:, 0:n], pst[:, 0:n], 1.0, [tp], t_rstd)

        L0 = ExitStack()
        with L0:
            mixc = sb(L0, [128, 4, T], BF16, "mixc")
            t_mixc = [Tok() for _ in TILES]
            ckvT = sb(L0, [128, 2, T], BF16, "ckvT")
            t_ckvT = [Tok() for _ in TILES]
            kper = sb(L0, [128, T], BF16, "kper")
            t_kper = [Tok() for _ in TILES]
            qall = sb(L0, [96, 8, T], BF16, "qall")
            t_qall = [Tok() for _ in TILES]
            kpes0 = sb(L0, [32, NS_], BF16, "kpes0")
            t_kpes0 = Tok()
            newkv = sb(L0, [4, 4, 257], BF16, "newkv")
            t_newkv = Tok()
            wukv = sb(L0, [128, 2, 1024], BF16, "wukv")
            t_wukv = Tok()
            d_w0 = k.dsem()
            wload(d_w0, wukv[:], w_ukv.rearrange("(c p) n -> p c n", p=128), [t_wukv])

            PA = ExitStack()
            with PA:
                winb = sb(PA, [128, 8, 1824], BF16, "winb")
                wkpe = sb(PA, [128, 8, 2, 96], BF16, "wkpe")
                wuqb = sb(PA, [128, 4, 768], BF16, "wuqb")
                wuqs = sb(PA, [128, 4, 8, 96], BF16, "wuqs")
                t_w = Tok()
                k.op(pool, lambda: nc.gpsimd.memset(wkpe[:], 0.0), w=[t_w])
                k.op(pool, lambda: nc.gpsimd.memset(wuqs[:], 0.0), w=[t_w])
                win_v = w_in.rearrange("(c p) n -> p c n", p=128)
                for c in range(8):
                    wload(d_w0, winb[:, c, :], w_in[c * 128:(c + 1) * 128, :], [t_w])
                wload(d_w0, wkpe[:, :, 0, 64:96], win_v[:, :, 1792:1824], [t_w])
                wload(d_w0, wkpe[:, :, 1, 64:80], win_v[:, :, 1808:1824], [t_w])
                wload(d_w0, wkpe[:, :, 1, 80:96], win_v[:, :, 1792:1808], [t_w])
                wuq_v = w_uq.rearrange("(c p) n -> p c n", p=128)
                wload(d_w0, wuqb[:], wuq_v, [t_w])
                wuq_v4 = w_uq.rearrange("(c p) (h d) -> p c h d", p=128, d=96)
                for c in range(4):
                    wload(d_w0, wuqs[:, c, :, 64:80], wuq_v4[:, c, :, 80:96], [t_w])
                    wload(d_w0, wuqs[:, c, :, 80:96], wuq_v4[:, c, :, 64:80], [t_w])

                xin = [sb(PA, [128, 1024], F32, "xin") for _ in range(2)]
                t_xin = [Tok(), Tok()]
                d_xin = [k.dsem(), k.dsem()]
                junk = sb(PA, [128, 1024], BF16, "junk")
                t_junk = Tok()
                ssq = sb(PA, [128, 4], F32, "ssq")
                h0 = sb(PA, [128, 8, 512], BF16, "h0")
                t_h0 = Tok()
                sqb = sb(PA, [128, 8, 512], BF16, "sqb")
                t_sqb = Tok()
                sig = sb(PA, [128, 512], F32, "sig")
                t_sig = Tok()
                uroll = sb(PA, [128, 4, 542], F32, "uroll")
                t_ur = Tok()
                us = sb(PA, [128, 4, 4, 34], F32, "us")
                t_us = Tok()
                acc = sb(PA, [128, 4, 512], F32, "acc")
                t_accs = [Tok() for _ in range(4)]
                mean_sb = sb(PA, [128, 512], F32, "mean")
                var_sb = sb(PA, [128, 512], F32, "var")
                rstd_c = sb(PA, [128, 512], F32, "rstdc")
                t_ln = Tok()
                tt1 = sb(PA, [128, 512], F32, "tt1")
                tt2 = sb(PA, [128, 512], F32, "tt2")
                t_tt = Tok()
                qdn = sb(PA, [128, 4, 512], F32, "qdn")
                t_qdn = Tok()
                rstd_q = sb(PA, [128, 512], F32, "rstdq")
                t_rq = Tok()
                qn = sb(PA, [128, 4, 512], BF16, "qn")
                t_qn = Tok()
                kvf = sb(PA, [128, 2, 512], F32, "kvf")
                t_kvf = Tok()
                rstd_k = sb(PA, [128, 512], F32, "rstdk")
                t_rk = Tok()
                rc_t = sb(PA, [128, 512], F32, "ropec")
                rs_t = sb(PA, [128, 512], F32, "ropes")
                t_rope = Tok()
                d_rope = k.dsem()
                kpef = sb(PA, [128, 512], F32, "kpef")
                t_kpef = Tok()
                ckvo = sb(PA, [128, 4, 256], F32, "ckvo")
                t_ckvo = Tok()
                kpeo = sb(PA, [128, 4, 32], F32, "kpeo")
                t_kpeo = Tok()
                cvo = sig[0:30, :]
                t_cvo = t_sig
                cvs = qdn[0:4, :, :]
                t_cvs = t_qdn
                scv = kpef[0:120, :]
                t_scv = t_kpef
                d_misc = k.dsem()

                k.op(dve, lambda: nc.vector.memset(uroll[:, :, 0:30], 0.0), w=[t_ur])
                k.dma(sp, d_misc, scv[:], sconv, w=[t_scv])
                for c in range(4):
                    pst, tp = ps()
                    k.op(pe, lambda c=c, pst=pst: nc.tensor.transpose(pst[:, 0:120], scv[:, c * 128:(c + 1) * 128],
                                                                      ident[0:120, 0:120]), r=[t_scv, t_c], w=[tp])
                    k.op(act, lambda c=c, pst=pst: nc.scalar.copy(
                        out=us[:, c, :, 0:30], in_=pst[:, 0:120].rearrange("p (b s) -> p b s", b=4)), r=[tp], w=[t_us])

                for j, (t0, n) in enumerate(TILES):
                    nsub = (n + 127) // 128
                    for s in range(nsub):
                        rows = min(128, n - s * 128)
                        bi = (j * 4 + s) % 2
                        src = xp[t0 + s * 128:t0 + s * 128 + rows, :] if j < 4 else xs[:, :]
                        k.dma(sp, d_xin[bi], xin[bi][0:rows, :], src, w=[t_xin[bi]])
                        k.op(act, lambda bi=bi, rows=rows, s=s: nc.scalar.activation(
                            out=junk[0:rows, :], in_=xin[bi][0:rows, :], func=AF.Square,
                            accum_out=ssq[0:rows, s:s + 1]), r=[t_xin[bi]], w=[t_junk])
                        rsqrt_to(ssq[0:rows, s:s + 1], ssq[0:rows, s:s + 1], 1.0 / 1024, [t_junk], t_junk)
                        k.op(dve, lambda bi=bi, rows=rows, s=s: nc.vector.tensor_scalar(
                            out=xin[bi][0:rows, :], in0=xin[bi][0:rows, :], scalar1=ssq[0:rows, s:s + 1], scalar2=1.0,
                            op0=ALU.mult, op1=ALU.mult), r=[t_junk, t_xin[bi]], w=[t_xin[bi]])
                        for half in range(2):
                            pst, tp = ps()

                            def tr(bi=bi, rows=rows, half=half, pst=pst):
                                ins = None
                                for cc in range(4):
                                    c = half * 4 + cc
                                    ins = nc.tensor.transpose(pst[:, cc * 128:cc * 128 + rows],
                                                              xin[bi][0:rows, c * 128:(c + 1) * 128],
                                                              ident[0:rows, 0:rows])
                                return ins
                            k.op(pe, tr, r=[t_xin[bi], t_c], w=[tp])
                            for cc in range(4):
                                c = half * 4 + cc
                                k.op(act, lambda c=c, cc=cc, s=s, rows=rows, pst=pst: nc.scalar.activation(
                                    out=h0[:, c, s * 128:s * 128 + rows], in_=pst[:, cc * 128:cc * 128 + rows],
                                    func=AF.Copy, scale=pv(PV_NM0, c)), r=[tp, t_c], w=[t_h0])

                    def proj(col0, m, wt=None, wsel=None):
                        pst, tp = ps()

                        def mm():
                            ins = None
                            for c in range(8):
                                l = winb[:, c, col0:col0 + m] if wt is None else wt[:, c, wsel, 0:m]
                                ins = nc.tensor.matmul(pst[0:m, 0:n], l, h0[:, c, 0:n], start=(c == 0), stop=(c == 7))
                            return ins
                        k.op(pe, mm, r=[t_h0, t_w], w=[tp])
                        return pst, tp

                    for c in range(4):
                        pa, tpa = proj(c * 128, 128)
                        pg, tpg = proj(512 + c * 128, 128)
                        k.op(act, lambda pg=pg: nc.scalar.activation(out=sig[:, 0:n], in_=pg[:, 0:n], func=AF.Sigmoid),
                             r=[tpg], w=[t_sig])
                        if j < 4:
                            k.op(dve, lambda c=c, pa=pa: nc.vector.tensor_tensor(
                                out=uroll[:, c, 30:30 + n], in0=pa[:, 0:n], in1=sig[:, 0:n], op=ALU.mult),
                                r=[tpa, t_sig], w=[t_ur])
                        else:
                            k.op(dve, lambda c=c, pa=pa: nc.vector.tensor_tensor(
                                out=us[:, c, :, 30:34], in0=pa[:, 0:16].rearrange("p (b t) -> p b t", b=4),
                                in1=sig[:, 0:16].rearrange("p (b t) -> p b t", b=4), op=ALU.mult),
                                r=[tpa, t_sig], w=[t_us])
                    for c in range(4):
                        def ext(kk, c=c):
                            return uroll[:, c, kk:kk + n] if j < 4 else us[:, c, :, kk:kk + 4]

                        def accv(c=c):
                            return acc[:, c, 0:n] if j < 4 else acc[:, c, 0:16].rearrange("p (b t) -> p b t", b=4)
                        tsrc = t_ur if j < 4 else t_us
                        ce = dve
                        ceh = nc.vector
                        k.op(ce, lambda c=c, ext=ext, accv=accv, ceh=ceh: ceh.tensor_scalar(
                            out=accv(), in0=ext(0), scalar1=pv(PV_CW, c), scalar2=pv(PV_CB, c),
                            op0=ALU.mult, op1=ALU.add), r=[tsrc, t_c], w=[t_accs[c]])
                        for kk in range(1, 31):
                            k.op(ce, lambda c=c, kk=kk, ext=ext, accv=accv, ceh=ceh: ceh.scalar_tensor_tensor(
                                out=accv(), in0=ext(kk), scalar=pv(PV_CW, kk * 4 + c), in1=accv(),
                                op0=ALU.mult, op1=ALU.add), r=[tsrc, t_c, t_accs[c]], w=[t_accs[c]])
                    if j == 3:
                        pst, tp = ps()

                        def trc(pst=pst):
                            ins = None
                            for c in range(4):
                                ins = nc.tensor.transpose(pst[0:30, c * 128:(c + 1) * 128], uroll[:, c, 512:542], ident)
                            return ins
                        k.op(pe, trc, r=[t_ur, t_c], w=[tp])
                        k.op(act, lambda pst=pst: nc.scalar.copy(out=cvo[:], in_=pst[0:30, :]), r=[tp], w=[t_cvo])
                        odma(o_convp, cvo[:], [t_cvo])
                    if j == 4:
                        for b in range(4):
                            pst, tp = ps()

                            def trs(pst=pst, b=b):
                                ins = None
                                for c in range(4):
                                    ins = nc.tensor.transpose(pst[0:4, c * 128:(c + 1) * 128], us[:, c, b, 30:34], ident)
                                return ins
                            k.op(pe, trs, r=[t_us, t_c], w=[tp])
                            k.op(act, lambda pst=pst, b=b: nc.scalar.copy(out=cvs[:, b, :], in_=pst[0:4, :]),
                                 r=[tp], w=[t_cvs])
                        odma(o_convs[:, 26:30, :].rearrange("b t f -> t b f"), cvs[:], [t_cvs])
                        odma(o_convs[:, 0:26, :], sconv.rearrange("(b s) f -> b s f", b=4)[:, 4:30, :], [])
                    if j < 3:
                        k.op(dve, lambda: nc.vector.tensor_copy(out=uroll[:, :, 0:30], in_=uroll[:, :, 512:542]),
                             r=t_accs, w=[t_ur])
                    for c in range(4):
                        k.op(act, lambda c=c: nc.scalar.copy(out=sqb[:, c, 0:n], in_=acc[:, c, 0:n]), r=[t_accs[c]], w=[t_sqb])
                        k.op(act, lambda c=c: nc.scalar.activation(out=sqb[:, 4 + c, 0:n], in_=acc[:, c, 0:n],
                                                                   func=AF.Square), r=[t_accs[c]], w=[t_sqb])
                    pm, tpm = ps()
                    pq, tpq = ps()

                    def mmst(pm=pm, pq=pq):
                        ins = None
                        for c in range(4):
                            nc.tensor.matmul(pm[:, 0:n], o512[:], sqb[:, c, 0:n], start=(c == 0), stop=(c == 3))
                        for c in range(4):
                            ins = nc.tensor.matmul(pq[:, 0:n], o512[:], sqb[:, 4 + c, 0:n], start=(c == 0), stop=(c == 3))
                        return ins
                    k.op(pe, mmst, r=[t_sqb, t_c], w=[tpm, tpq])
                    k.op(act, lambda pm=pm: nc.scalar.copy(out=mean_sb[:, 0:n], in_=pm[:, 0:n]), r=[tpm], w=[t_ln])
                    k.op(dve, lambda: nc.vector.tensor_tensor(out=var_sb[:, 0:n], in0=mean_sb[:, 0:n], in1=mean_sb[:, 0:n],
                                                              op=ALU.mult), r=[t_ln], w=[t_ln])
                    k.op(dve, lambda pq=pq: nc.vector.tensor_tensor(out=var_sb[:, 0:n], in0=pq[:, 0:n], in1=var_sb[:, 0:n],
                                                                    op=ALU.subtract), r=[tpq, t_ln], w=[t_ln])
                    rsqrt_to(rstd_c[:, 0:n], var_sb[:, 0:n], 1.0, [t_ln], t_ln)
                    for c in range(4):
                        k.op(dve, lambda c=c: nc.vector.tensor_tensor(out=tt1[:, 0:n], in0=acc[:, c, 0:n],
                                                                      in1=mean_sb[:, 0:n], op=ALU.subtract),
                             r=[t_accs[c], t_ln], w=[t_tt])
                        k.op(dve, lambda: nc.vector.tensor_tensor(out=tt2[:, 0:n], in0=tt1[:, 0:n], in1=rstd_c[:, 0:n],
                                                                  op=ALU.mult), r=[t_tt, t_ln], w=[t_tt])
                        k.op(act, lambda c=c: nc.scalar.activation(out=mixc[:, c, t0:t0 + n], in_=tt2[:, 0:n], func=AF.Silu,
                                                                   scale=pv(PV_LG, c), bias=pv(PV_LB, c)),
                             r=[t_tt, t_c], w=[t_mixc[j]])
                    for c in range(4):
                        pq_, tq_ = proj(1024 + c * 128, 128)
                        k.op(act, lambda c=c, pq_=pq_: nc.scalar.copy(out=qdn[:, c, 0:n], in_=pq_[:, 0:n]), r=[tq_], w=[t_qdn])
                    rstd_from([qdn[:, c, 0:n] for c in range(4)], n, o512, sqb, t_sqb, rstd_q, t_rq, [t_qdn])
                    for c in range(4):
                        k.op(dve, lambda c=c: nc.vector.scalar_tensor_tensor(
                            out=qn[:, c, 0:n], in0=qdn[:, c, 0:n], scalar=pv(PV_QN, c), in1=rstd_q[:, 0:n],
                            op0=ALU.mult, op1=ALU.mult), r=[t_qdn, t_rq, t_c], w=[t_qn])
                    k.dma(sp, d_rope, rc_t[64:96, 0:n], ropec_d[:, t0:t0 + n], w=[t_rope])
                    k.dma(sp, d_rope, rs_t[64:96, 0:n], ropes_d[:, t0:t0 + n], w=[t_rope])
                    for h in range(8):
                        pa_, ta_ = ps()
                        pb_, tb_ = ps()

                        def mmq(h=h, pa_=pa_, pb_=pb_):
                            ins = None
                            for c in range(4):
                                nc.tensor.matmul(pa_[0:96, 0:n], wuqb[:, c, h * 96:(h + 1) * 96], qn[:, c, 0:n],
                                                 start=(c == 0), stop=(c == 3))
                            for c in range(4):
                                ins = nc.tensor.matmul(pb_[0:96, 0:n], wuqs[:, c, h, :], qn[:, c, 0:n],
                                                       start=(c == 0), stop=(c == 3))
                            return ins
                        k.op(pe, mmq, r=[t_qn, t_w], w=[ta_, tb_])
                        k.op(act, lambda h=h, pa_=pa_: nc.scalar.copy(out=qall[0:64, h, t0:t0 + n], in_=pa_[0:64, 0:n]),
                             r=[ta_], w=[t_qall[j]])
                        k.op(dve, lambda pa_=pa_: nc.vector.tensor_tensor(out=tt1[64:96, 0:n], in0=pa_[64:96, 0:n],
                                                                          in1=rc_t[64:96, 0:n], op=ALU.mult),
                             r=[ta_, t_rope], w=[t_tt])
                        k.op(dve, lambda pb_=pb_: nc.vector.tensor_tensor(out=tt2[64:96, 0:n], in0=pb_[64:96, 0:n],
                                                                          in1=rs_t[64:96, 0:n], op=ALU.mult),
                             r=[tb_, t_rope], w=[t_tt])
                        k.op(dve, lambda h=h: nc.vector.tensor_tensor(out=qall[64:96, h, t0:t0 + n], in0=tt1[64:96, 0:n],
                                                                      in1=tt2[64:96, 0:n], op=ALU.add),
                             r=[t_tt], w=[t_qall[j]])
                    for c in range(2):
                        pk_, tk_ = proj(1536 + c * 128, 128)
                        k.op(act, lambda c=c, pk_=pk_: nc.scalar.copy(out=kvf[:, c, 0:n], in_=pk_[:, 0:n]), r=[tk_], w=[t_kvf])
                    rstd_from([kvf[:, c, 0:n] for c in range(2)], n, o256, sqb, t_sqb, rstd_k, t_rk, [t_kvf])
                    for c in range(2):
                        k.op(dve, lambda c=c: nc.vector.scalar_tensor_tensor(
                            out=kvf[:, c, 0:n], in0=kvf[:, c, 0:n], scalar=pv(PV_KVN, c), in1=rstd_k[:, 0:n],
                            op0=ALU.mult, op1=ALU.mult), r=[t_kvf, t_rk, t_c], w=[t_kvf])
                        k.op(act, lambda c=c: nc.scalar.copy(out=ckvT[:, c, t0:t0 + n], in_=kvf[:, c, 0:n]),
                             r=[t_kvf], w=[t_ckvT[j]])
                    if j < 4:
                        for s in range(4):
                            pst, tp = ps()

                            def trk(pst=pst, s=s):
                                ins = None
                                for c in range(2):
                                    ins = nc.tensor.transpose(pst[:, c * 128:(c + 1) * 128], kvf[:, c, s * 128:(s + 1) * 128], ident)
                                return ins
                            k.op(pe, trk, r=[t_kvf, t_c], w=[tp])
                            k.op(act, lambda pst=pst, s=s: nc.scalar.copy(out=ckvo[:, s, :], in_=pst[:, 0:256]), r=[tp], w=[t_ckvo])
                        odma(o_ckvp[t0:t0 + 512, :].rearrange("(s p) f -> p s f", p=128), ckvo[:], [t_ckvo])
                    else:
                        pst, tp = ps()

                        def trk2(pst=pst):
                            ins = None
                            for c in range(2):
                                ins = nc.tensor.transpose(pst[0:16, c * 128:(c + 1) * 128], kvf[:, c, 0:16], ident)
                            return ins
                        k.op(pe, trk2, r=[t_kvf, t_c], w=[tp])
                        k.op(act, lambda pst=pst: nc.scalar.copy(out=ckvo[0:16, 0, :], in_=pst[0:16, 0:256]), r=[tp], w=[t_ckvo])
                        odma(o_ckvs, ckvo[0:16, 0, :], [t_ckvo])
                        k.op(dve, lambda: nc.vector.memset(newkv[:], 1.0), w=[t_newkv])
                        for b in range(4):
                            pst, tp = ps()

                            def trk3(pst=pst, b=b):
                                ins = None
                                for c in range(2):
                                    ins = nc.tensor.transpose(pst[0:4, c * 128:(c + 1) * 128], kvf[:, c, 4 * b:4 * b + 4], ident)
                                return ins
                            k.op(pe, trk3, r=[t_kvf, t_c], w=[tp])
                            k.op(act, lambda pst=pst, b=b: nc.scalar.copy(out=newkv[:, b, 0:256], in_=pst[0:4, 0:256]),
                                 r=[tp], w=[t_newkv])
                    pka, tka = proj(0, 96, wkpe, 0)
                    pkb, tkb = proj(0, 96, wkpe, 1)
                    k.op(dve, lambda pka=pka: nc.vector.tensor_tensor(out=tt1[64:96, 0:n], in0=pka[64:96, 0:n],
                                                                      in1=rc_t[64:96, 0:n], op=ALU.mult),
                         r=[tka, t_rope], w=[t_tt])
                    k.op(dve, lambda pkb=pkb: nc.vector.tensor_tensor(out=tt2[64:96, 0:n], in0=pkb[64:96, 0:n],
                                                                      in1=rs_t[64:96, 0:n], op=ALU.mult),
                         r=[tkb, t_rope], w=[t_tt])
                    k.op(dve, lambda: nc.vector.tensor_tensor(out=kpef[64:96, 0:n], in0=tt1[64:96, 0:n],
                                                              in1=tt2[64:96, 0:n], op=ALU.add), r=[t_tt], w=[t_kpef])
                    k.op(act, lambda: nc.scalar.copy(out=kper[64:96, t0:t0 + n], in_=kpef[64:96, 0:n]),
                         r=[t_kpef], w=[t_kper[j]])
                    if j < 4:
                        pst, tp = ps()

                        def trp(pst=pst):
                            ins = None
                            for s in range(4):
                                ins = nc.tensor.transpose(pst[:, s * 32:(s + 1) * 32], kpef[64:96, s * 128:(s + 1) * 128],
                                                          ident[64:96, 64:96])
                            return ins
                        k.op(pe, trp, r=[t_kpef, t_c], w=[tp])
                        k.op(act, lambda pst=pst: nc.scalar.copy(out=kpeo[:].rearrange("p s f -> p (s f)"), in_=pst[:, 0:128]),
                             r=[tp], w=[t_kpeo])
                        odma(o_kpep[t0:t0 + 512, :].rearrange("(s p) f -> p s f", p=128), kpeo[:], [t_kpeo])
                    else:
                        pst, tp = ps()
                        k.op(pe, lambda pst=pst: nc.tensor.transpose(pst[0:16, 0:32], kpef[64:96, 0:16], ident[64:96, 64:96]),
                             r=[t_kpef, t_c], w=[tp])
                        k.op(act, lambda pst=pst: nc.scalar.copy(out=kpeo[0:16, 0, :], in_=pst[0:16, 0:32]), r=[tp], w=[t_kpeo])
                        odma(o_kpes, kpeo[0:16, 0, :], [t_kpeo])
                        k.op(dve, lambda: nc.vector.tensor_copy(out=kpes0[:, :], in_=kpef[64:96, 0:16]), r=[t_kpef], w=[t_kpes0])
                k.barrier()
            if stage <= 1:
                _finish(nc, k, out_ds)
                return nc

            AT = ExitStack()
            with AT:
                mixa = sb(AT, [128, 4, T], BF16, "mixa")
                t_mixa = Tok()
                kh = sb(AT, [96, NP_], BF16, "kh")
                t_kh = Tok()
                VV = [sb(AT, [128, 16, 128], BF16, "VA"), sb(AT, [128, 16, 128], BF16, "VB")]
                t_V = [Tok(), Tok()]
                pts = [sb(AT, [128, 512], BF16, "pt") for _ in range(3)]
                t_pts = [Tok() for _ in range(3)]
                rd = sb(AT, [128, 512], F32, "rd")
                t_rd = Tok()
                k.op(dve, lambda: nc.vector.memset(VV[0][:], 1.0), w=[t_V[0]])
                k.op(dve, lambda: nc.vector.memset(VV[1][:], 1.0), w=[t_V[1]])
                k.op(dve, lambda: nc.vector.tensor_copy(out=kh[64:96, :], in_=kper[64:96, 0:NP_]), r=t_kper, w=[t_kh])
                SA = AT
                if True:
                    wkf = sb(SA, [128, 2, 1024], F32, "wkf")
                    t_wkf = Tok()
                    wukvT = sb(SA, [128, 8, 256], BF16, "wukvT")
                    t_wT = Tok()
                    ptb = sb(SA, [128, 512], I32, "ptb")
                    idxf = sb(SA, [128, 512], F32, "idxf")
                    idx = sb(SA, [128, 512], I32, "idx")
                    t_idx = Tok()
                    d_sa = k.dsem()
                    NPG = 16
                    pgf = [sb(SA, [128, 288], F32, "pgf") for _ in range(NPG)]
                    t_pgf = [Tok() for _ in range(NPG)]
                    d_pg = [k.dsem() for _ in range(NPG)]
                    CT = [sb(SA, [128, 3, 128], BF16, "CT") for _ in range(2)]
                    t_CT = [Tok(), Tok()]
                    pgb = [sb(SA, [128, 257], BF16, "pgb") for _ in range(32)]
                    t_pgb = [Tok() for _ in range(32)]
                    QL = sb(SA, [128, 3, 32], BF16, "QL")
                    t_QL = Tok()
                    PT = sb(SA, [128, 512], BF16, "PT")
                    t_PT = Tok()
                    pn = sb(SA, [4, 32], BF16, "pn")
                    t_pn = Tok()
                    rds = sb(SA, [32, 1], F32, "rds")
                    ol = sb(SA, [32, 256], F32, "ol")
                    t_ol = Tok()
                    olT = sb(SA, [128, 2, 32], BF16, "olT")
                    t_olT = Tok()
                    if stage >= 3:
                        k.dma(sp, d_sa, wkf[:], w_ukv.rearrange("(c p) n -> p c n", p=128), w=[t_wkf])
                        k.dma(sp, d_sa, ptb[:], ptab, w=[t_idx])
                        for h in range(8):
                            pst, tp = ps()

                            def trw(pst=pst, h=h):
                                ins = None
                                for c in range(2):
                                    ins = nc.tensor.transpose(pst[:, c * 128:(c + 1) * 128], wkf[:, c, h * 128:(h + 1) * 128], ident)
                                return ins
                            k.op(pe, trw, r=[t_wkf, t_c], w=[tp])
                            k.op(act, lambda pst=pst, h=h: nc.scalar.copy(out=wukvT[:, h, :], in_=pst[:, 0:256]), r=[tp], w=[t_wT])
                        k.op(dve, lambda: nc.vector.tensor_copy(out=idxf[:], in_=ptb[:]), r=[t_idx], w=[t_idx])
                        k.op(dve, lambda: nc.vector.tensor_scalar(out=idxf[:], in0=idxf[:], scalar1=128.0,
                                                                  scalar2=cst[:, C_IOTA:C_IOTA + 1], op0=ALU.mult, op1=ALU.add),
                             r=[t_idx, t_c], w=[t_idx])
                        k.op(dve, lambda: nc.vector.tensor_copy(out=idx[:], in_=idxf[:]), r=[t_idx], w=[t_idx])
                        for i in range(32):
                            k.op(dve, lambda i=i: nc.vector.memset(pgb[i][:], 1.0), w=[t_pgb[i]])
                        npages = 128 if not SMALLW else 2
                    def sample_gen():
                        for b in range(4):
                            c0 = NP_ + 4 * b
                            pst, tp = ps()

                            def mmql(pst=pst, c0=c0):
                                ins = None
                                for rc in range(2):
                                    for h in range(8):
                                        ins = nc.tensor.matmul(pst[:, rc * 32 + h * 4:rc * 32 + h * 4 + 4],
                                                               wukvT[0:64, h, rc * 128:(rc + 1) * 128],
                                                               qall[0:64, h, c0:c0 + 4], start=True, stop=True)
                                return ins
                            k.op(pe, mmql, r=[t_wT, t_qall[4]], w=[tp])
                            k.op(act, lambda pst=pst: nc.scalar.copy(out=QL[:, 0:2, :],
                                                                     in_=pst[:, 0:64].rearrange("p (r q) -> p r q", r=2)),
                                 r=[tp], w=[t_QL])
                            k.op(dve, lambda c0=c0: nc.vector.tensor_copy(
                                out=QL[0:32, 2, :].rearrange("p (h t) -> p h t", h=8), in_=qall[64:96, :, c0:c0 + 4]),
                                r=[t_qall[4]], w=[t_QL])
                            pso, tpso = banks[7]
                            first = [True]
                            for g0 in range(0, npages, 16):
                                gn = min(16, npages - g0)
                                psS, tpS = banks[4]
                                for s_ in range(gn):
                                    pg = g0 + s_
                                    col = b * 128 + pg
                                    bi = pg % NPG
                                    pb = pg % 32
                                    k.idma(d_pg[bi], pgf[bi][:, 0:288], ccat, idx[:, col:col + 1], r=[t_idx], w=[t_pgf[bi]])
                                    pst, tp = ps()

                                    def trp2(pst=pst, bi=bi):
                                        nc.tensor.transpose(pst[:, 0:128], pgf[bi][:, 0:128], ident)
                                        nc.tensor.transpose(pst[:, 128:256], pgf[bi][:, 128:256], ident)
                                        return nc.tensor.transpose(pst[0:32, 256:384], pgf[bi][:, 256:288], ident)
                                    k.op(pe, trp2, r=[t_pgf[bi], t_c], w=[tp])
                                    ct = CT[pg % 2]
                                    tct = t_CT[pg % 2]
                                    k.op(act, lambda pst=pst, ct=ct: nc.scalar.copy(
                                        out=ct[:, 0:2, :], in_=pst[:, 0:256].rearrange("p (r q) -> p r q", r=2)), r=[tp], w=[tct])
                                    k.op(dve, lambda pst=pst, ct=ct: nc.vector.tensor_copy(out=ct[0:32, 2, :], in_=pst[0:32, 256:384]),
                                         r=[tp], w=[tct])
                                    k.op(dve, lambda bi=bi, pb=pb: nc.vector.tensor_copy(out=pgb[pb][:, 0:256], in_=pgf[bi][:, 0:256]),
                                         r=[t_pgf[bi]], w=[t_pgb[pb]])

                                    def mms(psS=psS, ct=ct, s_=s_):
                                        nc.tensor.matmul(psS[:, s_ * 32:(s_ + 1) * 32], ct[:, 0, :], QL[:, 0, :], start=True, stop=False)
                                        nc.tensor.matmul(psS[:, s_ * 32:(s_ + 1) * 32], ct[:, 1, :], QL[:, 1, :], start=False, stop=False)
                                        return nc.tensor.matmul(psS[:, s_ * 32:(s_ + 1) * 32], ct[0:32, 2, :], QL[0:32, 2, :],
                                                                start=False, stop=True)
                                    k.op(pe, mms, r=[tct, t_QL], w=[tpS])
                                k.op(act, lambda psS=psS, gn=gn: nc.scalar.activation(out=PT[:, 0:gn * 32], in_=psS[:, 0:gn * 32],
                                                                                     func=AF.Exp, scale=SCALE), r=[tpS], w=[t_PT])
                                for s_ in range(gn):
                                    pb = (g0 + s_) % 32
                                    k.op(pe, lambda pso=pso, s_=s_, pb=pb, st=first[0]: nc.tensor.matmul(
                                        pso[0:32, 0:257], PT[:, s_ * 32:(s_ + 1) * 32], pgb[pb][:, :], start=st, stop=False,
                                        skip_group_check=True), r=[t_PT, t_pgb[pb]], w=[tpso])
                                    first[0] = False
                                yield
                            psn, tpn = ps()

                            def mmn(psn=psn, c0=c0, b=b):
                                nc.tensor.matmul(psn[0:4, 0:32], ckvT[:, 0, c0:c0 + 4], QL[:, 0, :], start=True, stop=False)
                                nc.tensor.matmul(psn[0:4, 0:32], ckvT[:, 1, c0:c0 + 4], QL[:, 1, :], start=False, stop=False)
                                return nc.tensor.matmul(psn[0:4, 0:32], kpes0[:, 4 * b:4 * b + 4], QL[0:32, 2, :], start=False, stop=True)
                            k.op(pe, mmn, r=[t_ckvT[4], t_QL, t_kpes0], w=[tpn])
                            k.op(act, lambda psn=psn: nc.scalar.activation(out=pn[:, :], in_=psn[0:4, 0:32], func=AF.Exp, scale=SCALE),
                                 r=[tpn], w=[t_pn])
                            k.op(dve, lambda: nc.vector.tensor_tensor(out=pn[:, :], in0=pn[:, :], in1=mnew[:, :], op=ALU.mult),
                                 r=[t_pn, t_c], w=[t_pn])
                            k.op(pe, lambda pso=pso, b=b, st=first[0]: nc.tensor.matmul(
                                pso[0:32, 0:257], pn[:, :], newkv[:, b, :], start=st, stop=True, skip_group_check=True),
                                r=[t_pn, t_newkv], w=[tpso])
                            k.op(dve, lambda pso=pso: nc.vector.reciprocal(out=rds[:, :], in_=pso[0:32, 256:257]), r=[tpso], w=[t_ol])
                            k.op(dve, lambda pso=pso: nc.vector.tensor_scalar(out=ol[:, :], in0=pso[0:32, 0:256], scalar1=rds[:, 0:1],
                                                                              scalar2=1.0, op0=ALU.mult, op1=ALU.mult),
                                 r=[tpso, t_ol], w=[t_ol])
                            pst, tp = ps()

                            def tro(pst=pst):
                                nc.tensor.transpose(pst[:, 0:32], ol[:, 0:128], ident[0:32, 0:32])
                                return nc.tensor.transpose(pst[:, 32:64], ol[:, 128:256], ident[0:32, 0:32])
                            k.op(pe, tro, r=[t_ol, t_c], w=[tp])
                            k.op(act, lambda pst=pst: nc.scalar.copy(out=olT[:, :, :], in_=pst[:, 0:64].rearrange("p (r q) -> p r q", r=2)),
                                 r=[tp], w=[t_olT])
                            pst, tp = ps()

                            def mmo(pst=pst):
                                ins = None
                                for h in range(8):
                                    for rc in range(2):
                                        ins = nc.tensor.matmul(pst[0:64, h * 4:h * 4 + 4], wukv[:, rc, h * 128 + 64:h * 128 + 128],
                                                               olT[:, rc, h * 4:h * 4 + 4], start=(rc == 0), stop=(rc == 1))
                                return ins
                            k.op(pe, mmo, r=[t_olT, t_wukv], w=[tp])
                            pv_ = pst[0:64, 0:32].rearrange("p (g e t) -> p g e t", g=4, e=2)
                            k.op(act, lambda pv_=pv_, c0=c0: nc.scalar.copy(out=mixa[0:64, :, c0:c0 + 4], in_=pv_[:, :, 0, :]),
                                 r=[tp], w=[t_mixa])
                            k.op(dve, lambda pv_=pv_, c0=c0: nc.vector.tensor_copy(out=mixa[64:128, :, c0:c0 + 4], in_=pv_[:, :, 1, :]),
                                 r=[tp], w=[t_mixa])
                            yield

                pti = [0]
                nrot[0] = 4
                sgen = sample_gen() if stage >= 3 else iter(())
                for h in range(8 if stage >= 2 else 0):
                    V = VV[h % 2]
                    tV = t_V[h % 2]
                    noff = 0 if h % 2 == 0 else 64
                    doff = 64 - noff
                    for j in range(4):
                        t0 = j * 512
                        pst, tp = ps()

                        def mmk(pst=pst, t0=t0, h=h):
                            ins = None
                            for c in range(2):
                                ins = nc.tensor.matmul(pst[0:64, 0:512], wukv[:, c, h * 128:h * 128 + 64],
                                                       ckvT[:, c, t0:t0 + 512], start=(c == 0), stop=(c == 1))
                            return ins
                        k.op(pe, mmk, r=[t_wukv, t_ckvT[j]], w=[tp])
                        k.op(act, lambda pst=pst, t0=t0: nc.scalar.copy(out=kh[0:64, t0:t0 + 512], in_=pst[0:64, 0:512]),
                             r=[tp], w=[t_kh])
                    for half in range(2):
                        pst, tp = ps()

                        def mmv(pst=pst, half=half, h=h):
                            ins = None
                            for kt in range(8):
                                for c in range(2):
                                    ins = nc.tensor.matmul(pst[:, kt * 64:(kt + 1) * 64],
                                                           ckvT[:, c, (half * 8 + kt) * 128:(half * 8 + kt + 1) * 128],
                                                           wukv[:, c, h * 128 + 64:h * 128 + 128], start=(c == 0), stop=(c == 1))
                            return ins
                        k.op(pe, mmv, r=[t_wukv] + t_ckvT[0:4], w=[tp])
                        k.op(act, lambda pst=pst, half=half, V=V, noff=noff: nc.scalar.copy(
                            out=V[:, half * 8:(half + 1) * 8, noff:noff + 64],
                            in_=pst[:, 0:512].rearrange("p (k d) -> p k d", k=8)), r=[tp], w=[tV])
                    for j in range(4):
                        po, tpo = psacc()
                        nkt = 4 * j + 4
                        for kt in range(nkt):
                            qlo = max(0, kt * 128 - j * 512)
                            pss, tps = ps()
                            k.op(pe, lambda pss=pss, kt=kt, qlo=qlo, j=j, h=h: nc.tensor.matmul(
                                pss[:, qlo:512], kh[:, kt * 128:(kt + 1) * 128], qall[:, h, j * 512 + qlo:(j + 1) * 512],
                                start=True, stop=True), r=[t_kh, t_qall[j]], w=[tps])
                            pt = pts[pti[0] % 3]
                            tpt = t_pts[pti[0] % 3]
                            pti[0] += 1
                            k.op(act, lambda pss=pss, pt=pt, qlo=qlo: nc.scalar.activation(
                                out=pt[:, qlo:512], in_=pss[:, qlo:512], func=AF.Exp, scale=SCALE), r=[tps], w=[tpt])
                            if kt >= 4 * j:
                                k.op(dve, lambda pt=pt, qlo=qlo: nc.vector.tensor_tensor(
                                    out=pt[:, qlo:qlo + 128], in0=pt[:, qlo:qlo + 128], in1=tri[:], op=ALU.mult),
                                    r=[tpt, t_c], w=[tpt])
                            k.op(pe, lambda po=po, pt=pt, kt=kt, qlo=qlo, nkt=nkt, V=V: nc.tensor.matmul(
                                po[:, qlo:512], V[:, kt, :], pt[:, qlo:512], start=(kt == 0), stop=(kt == nkt - 1),
                                skip_group_check=True), r=[tpt, tV], w=[tpo])
                        k.op(dve, lambda po=po, noff=noff, doff=doff: nc.vector.reciprocal(
                            out=rd[noff:noff + 64, :], in_=po[doff:doff + 64, :]), r=[tpo], w=[t_rd])
                        k.op(dve, lambda po=po, noff=noff, h=h, j=j: nc.vector.tensor_tensor(
                            out=mixa[noff:noff + 64, h // 2, j * 512:(j + 1) * 512], in0=po[noff:noff + 64, :],
                            in1=rd[noff:noff + 64, :], op=ALU.mult), r=[tpo, t_rd], w=[t_mixa])
                        next(sgen, None)

                for _ in sgen:
                    pass
                k.barrier()
                nrot[0] = 6
                OP = ExitStack()
                with OP:
                    woutb = sb(OP, [128, 8, 1024], BF16, "woutb")
                    t_wo = Tok()
                    wload(d_w0, woutb[:], w_out.rearrange("(c p) n -> p c n", p=128), [t_wo])
                    xin2 = [sb(OP, [128, 1024], F32, "xin2") for _ in range(2)]
                    t_xin2 = [Tok(), Tok()]
                    d_xin2 = [k.dsem(), k.dsem()]
                    xt = sb(OP, [128, 8, 512], F32, "xt")
                    t_xt = Tok()
                    d_xs = k.dsem()
                    if stage >= 4:
                        for j, (t0, n) in enumerate(TILES):
                            nsub = (n + 127) // 128
                            for s in range(nsub):
                                rows = min(128, n - s * 128)
                                bi = (j * 4 + s) % 2
                                src = xp[t0 + s * 128:t0 + s * 128 + rows, :] if j < 4 else xs[:, :]
                                k.dma(sp, d_xin2[bi], xin2[bi][0:rows, :], src, w=[t_xin2[bi]])
                                for half in range(2):
                                    pst, tp = ps()

                                    def tr(bi=bi, rows=rows, half=half, pst=pst):
                                        ins = None
                                        for cc in range(4):
                                            c = half * 4 + cc
                                            ins = nc.tensor.transpose(pst[:, cc * 128:cc * 128 + rows],
                                                                      xin2[bi][0:rows, c * 128:(c + 1) * 128], ident[0:rows, 0:rows])
                                        return ins
                                    k.op(pe, tr, r=[t_xin2[bi], t_c], w=[tp])
                                    k.op(act, lambda half=half, s=s, rows=rows, pst=pst: nc.scalar.copy(
                                        out=xt[:, half * 4:half * 4 + 4, s * 128:s * 128 + rows],
                                        in_=pst[:, 0:512].rearrange("p (c t) -> p c t", c=4)[:, :, 0:rows]), r=[tp], w=[t_xt])
                            for oc in range(8):
                                pst, tp = ps()

                                def mmo2(pst=pst, oc=oc, t0=t0, n=n):
                                    ins = None
                                    for c in range(8):
                                        rhs = mixc[:, c, t0:t0 + n] if c < 4 else mixa[:, c - 4, t0:t0 + n]
                                        ins = nc.tensor.matmul(pst[:, 0:n], woutb[:, c, oc * 128:(oc + 1) * 128], rhs,
                                                               start=(c == 0), stop=(c == 7))
                                    return ins
                                k.op(pe, mmo2, r=[t_wo, t_mixc[j], t_mixa], w=[tp])
                                k.op(dve, lambda pst=pst, oc=oc, n=n: nc.vector.tensor_tensor(
                                    out=xt[:, oc, 0:n], in0=pst[:, 0:n], in1=xt[:, oc, 0:n], op=ALU.add), r=[tp, t_xt], w=[t_xt])
                            k.dma(sp, d_xs, xscr[:, :, t0:t0 + n], xt[:, :, 0:n], r=[t_xt])
                    k.barrier()


        xfm = sb(es, [128, 8, T], F32, "xfm")
        t_x = [Tok() for _ in TILES]
        d_x = k.dsem()
        for j, (t0, n) in enumerate(TILES):
            k.dma(sp, d_x, xfm[:, :, t0:t0 + n], xscr[:, :, t0:t0 + n], w=[t_x[j]])
        for j in range(len(TILES)):
            t_x[j].w = (d_x, d_x.cnt)
        sq2 = sb(es, [128, 8, 512], BF16, "sq2")
        t_sq2 = Tok()
        rstd2 = sb(es, [128, 512], F32, "rstd2")
        t_r2 = Tok()

        def fm_rstd(j):
            t0, n = TILES[j]
            rstd_from([xfm[:, c, t0:t0 + n] for c in range(8)], n, o1024, sq2, t_sq2, rstd2, t_r2, [t_x[j]])

        d_wgu = [k.dsem(), k.dsem()]
        d_wd = [k.dsem(), k.dsem()]
        hb = t_hb = wgu = t_wgu = wdb = t_wdb = hid = t_hid = sgt = t_sg = tmpm = t_tm = gbc = t_gbc = None
        gi = [0]

        def alloc_ff(FF):
            nonlocal hb, t_hb, wgu, t_wgu, wdb, t_wdb, hid, t_hid, sgt, t_sg, tmpm, t_tm, gbc, t_gbc
            hb = sb(FF, [128, 8, T], BF16, "hb")
            t_hb = [Tok() for _ in TILES]
            wgu = [sb(FF, [128, 2, 8, 512], BF16, "wgu") for _ in range(2)]
            t_wgu = [Tok(), Tok()]
            wdb = [sb(FF, [128, 4, 1024], BF16, "wdb") for _ in range(2)]
            t_wdb = [Tok(), Tok()]
            hid = sb(FF, [128, 4, T], BF16, "hid")
            t_hid = [Tok() for _ in TILES]
            sgt = sb(FF, [128, 512], F32, "sgt")
            t_sg = Tok()
            tmpm = sb(FF, [128, 512], F32, "tmpm")
            t_tm = Tok()
            gbc = sb(FF, [128, T], F32, "gbc")
            t_gbc = Tok()
        if True:

            def gated_ffn(Wg, Wu, Wd, dff, use_gate):
                nch = dff // 128
                for g0 in range(0, nch, 4):
                    gc = min(4, nch - g0)
                    bi = gi[0] % 2
                    gi[0] += 1
                    f0 = g0 * 128
                    wload(d_wgu[bi], wgu[bi][:, 0, :, 0:gc * 128], Wg[:, f0:f0 + gc * 128].rearrange("(c p) n -> p c n", p=128), [t_wgu[bi]])
                    wload(d_wgu[bi], wgu[bi][:, 1, :, 0:gc * 128], Wu[:, f0:f0 + gc * 128].rearrange("(c p) n -> p c n", p=128), [t_wgu[bi]])
                    wload(d_wd[bi], wdb[bi][:, 0:gc, :], Wd[f0:f0 + gc * 128, :].rearrange("(c p) n -> p c n", p=128), [t_wdb[bi]])
                    for j, (t0, n) in enumerate(TILES):
                        for fc in range(gc):
                            pg_, tg_ = ps()
                            pu_, tu_ = ps()

                            def mmgu(pg_=pg_, pu_=pu_, fc=fc, t0=t0, n=n, bi=bi):
                                ins = None
                                for c in range(8):
                                    nc.tensor.matmul(pg_[:, 0:n], wgu[bi][:, 0, c, fc * 128:(fc + 1) * 128], hb[:, c, t0:t0 + n],
                                                     start=(c == 0), stop=(c == 7))
                                for c in range(8):
                                    ins = nc.tensor.matmul(pu_[:, 0:n], wgu[bi][:, 1, c, fc * 128:(fc + 1) * 128], hb[:, c, t0:t0 + n],
                                                           start=(c == 0), stop=(c == 7))
                                return ins
                            k.op(pe, mmgu, r=[t_wgu[bi], t_hb[j]], w=[tg_, tu_])
                            k.op(act, lambda pg_=pg_, n=n: nc.scalar.activation(out=sgt[:, 0:n], in_=pg_[:, 0:n], func=AF.Silu),
                                 r=[tg_], w=[t_sg])
                            if use_gate:
                                k.op(dve, lambda pu_=pu_, n=n: nc.vector.tensor_tensor(out=tmpm[:, 0:n], in0=pu_[:, 0:n], in1=sgt[:, 0:n],
                                                                                       op=ALU.mult), r=[tu_, t_sg], w=[t_tm])
                                k.op(dve, lambda fc=fc, t0=t0, n=n: nc.vector.tensor_tensor(
                                    out=hid[:, fc, t0:t0 + n], in0=tmpm[:, 0:n], in1=gbc[:, t0:t0 + n], op=ALU.mult),
                                    r=[t_tm, t_gbc], w=[t_hid[j]])
                            else:
                                k.op(dve, lambda pu_=pu_, fc=fc, t0=t0, n=n: nc.vector.tensor_tensor(
                                    out=hid[:, fc, t0:t0 + n], in0=pu_[:, 0:n], in1=sgt[:, 0:n], op=ALU.mult),
                                    r=[tu_, t_sg], w=[t_hid[j]])
                        for oc in range(8):
                            pd_, td_ = ps()

                            def mmd(pd_=pd_, oc=oc, t0=t0, n=n, bi=bi, gc=gc):
                                ins = None
                                for fc in range(gc):
                                    ins = nc.tensor.matmul(pd_[:, 0:n], wdb[bi][:, fc, oc * 128:(oc + 1) * 128], hid[:, fc, t0:t0 + n],
                                                           start=(fc == 0), stop=(fc == gc - 1))
                                return ins
                            k.op(pe, mmd, r=[t_wdb[bi], t_hid[j]], w=[td_])
                            k.op(dve, lambda pd_=pd_, oc=oc, t0=t0, n=n: nc.vector.tensor_tensor(
                                out=xfm[:, oc, t0:t0 + n], in0=pd_[:, 0:n], in1=xfm[:, oc, t0:t0 + n], op=ALU.add),
                                r=[td_, t_x[j]], w=[t_x[j]])

            def norm_to_hb(pvcol):
                for j, (t0, n) in enumerate(TILES):
                    fm_rstd(j)
                    for c in range(8):
                        k.op(dve, lambda c=c, t0=t0, n=n: nc.vector.scalar_tensor_tensor(
                            out=hb[:, c, t0:t0 + n], in0=xfm[:, c, t0:t0 + n], scalar=pv(pvcol, c), in1=rstd2[:, 0:n],
                            op0=ALU.mult, op1=ALU.mult), r=[t_x[j], t_r2, t_c], w=[t_hb[j]])

            if stage >= 5:
                FF1 = ExitStack()
                with FF1:
                    alloc_ff(FF1)
                    norm_to_hb(PV_NF0)
                    gated_ffn(ffn_g, ffn_u, ffn_d, DFF, False)
                    k.barrier()

            if stage >= 6:
                PM = ExitStack()
                with PM:
                    tmpm = sb(PM, [128, 512], F32, "tmpm2")
                    t_tm = Tok()
                    pwb = sb(PM, [128, 4, 2, 256], BF16, "pwb")
                    t_pw = Tok()
                    wload(d_w0, pwb[:], pool_w.rearrange("g (c p) n -> p g c n", p=128), [t_pw])
                    L = 15 + 512
                    hf = sb(PM, [128, 8, L], F32, "hf")
                    t_hf = Tok()
                    sA = sb(PM, [128, 2, L], F32, "sA")
                    sB = sb(PM, [128, 2, L], F32, "sB")
                    t_s = Tok()
                    dif = sb(PM, [128, 8, 512], BF16, "dif")
                    t_dif = Tok()
                    hs = sb(PM, [128, 8, 4, 19], F32, "hs")
                    t_hs = Tok()
                    sAs = sb(PM, [128, 2, 4, 19], F32, "sAs")
                    sBs = sb(PM, [128, 2, 4, 19], F32, "sBs")
                    spl = sb(PM, [60, 1024], F32, "spl")
                    t_spl = Tok()
                    pout = sb(PM, [15, 1024], F32, "pout")
                    t_pout = Tok()
                    pouts = sb(PM, [4, 4, 1024], F32, "pouts")
                    t_pouts = Tok()
                    d_pm = k.dsem()
                    k.dma(sp, d_pm, spl[:], spool, w=[t_spl])
                    for c in range(8):
                        pst, tp = ps()
                        k.op(pe, lambda pst=pst, c=c: nc.tensor.transpose(pst[:, 0:60], spl[:, c * 128:(c + 1) * 128], ident[0:60, 0:60]),
                             r=[t_spl, t_c], w=[tp])
                        k.op(act, lambda pst=pst, c=c: nc.scalar.copy(out=hs[:, c, :, 0:15],
                                                                    in_=pst[:, 0:60].rearrange("p (b s) -> p b s", b=4)), r=[tp], w=[t_hs])
                    k.op(dve, lambda: nc.vector.memset(hf[:, :, 0:15], 0.0), w=[t_hf])
                    for j, (t0, n) in enumerate(TILES):
                        fm_rstd(j)
                        samp = (j == 4)
                        for c in range(8):
                            if not samp:
                                dst = hf[:, c, 15:15 + n]
                                xin_ = xfm[:, c, t0:t0 + n]
                                rin = rstd2[:, 0:n]
                            else:
                                dst = hs[:, c, :, 15:19]
                                xin_ = xfm[:, c, t0:t0 + 16].rearrange("p (b t) -> p b t", b=4)
                                rin = rstd2[:, 0:16].rearrange("p (b t) -> p b t", b=4)
                            k.op(dve, lambda c=c, dst=dst, xin_=xin_, rin=rin: nc.vector.scalar_tensor_tensor(
                                out=dst, in0=xin_, scalar=pv(PV_NM1, c), in1=rin, op0=ALU.mult, op1=ALU.mult),
                                r=[t_x[j], t_r2, t_c], w=[t_hs if samp else t_hf])
                        th = t_hs if samp else t_hf
                        if j == 3:
                            for half in range(2):
                                pst, tp = ps()

                                def trh(pst=pst, half=half):
                                    ins = None
                                    for cc in range(4):
                                        ins = nc.tensor.transpose(pst[0:15, cc * 128:(cc + 1) * 128], hf[:, half * 4 + cc, 512:527], ident)
                                    return ins
                                k.op(pe, trh, r=[t_hf, t_c], w=[tp])
                                k.op(act, lambda pst=pst, half=half: nc.scalar.copy(out=pout[:, half * 512:(half + 1) * 512], in_=pst[0:15, :]),
                                     r=[tp], w=[t_pout])
                            odma(o_poolp, pout[:], [t_pout])
                        if samp:
                            for b in range(4):
                                for half in range(2):
                                    pst, tp = ps()

                                    def trh2(pst=pst, half=half, b=b):
                                        ins = None
                                        for cc in range(4):
                                            ins = nc.tensor.transpose(pst[0:4, cc * 128:(cc + 1) * 128], hs[:, half * 4 + cc, b, 15:19], ident)
                                        return ins
                                    k.op(pe, trh2, r=[t_hs, t_c], w=[tp])
                                    k.op(act, lambda pst=pst, half=half, b=b: nc.scalar.copy(
                                        out=pouts[:, b, half * 512:(half + 1) * 512], in_=pst[0:4, :]), r=[tp], w=[t_pouts])
                            odma(o_pools[:, 11:15, :].rearrange("b t f -> t b f"), pouts[:], [t_pouts])
                            odma(o_pools[:, 0:11, :], spool.rearrange("(b s) f -> b s f", b=4)[:, 4:15, :], [])
                        for g in range(4):
                            w_ = 2 << g
                            if not samp:
                                cur = hf[:, 2 * g:2 * g + 2, :]
                                A, B = sA[:, :, :], sB[:, :, :]
                                LL = 15 + n
                            else:
                                cur = hs[:, 2 * g:2 * g + 2, :, :]
                                A, B = sAs[:, :, :, :], sBs[:, :, :, :]
                                LL = 19
                            sh = 1
                            src = cur
                            tsrc = th
                            flip = 0
                            while sh < w_:
                                dst = A if flip == 0 else B
                                if not samp:
                                    o_, a_, b_ = dst[:, :, sh:LL], src[:, :, sh:LL], src[:, :, 0:LL - sh]
                                else:
                                    o_, a_, b_ = dst[:, :, :, sh:LL], src[:, :, :, sh:LL], src[:, :, :, 0:LL - sh]
                                k.op(dve, lambda o_=o_, a_=a_, b_=b_: nc.vector.tensor_tensor(out=o_, in0=a_, in1=b_, op=ALU.add),
                                     r=[tsrc, t_s], w=[t_s])
                                src = dst
                                tsrc = t_s
                                flip ^= 1
                                sh *= 2
                            if not samp:
                                sw = src[:, :, 15:15 + n]
                                hh = hf[:, 2 * g:2 * g + 2, 15:15 + n]
                                dd = dif[:, 2 * g:2 * g + 2, 0:n]
                            else:
                                sw = src[:, :, :, 15:19]
                                hh = hs[:, 2 * g:2 * g + 2, :, 15:19]
                                dd = dif[:, 2 * g:2 * g + 2, 0:16].rearrange("p c (b t) -> p c b t", b=4)
                            k.op(dve, lambda sw=sw, hh=hh, dd=dd, w_=w_: nc.vector.scalar_tensor_tensor(
                                out=dd, in0=sw, scalar=1.0 / w_, in1=hh, op0=ALU.mult, op1=ALU.subtract), r=[t_s, th], w=[t_dif])
                            if j == 0:
                                for cc in range(2):
                                    k.op(dve, lambda cc=cc, g=g, src=src: nc.vector.tensor_tensor(
                                        out=tmpm[:, 0:16], in0=src[:, cc, 15:31], in1=cst[:, C_INV + g * 16:C_INV + g * 16 + 16],
                                        op=ALU.mult), r=[t_s, t_c], w=[t_tm])
                                    k.op(dve, lambda cc=cc, g=g: nc.vector.tensor_tensor(
                                        out=dif[:, 2 * g + cc, 0:16], in0=tmpm[:, 0:16], in1=hf[:, 2 * g + cc, 15:31], op=ALU.subtract),
                                        r=[t_tm, t_hf], w=[t_dif])
                        for g in range(4):
                            for oc in range(2):
                                pst, tp = ps()

                                def mmp(pst=pst, g=g, oc=oc, n=n):
                                    ins = None
                                    for c in range(2):
                                        ins = nc.tensor.matmul(pst[:, 0:n], pwb[:, g, c, oc * 128:(oc + 1) * 128], dif[:, 2 * g + c, 0:n],
                                                               start=(c == 0), stop=(c == 1))
                                    return ins
                                k.op(pe, mmp, r=[t_pw, t_dif], w=[tp])
                                ch = 2 * g + oc
                                k.op(dve, lambda pst=pst, ch=ch, t0=t0, n=n: nc.vector.scalar_tensor_tensor(
                                    out=xfm[:, ch, t0:t0 + n], in0=pst[:, 0:n], scalar=pv(PV_PS, ch), in1=xfm[:, ch, t0:t0 + n],
                                    op0=ALU.mult, op1=ALU.add), r=[tp, t_x[j], t_c], w=[t_x[j]])
                        if j < 3:
                            k.op(dve, lambda: nc.vector.tensor_copy(out=hf[:, :, 0:15], in_=hf[:, :, 512:527]), r=[t_dif, t_s], w=[t_hf])
                    k.barrier()

            if stage >= 7:
                MO = ExitStack()
                with MO:
                    alloc_ff(MO)
                    rw = sb(MO, [128, 8, 8], F32, "rw")
                    t_rw = Tok()
                    d_mo = k.dsem()
                    k.dma(sp, d_mo, rw[:], router_w.rearrange("(c p) e -> p c e", p=128), w=[t_rw])
                    hn = sb(MO, [128, 2, 512], F32, "hn")
                    t_hn = [Tok(), Tok()]
                    lgT = sb(MO, [8, 512], F32, "lgT")
                    t_lgT = Tok()
                    lg = sb(MO, [128, 17, 8], F32, "lg")
                    t_lg = Tok()
                    gT = sb(MO, [8, T], F32, "gT")
                    t_gT = Tok()
                    m1 = sb(MO, [128, 17], F32, "m1")
                    m2 = sb(MO, [128, 17], F32, "m2")
                    wk = sb(MO, [128, 17, 8], F32, "wk")
                    eq1 = sb(MO, [128, 17, 8], F32, "eq1")
                    eq2 = sb(MO, [128, 17, 8], F32, "eq2")
                    g1 = sb(MO, [128, 17], F32, "g1")
                    g2 = sb(MO, [128, 17], F32, "g2")
                    sel = sb(MO, [8, 8, 128], F32, "sel")
                    t_sel = Tok()
                    k.op(dve, lambda: nc.vector.memset(lg[:], 0.0), w=[t_lg])
                    for e in range(8):
                        k.op(dve, lambda e=e: nc.vector.tensor_copy(out=sel[:, e, :], in_=cst[0:8, C_ID + e:C_ID + e + 1].broadcast_to([8, 128])),
                             r=[t_c], w=[t_sel])
                    for j, (t0, n) in enumerate(TILES):
                        fm_rstd(j)
                        nsub = (n + 127) // 128
                        pst, tp = ps()
                        for c in range(8):
                            hb_ = c % 2
                            k.op(dve, lambda c=c, t0=t0, n=n, hb_=hb_: nc.vector.scalar_tensor_tensor(
                                out=hn[:, hb_, 0:n], in0=xfm[:, c, t0:t0 + n], scalar=pv(PV_NF1, c), in1=rstd2[:, 0:n],
                                op0=ALU.mult, op1=ALU.mult), r=[t_x[j], t_r2, t_c], w=[t_hn[hb_]])
                            k.op(act, lambda c=c, t0=t0, n=n, hb_=hb_: nc.scalar.copy(out=hb[:, c, t0:t0 + n], in_=hn[:, hb_, 0:n]),
                                 r=[t_hn[hb_]], w=[t_hb[j]])

                            k.op(pe, lambda pst=pst, n=n, c=c, hb_=hb_: nc.tensor.matmul(
                                pst[0:8, 0:n], rw[:, c, :], hn[:, hb_, 0:n], start=(c == 0), stop=(c == 7)),
                                r=[t_hn[hb_], t_rw], w=[tp])
                        k.op(act, lambda pst=pst, n=n: nc.scalar.copy(out=lgT[:, 0:n], in_=pst[0:8, 0:n]), r=[tp], w=[t_lgT])
                        pst2, tp2 = ps()

                        def trl(pst2=pst2, nsub=nsub, n=n):
                            ins = None
                            for s_ in range(nsub):
                                rows = min(128, n - s_ * 128)
                                ins = nc.tensor.transpose(pst2[0:rows, s_ * 8:s_ * 8 + 8], lgT[0:8, s_ * 128:s_ * 128 + rows], ident[0:8, 0:8])
                            return ins
                        k.op(pe, trl, r=[t_lgT, t_c], w=[tp2])
                        rows_all = min(128, n)
                        k.op(act, lambda pst2=pst2, j=j, nsub=nsub, rows_all=rows_all: nc.scalar.copy(
                            out=lg[0:rows_all, j * 4:j * 4 + nsub, :], in_=pst2[0:rows_all, 0:nsub * 8].rearrange("p (s e) -> p s e", e=8)),
                            r=[tp2], w=[t_lg])
                    X = mybir.AxisListType.X
                    k.op(dve, lambda: nc.vector.tensor_reduce(out=m1[:], in_=lg[:], axis=X, op=ALU.max), r=[t_lg], w=[t_lg])
                    k.op(dve, lambda: nc.vector.tensor_tensor(out=eq1[:], in0=lg[:], in1=m1[:].unsqueeze(2).broadcast_to([128, 17, 8]),
                                                              op=ALU.is_equal), r=[t_lg], w=[t_lg])
                    k.op(dve, lambda: nc.vector.scalar_tensor_tensor(out=wk[:], in0=eq1[:], scalar=-1e30, in1=lg[:], op0=ALU.mult,
                                                                     op1=ALU.add), r=[t_lg], w=[t_lg])
                    k.op(dve, lambda: nc.vector.tensor_reduce(out=m2[:], in_=wk[:], axis=X, op=ALU.max), r=[t_lg], w=[t_lg])
                    k.op(dve, lambda: nc.vector.tensor_tensor(out=eq2[:], in0=wk[:], in1=m2[:].unsqueeze(2).broadcast_to([128, 17, 8]),
                                                              op=ALU.is_equal), r=[t_lg], w=[t_lg])
                    k.op(dve, lambda: nc.vector.tensor_tensor(out=g1[:], in0=m1[:], in1=m2[:], op=ALU.subtract), r=[t_lg], w=[t_lg])
                    k.op(act, lambda: nc.scalar.activation(out=g1[:], in_=g1[:], func=AF.Sigmoid), r=[t_lg], w=[t_lg])
                    k.op(dve, lambda: nc.vector.tensor_scalar(out=g2[:], in0=g1[:], scalar1=-1.0, scalar2=1.0, op0=ALU.mult, op1=ALU.add),
                         r=[t_lg], w=[t_lg])
                    k.op(dve, lambda: nc.vector.tensor_tensor(out=eq1[:], in0=eq1[:], in1=g1[:].unsqueeze(2).broadcast_to([128, 17, 8]),
                                                              op=ALU.mult), r=[t_lg], w=[t_lg])
                    k.op(dve, lambda: nc.vector.tensor_tensor(out=eq2[:], in0=eq2[:], in1=g2[:].unsqueeze(2).broadcast_to([128, 17, 8]),
                                                              op=ALU.mult), r=[t_lg], w=[t_lg])
                    k.op(dve, lambda: nc.vector.tensor_tensor(out=wk[:], in0=eq1[:], in1=eq2[:], op=ALU.add), r=[t_lg], w=[t_lg])
                    for st in range(17):
                        rows = 128 if st < 16 else 16
                        pst, tp = ps()
                        k.op(pe, lambda pst=pst, st=st, rows=rows: nc.tensor.transpose(pst[0:8, 0:rows], wk[0:rows, st, :], ident[0:rows, 0:rows]),
                             r=[t_lg, t_c], w=[tp])
                        k.op(act, lambda pst=pst, st=st, rows=rows: nc.scalar.copy(out=gT[:, st * 128:st * 128 + rows], in_=pst[0:8, 0:rows]),
                             r=[tp], w=[t_gT])
                    for e in range(NE if not SMALLW else 1):
                        for j, (t0, n) in enumerate(TILES):
                            pst, tp = ps()
                            k.op(pe, lambda pst=pst, e=e, t0=t0, n=n: nc.tensor.matmul(pst[:, 0:n], sel[:, e, :], gT[:, t0:t0 + n],
                                                                                     start=True, stop=True), r=[t_sel, t_gT], w=[tp])
                            k.op(act, lambda pst=pst, t0=t0, n=n: nc.scalar.copy(out=gbc[:, t0:t0 + n], in_=pst[:, 0:n]), r=[tp], w=[t_gbc])
                        gated_ffn(moe_g[e], moe_u[e], moe_d[e], DFE, True)
                    k.barrier()

        if stage >= 8:
            FN = ExitStack()
            with FN:
                yt = sb(FN, [128, 8, 512], F32, "yt")
                t_yt = Tok()
                yo = [sb(FN, [128, 1024], F32, "yo") for _ in range(2)]
                t_yo = [Tok(), Tok()]
                for j, (t0, n) in enumerate(TILES):
                    fm_rstd(j)
                    for c in range(8):
                        k.op(dve, lambda c=c, t0=t0, n=n: nc.vector.scalar_tensor_tensor(
                            out=yt[:, c, 0:n], in0=xfm[:, c, t0:t0 + n], scalar=pv(PV_NFIN, c), in1=rstd2[:, 0:n],
                            op0=ALU.mult, op1=ALU.mult), r=[t_x[j], t_r2, t_c], w=[t_yt])
                    nsub = (n + 127) // 128
                    for s_ in range(nsub):
                        rows = min(128, n - s_ * 128)
                        bi = (j * 4 + s_) % 2
                        for half in range(2):
                            pst, tp = ps()

                            def try_(pst=pst, half=half, s_=s_, rows=rows):
                                ins = None
                                for cc in range(4):
                                    ins = nc.tensor.transpose(pst[0:rows, cc * 128:(cc + 1) * 128], yt[:, half * 4 + cc, s_ * 128:s_ * 128 + rows], ident)
                                return ins
                            k.op(pe, try_, r=[t_yt, t_c], w=[tp])
                            k.op(act, lambda pst=pst, half=half, rows=rows, bi=bi: nc.scalar.copy(
                                out=yo[bi][0:rows, half * 512:(half + 1) * 512], in_=pst[0:rows, :]), r=[tp], w=[t_yo[bi]])
                        dst = o_yp[t0 + s_ * 128:t0 + s_ * 128 + rows, :] if j < 4 else o_ys[:, :]
                        odma(dst, yo[bi][0:rows, :], [t_yo[bi]])
        _finish(nc, k, out_ds)
    return nc


def _finish(nc, k, out_ds):
    print("K ops:", k.n, {e.name: e.cnt for e in [k.pe, k.act, k.dve, k.pool, k.sp]})
    for d in out_ds:
        if d.cnt:
            k.sp.h.wait_ge(d.sem, d.cnt)


def _host_consts():
    cst = np.zeros((128, C_W), np.float32)
    cst[:, C_ID:C_ID + 128] = np.eye(128, dtype=np.float32)
    kk = np.arange(128)[:, None]
    qq = np.arange(128)[None, :]
    cst[:, C_TRI:C_TRI + 128] = (kk <= qq).astype(np.float32)
    for kx in range(4):
        for h in range(8):
            for t in range(4):
                cst[kx, C_MN + h * 4 + t] = 1.0 if kx <= t else 0.0
    for g, w in enumerate((2, 4, 8, 16)):
        for p in range(16):
            cst[:, C_INV + g * 16 + p] = 1.0 / min(p + 1, w)
    cst[:, C_IOTA] = np.arange(128, dtype=np.float32)
    half = 16
    inv_freq = np.power(np.float32(10000.0), -np.arange(half, dtype=np.float32) / np.float32(half)).astype(np.float32)
    pos = np.concatenate([np.arange(NP_, dtype=np.float32),
                          np.tile(16384 + np.arange(4, dtype=np.float32), 4)]).astype(np.float32)
    ang = (pos[None, :] * inv_freq[:, None]).astype(np.float32)
    cos = np.cos(ang.astype(np.float64)).astype(np.float32)
    sin = np.sin(ang.astype(np.float64)).astype(np.float32)
    ropec = np.concatenate([cos, cos], 0)
    ropes = np.concatenate([-sin, sin], 0)
    return cst, np.ascontiguousarray(ropec), np.ascontiguousarray(ropes)


def _fm(v):
    return np.ascontiguousarray(np.asarray(v, np.float32).reshape(-1, 128).T)


def kernel(stage=99, limit=10 ** 9, **inp):
    f = lambda a: np.ascontiguousarray(np.asarray(a, dtype=np.float32))
    cst, ropec, ropes = _host_consts()
    pvec = np.zeros((128, PV_W), np.float32)
    pvec[:, PV_NM0:PV_NM0 + 8] = _fm(inp["norm_mix"][0])
    pvec[:, PV_NF0:PV_NF0 + 8] = _fm(inp["norm_ffn"][0])
    pvec[:, PV_NM1:PV_NM1 + 8] = _fm(inp["norm_mix"][1])
    pvec[:, PV_NF1:PV_NF1 + 8] = _fm(inp["norm_ffn"][1])
    pvec[:, PV_NFIN:PV_NFIN + 8] = _fm(inp["norm_final"])
    pvec[:, PV_CB:PV_CB + 4] = _fm(inp["conv_b"][0])
    pvec[:, PV_LG:PV_LG + 4] = _fm(inp["conv_ln_g"][0])
    pvec[:, PV_LB:PV_LB + 4] = _fm(inp["conv_ln_b"][0])
    pvec[:, PV_QN:PV_QN + 4] = _fm(inp["q_norm"][0])
    pvec[:, PV_KVN:PV_KVN + 2] = _fm(inp["kv_norm"][0])
    pvec[:, PV_PS:PV_PS + 8] = _fm(inp["pool_scale"][0])
    cw = np.asarray(inp["conv_w"][0], np.float32)
    pvec[:, PV_CW:PV_CW + 124] = cw.reshape(31, 4, 128).transpose(2, 0, 1).reshape(128, 124)
    shared = {
        "ccat": np.concatenate([f(inp["cache_ckv"][0][:CACHE_PAGES]).reshape(CACHE_PAGES * 128, 256),
                                f(inp["cache_kpe"][0][:CACHE_PAGES]).reshape(CACHE_PAGES * 128, 32)], axis=1),
        "pvec": pvec, "cst": cst, "ropec": ropec, "ropes": ropes,
        "w_in": f(inp["w_in"][0]), "w_uq": f(inp["w_uq"][0]), "w_ukv": f(inp["w_ukv"][0]), "w_out": f(inp["w_out"][0]),
        "ffn_g": f(inp["ffn_w_gate"][0]), "ffn_u": f(inp["ffn_w_up"][0]), "ffn_d": f(inp["ffn_w_down"][0]),
        "pool_w": f(inp["pool_w"][0]), "router_w": f(inp["router_w"][0]),
        "moe_g": f(inp["moe_w_gate"][0][:(1 if SMALLW else NE)]), "moe_u": f(inp["moe_w_up"][0][:(1 if SMALLW else NE)]),
        "moe_d": f(inp["moe_w_down"][0][:(1 if SMALLW else NE)]),
    }
    xpr = f(inp["x_prompt"])
    xsa = f(inp["x_sample"])
    pt = np.ascontiguousarray(np.asarray(inp["page_table"], dtype=np.int32))
    sc = f(inp["state_conv"][0])
    spl = f(inp["state_pool"][0])
    in_maps = []
    for c in range(8):
        m = dict(shared)
        m["xp"] = xpr[c]
        m["xs"] = np.ascontiguousarray(xsa[4 * c:4 * c + 4].reshape(16, 1024))
        m["ptab"] = np.ascontiguousarray(np.broadcast_to(pt[4 * c:4 * c + 4].reshape(1, 512), (128, 512)))
        m["sconv"] = np.ascontiguousarray(sc[4 * c:4 * c + 4].reshape(120, 512))
        m["spool"] = np.ascontiguousarray(spl[4 * c:4 * c + 4].reshape(60, 1024))
        in_maps.append(m)
    nc = build(stage, limit)
    res = run_bass_kernel_spmd(nc, in_maps, core_ids=list(range(8)))
    R = res.results
    cat = lambda key: np.stack([np.asarray(R[c][key], np.float32) for c in range(8)], 0)
    y_p = cat("o_yp")
    y_s = cat("o_ys").reshape(32, 4, 1024)
    ckv_p = cat("o_ckvp")[None]
    kpe_p = cat("o_kpep")[None]
    conv_p = cat("o_convp")[None]
    pool_p = cat("o_poolp")[None]
    ckv_s = cat("o_ckvs").reshape(1, 32, 4, 256)
    kpe_s = cat("o_kpes").reshape(1, 32, 4, 32)
    conv_s = cat("o_convs").reshape(1, 32, 30, 512)
    pool_s = cat("o_pools").reshape(1, 32, 15, 1024)
    return (y_p, y_s, ckv_p, kpe_p, conv_p, pool_p, ckv_s, kpe_s, conv_s, pool_s)
```

```python
from contextlib import ExitStack
import numpy as np
import concourse.bass as bass
import concourse.mybir as mybir
from concourse.bass_utils import run_bass_kernel_spmd

F32 = mybir.dt.float32
BF16 = mybir.dt.bfloat16
I32 = mybir.dt.int32
AF = mybir.ActivationFunctionType
ALU = mybir.AluOpType

NP_ = 2048
NS_ = 16
T = NP_ + NS_
TILES = [(0, 512), (512, 512), (1024, 512), (1536, 512), (2048, 16)]
EPS = 1e-6
SCALE = 96.0 ** -0.5
DFF = 2816
DFE = 3584
NE = 8
CACHE_PAGES = 5120
SMALLW = False
PV_NM0, PV_NF0, PV_NM1, PV_NF1, PV_NFIN = 0, 8, 16, 24, 32
PV_CB, PV_LG, PV_LB, PV_QN, PV_KVN, PV_PS, PV_CW = 40, 44, 48, 52, 56, 58, 66
PV_W = 66 + 124
C_ID, C_TRI, C_MN, C_INV, C_IOTA = 0, 128, 256, 288, 352
C_W = 353


class Eng:
    def __init__(self, name, h, sem):
        self.name, self.h, self.sem, self.cnt, self.seen = name, h, sem, 0, {}


class Tok:
    __slots__ = ("w", "r")

    def __init__(self):
        self.w = None
        self.r = {}


class K:
    def __init__(self, nc, es):
        self.nc = nc
        self.es = es
        mk = lambda n, h: Eng(n, h, es.enter_context(nc.semaphore("s_" + n)))
        self.pe = mk("pe", nc.tensor)
        self.act = mk("act", nc.scalar)
        self.dve = mk("dve", nc.vector)
        self.pool = mk("pool", nc.gpsimd)
        self.sp = mk("sp", nc.sync)
        self.dsems = []
        self.nds = 0
        self.n = 0
        self.limit = 10 ** 9
        self.log = []

    def dsem(self):
        self.nds += 1
        d = Eng("d%d" % self.nds, None, self.es.enter_context(self.nc.semaphore("s_d%d" % self.nds)))
        self.dsems.append(d)
        return d

    def _waits(self, eng, r, w):
        need = {}

        def add(p):
            if p is None:
                return
            e, c = p
            if e is eng and eng is self.pe:
                return
            if need.get(e, 0) < c:
                need[e] = c

        for t in r:
            add(t.w)
        for t in w:
            add(t.w)
            for e, c in t.r.items():
                add((e, c))
        for e, c in need.items():
            if eng.seen.get(e, 0) >= c:
                continue
            eng.h.wait_ge(e.sem, c)
            eng.seen[e] = c

    def op(self, eng, fn, r=(), w=()):
        self.n += 1
        if self.n > self.limit:
            return None
        self._waits(eng, r, w)
        ins = fn()
        eng.cnt += 1
        ins.then_inc(eng.sem, 1)
        for t in r:
            t.r[eng] = eng.cnt
        for t in w:
            t.w = (eng, eng.cnt)
            t.r = {}
        return ins

    def dma(self, q, ds, out, in_, r=(), w=(), **kw):
        self.n += 1
        if self.n > self.limit:
            return None
        self._waits(q, r, w)
        ins = q.h.dma_start(out=out, in_=in_, **kw)
        ds.cnt += 16
        ins.then_inc(ds.sem, 16)
        for t in r:
            t.r[ds] = ds.cnt
        for t in w:
            t.w = (ds, ds.cnt)
            t.r = {}

    def idma(self, ds, out, in_, idx_ap, r=(), w=()):
        q = self.pool
        self.n += 1
        if self.n > self.limit:
            return None
        self._waits(q, r, w)
        ins = q.h.indirect_dma_start(out=out, out_offset=None, in_=in_,
                                     in_offset=bass.IndirectOffsetOnAxis(ap=idx_ap, axis=0))
        ds.cnt += 16
        ins.then_inc(ds.sem, 16)
        for t in r:
            t.r[ds] = ds.cnt
        for t in w:
            t.w = (ds, ds.cnt)
            t.r = {}

    def barrier(self):
        engs = [self.pe, self.act, self.dve, self.pool, self.sp]
        for e in engs:
            for o in engs + self.dsems:
                if o is e or o.cnt == 0:
                    continue
                if e.seen.get(o, 0) < o.cnt:
                    e.h.wait_ge(o.sem, o.cnt)
                    e.seen[o] = o.cnt


def build(stage=99, limit=10 ** 9):
    nc = bass.Bass("TRN2", target_bir_lowering=False)

    def din(name, shape, dt=F32):
        return nc.dram_tensor(name, list(shape), dt, kind="ExternalInput").ap()

    def dout(name, shape):
        return nc.dram_tensor(name, list(shape), F32, kind="ExternalOutput").ap()

    xp = din("xp", [NP_, 1024])
    xs = din("xs", [NS_, 1024])
    ccat = din("ccat", [CACHE_PAGES * 128, 288])
    ptab = din("ptab", [128, 512], I32)
    sconv = din("sconv", [120, 512])
    spool = din("spool", [60, 1024])
    pvec_d = din("pvec", [128, PV_W])
    cst_d = din("cst", [128, C_W])
    ropec_d = din("ropec", [32, T])
    ropes_d = din("ropes", [32, T])
    w_in = din("w_in", [1024, 1824])
    w_uq = din("w_uq", [512, 768])
    w_ukv = din("w_ukv", [256, 1024])
    w_out = din("w_out", [1024, 1024])
    ffn_g = din("ffn_g", [1024, DFF])
    ffn_u = din("ffn_u", [1024, DFF])
    ffn_d = din("ffn_d", [DFF, 1024])
    pool_w = din("pool_w", [4, 256, 256])
    router_w = din("router_w", [1024, 8])
    ne_ = 1 if SMALLW else NE
    moe_g = din("moe_g", [ne_, 1024, DFE])
    moe_u = din("moe_u", [ne_, 1024, DFE])
    moe_d = din("moe_d", [ne_, DFE, 1024])

    o_yp = dout("o_yp", [NP_, 1024])
    o_ys = dout("o_ys", [NS_, 1024])
    o_ckvp = dout("o_ckvp", [NP_, 256])
    o_kpep = dout("o_kpep", [NP_, 32])
    o_convp = dout("o_convp", [30, 512])
    o_poolp = dout("o_poolp", [15, 1024])
    o_ckvs = dout("o_ckvs", [NS_, 256])
    o_kpes = dout("o_kpes", [NS_, 32])
    o_convs = dout("o_convs", [4, 30, 512])
    o_pools = dout("o_pools", [4, 15, 1024])
    xscr = nc.dram_tensor("xscr", [128, 8, T], F32, kind="Internal").ap()

    es = ExitStack()
    with es:
        k = K(nc, es)
        k.limit = limit
        pe, act, dve, pool, sp = k.pe, k.act, k.dve, k.pool, k.sp
        ctr = [0]

        def sb(stack, shape, dt=F32, name=None):
            ctr[0] += 1
            return stack.enter_context(nc.sbuf_tensor("%s_%d" % (name or "t", ctr[0]), list(shape), dt))

        banks = []
        for i in range(8):
            banks.append((es.enter_context(nc.psum_tensor("ps%d" % i, [128, 512], F32)), Tok()))
        pctr = [0]

        nrot = [6]

        def ps():
            b = banks[pctr[0] % nrot[0]]
            pctr[0] += 1
            return b
        actr = [0]

        def psacc():
            b = banks[5 + actr[0] % 2]
            actr[0] += 1
            return b

        out_ds = []
        octr = [0]

        odm = {}

        def odma(out, in_, r):
            key = id(r[0]) if r else 0
            if key not in odm:
                odm[key] = k.dsem()
                out_ds.append(odm[key])
            k.dma(sp, odm[key], out, in_, r=r)

        cst = sb(es, [128, C_W], name="cst")
        pvec = sb(es, [128, PV_W], name="pvec")
        t_c = Tok()
        d_c = k.dsem()
        k.dma(sp, d_c, cst[:], cst_d, w=[t_c])
        k.dma(sp, d_c, pvec[:], pvec_d, w=[t_c])
        ident = cst[:, C_ID:C_ID + 128]
        o1024 = sb(es, [128, 128], BF16, "o1024")
        o512 = sb(es, [128, 128], BF16, "o512")
        o256 = sb(es, [128, 128], BF16, "o256")
        tri = sb(es, [128, 128], BF16, "tri")
        mnew = sb(es, [4, 32], BF16, "mnew")
        k.op(dve, lambda: nc.vector.memset(o1024[:], 1.0 / 1024), w=[t_c])
        k.op(dve, lambda: nc.vector.memset(o512[:], 1.0 / 512), w=[t_c])
        k.op(dve, lambda: nc.vector.memset(o256[:], 1.0 / 256), w=[t_c])
        k.op(dve, lambda: nc.vector.tensor_copy(out=tri[:], in_=cst[:, C_TRI:C_TRI + 128]), r=[t_c], w=[t_c])
        k.op(dve, lambda: nc.vector.tensor_copy(out=mnew[:], in_=cst[0:4, C_MN:C_MN + 32]), r=[t_c], w=[t_c])

        def pv(col, c=0):
            return pvec[:, col + c:col + c + 1]

        def wload(ds, dst, src, w):
            k.dma(pool, ds, dst, src, w=w)
            if ds.cnt >= 48 and pool.seen.get(ds, 0) < ds.cnt - 32:
                pool.h.wait_ge(ds.sem, ds.cnt - 32)
                pool.seen[ds] = ds.cnt - 32

        epsb = sb(es, [128, 1], F32, "epsb")
        k.op(dve, lambda: nc.vector.memset(epsb[:], EPS), w=[t_c])

        def rsqrt_to(out_ap, in_ap, scale, r, t_out):
            np_ = out_ap.shape[0]
            k.op(act, lambda: nc.scalar.activation(out=out_ap, in_=in_ap, func=AF.Sqrt, scale=scale,
                                                   bias=epsb[0:np_, 0:1]), r=list(r) + [t_c], w=[t_out])
            k.op(dve, lambda: nc.vector.reciprocal(out=out_ap, in_=out_ap), r=[t_out], w=[t_out])

        def rstd_from(srcs, n, ones_t, sqbuf, t_sq, rstd_ap, t_rstd, r):
            for i, s in enumerate(srcs):
                k.op(act, (lambda s=s, i=i: nc.scalar.activation(out=sqbuf[:, i, 0:n], in_=s, func=AF.Square)),
                     r=r, w=[t_sq])
            pst, tp = ps()

            def mm():
                ins = None
                for i in range(len(srcs)):
                    ins = nc.tensor.matmul(pst[:, 0:n], ones_t[:], sqbuf[:, i, 0:n], start=(i == 0),
                                           stop=(i == len(srcs) - 1))
                return ins
            k.op(pe, mm, r=[t_sq, t_c], w=[tp])
            rsqrt_to(rstd_ap[:, 0:n], pst[:, 0:n], 1.0, [tp], t_rstd)

        L0 = ExitStack()
        with L0:
            ustore = sb(L0, [128, 4, 30 + NP_], BF16, "ustore")
            t_ust = [Tok() for _ in range(4)]
            mixc_s = sb(L0, [128, 4, NS_], BF16, "mixc_s")
            t_mixc = [Tok() for _ in TILES]
            k.op(dve, lambda: nc.vector.memset(ustore[:], 0.0), w=t_ust)
            ckvT = sb(L0, [128, 2, T], BF16, "ckvT")
            t_ckvT = [Tok() for _ in TILES]
            kper = sb(L0, [128, T], BF16, "kper")
            t_kper = [Tok() for _ in TILES]
            qall = sb(L0, [96, 8, T], BF16, "qall")
            t_qall = [Tok() for _ in TILES]
            kpes0 = sb(L0, [32, NS_], BF16, "kpes0")
            t_kpes0 = Tok()
            newkv = sb(L0, [4, 4, 257], BF16, "newkv")
            t_newkv = Tok()
            wukv = sb(L0, [128, 2, 1024], BF16, "wukv")
            t_wukv = Tok()
            d_w0 = k.dsem()
            wload(d_w0, wukv[:], w_ukv.rearrange("(c p) n -> p c n", p=128), [t_wukv])

            PA = ExitStack()
            with PA:
                winb = sb(PA, [128, 8, 1824], BF16, "winb")
                wkpe = sb(PA, [128, 8, 2, 96], BF16, "wkpe")
                wuqb = sb(PA, [128, 4, 768], BF16, "wuqb")
                wuqs = sb(PA, [128, 4, 8, 96], BF16, "wuqs")
                t_w = Tok()
                k.op(pool, lambda: nc.gpsimd.memset(wkpe[:], 0.0), w=[t_w])
                k.op(pool, lambda: nc.gpsimd.memset(wuqs[:], 0.0), w=[t_w])
                win_v = w_in.rearrange("(c p) n -> p c n", p=128)
                for c in range(8):
                    wload(d_w0, winb[:, c, :], w_in[c * 128:(c + 1) * 128, :], [t_w])
                wload(d_w0, wkpe[:, :, 0, 64:96], win_v[:, :, 1792:1824], [t_w])
                wload(d_w0, wkpe[:, :, 1, 64:80], win_v[:, :, 1808:1824], [t_w])
                wload(d_w0, wkpe[:, :, 1, 80:96], win_v[:, :, 1792:1808], [t_w])
                wuq_v = w_uq.rearrange("(c p) n -> p c n", p=128)
                wload(d_w0, wuqb[:], wuq_v, [t_w])
                wuq_v4 = w_uq.rearrange("(c p) (h d) -> p c h d", p=128, d=96)
                for c in range(4):
                    wload(d_w0, wuqs[:, c, :, 64:80], wuq_v4[:, c, :, 80:96], [t_w])
                    wload(d_w0, wuqs[:, c, :, 80:96], wuq_v4[:, c, :, 64:80], [t_w])

                xin = [sb(PA, [128, 1024], F32, "xin") for _ in range(2)]
                t_xin = [Tok(), Tok()]
                d_xin = [k.dsem(), k.dsem()]
                junk = sb(PA, [128, 1024], BF16, "junk")
                t_junk = Tok()
                ssq = sb(PA, [128, 4], F32, "ssq")
                h0 = sb(PA, [128, 8, 512], BF16, "h0")
                t_h0 = Tok()
                sqb = sb(PA, [128, 8, 512], BF16, "sqb")
                t_sqb = Tok()
                sig = sb(PA, [128, 512], F32, "sig")
                t_sig = Tok()
                uroll = sb(PA, [128, 4, 542], F32, "uroll")
                t_ur = Tok()
                us = sb(PA, [128, 4, 4, 34], F32, "us")
                t_us = Tok()
                acc = sb(PA, [128, 4, 512], F32, "acc")
                t_accs = [Tok() for _ in range(4)]
                mean_sb = sb(PA, [128, 512], F32, "mean")
                var_sb = sb(PA, [128, 512], F32, "var")
                rstd_c = sb(PA, [128, 512], F32, "rstdc")
                t_ln = Tok()
                tt1 = sb(PA, [128, 512], F32, "tt1")
                tt2 = sb(PA, [128, 512], F32, "tt2")
                t_tt = Tok()
                qdn = sb(PA, [128, 4, 512], F32, "qdn")
                t_qdn = Tok()
                rstd_q = sb(PA, [128, 512], F32, "rstdq")
                t_rq = Tok()
                qn = sb(PA, [128, 4, 512], BF16, "qn")
                t_qn = Tok()
                kvf = sb(PA, [128, 2, 512], F32, "kvf")
                t_kvf = Tok()
                rstd_k = sb(PA, [128, 512], F32, "rstdk")
                t_rk = Tok()
                rc_t = sb(PA, [128, 512], F32, "ropec")
                rs_t = sb(PA, [128, 512], F32, "ropes")
                t_rope = Tok()
                d_rope = k.dsem()
                kpef = sb(PA, [128, 512], F32, "kpef")
                t_kpef = Tok()
                ckvo = sb(PA, [128, 4, 256], F32, "ckvo")
                t_ckvo = Tok()
                kpeo = sb(PA, [128, 4, 32], F32, "kpeo")
                t_kpeo = Tok()
                cvo = sig[0:30, :]
                t_cvo = t_sig
                cvs = qdn[0:4, :, :]
                t_cvs = t_qdn
                scv = kpef[0:120, :]
                t_scv = t_kpef
                d_misc = k.dsem()

                k.op(dve, lambda: nc.vector.memset(uroll[:, :, 0:30], 0.0), w=[t_ur])
                k.dma(sp, d_misc, scv[:], sconv, w=[t_scv])
                for c in range(4):
                    pst, tp = ps()
                    k.op(pe, lambda c=c, pst=pst: nc.tensor.transpose(pst[:, 0:120], scv[:, c * 128:(c + 1) * 128],
                                                                      ident[0:120, 0:120]), r=[t_scv, t_c], w=[tp])
                    k.op(act, lambda c=c, pst=pst: nc.scalar.copy(
                        out=us[:, c, :, 0:30], in_=pst[:, 0:120].rearrange("p (b s) -> p b s", b=4)), r=[tp], w=[t_us])

                for j, (t0, n) in enumerate(TILES):
                    nsub = (n + 127) // 128
                    for s in range(nsub):
                        rows = min(128, n - s * 128)
                        bi = (j * 4 + s) % 2
                        src = xp[t0 + s * 128:t0 + s * 128 + rows, :] if j < 4 else xs[:, :]
                        k.dma(sp, d_xin[bi], xin[bi][0:rows, :], src, w=[t_xin[bi]])
                        k.op(act, lambda bi=bi, rows=rows, s=s: nc.scalar.activation(
                            out=junk[0:rows, :], in_=xin[bi][0:rows, :], func=AF.Square,
                            accum_out=ssq[0:rows, s:s + 1]), r=[t_xin[bi]], w=[t_junk])
                        rsqrt_to(ssq[0:rows, s:s + 1], ssq[0:rows, s:s + 1], 1.0 / 1024, [t_junk], t_junk)
                        k.op(dve, lambda bi=bi, rows=rows, s=s: nc.vector.tensor_scalar(
                            out=xin[bi][0:rows, :], in0=xin[bi][0:rows, :], scalar1=ssq[0:rows, s:s + 1], scalar2=1.0,
                            op0=ALU.mult, op1=ALU.mult), r=[t_junk, t_xin[bi]], w=[t_xin[bi]])
                        for half in range(2):
                            pst, tp = ps()

                            def tr(bi=bi, rows=rows, half=half, pst=pst):
                                ins = None
                                for cc in range(4):
                                    c = half * 4 + cc
                                    ins = nc.tensor.transpose(pst[:, cc * 128:cc * 128 + rows],
                                                              xin[bi][0:rows, c * 128:(c + 1) * 128],
                                                              ident[0:rows, 0:rows])
                                return ins
                            k.op(pe, tr, r=[t_xin[bi], t_c], w=[tp])
                            for cc in range(4):
                                c = half * 4 + cc
                                k.op(act, lambda c=c, cc=cc, s=s, rows=rows, pst=pst: nc.scalar.activation(
                                    out=h0[:, c, s * 128:s * 128 + rows], in_=pst[:, cc * 128:cc * 128 + rows],
                                    func=AF.Copy, scale=pv(PV_NM0, c)), r=[tp, t_c], w=[t_h0])

                    def proj(col0, m, wt=None, wsel=None):
                        pst, tp = ps()

                        def mm():
                            ins = None
                            for c in range(8):
                                l = winb[:, c, col0:col0 + m] if wt is None else wt[:, c, wsel, 0:m]
                                ins = nc.tensor.matmul(pst[0:m, 0:n], l, h0[:, c, 0:n], start=(c == 0), stop=(c == 7))
                            return ins
                        k.op(pe, mm, r=[t_h0, t_w], w=[tp])
                        return pst, tp

                    for c in range(4):
                        pa, tpa = proj(c * 128, 128)
                        pg, tpg = proj(512 + c * 128, 128)
                        k.op(act, lambda pg=pg: nc.scalar.activation(out=sig[:, 0:n], in_=pg[:, 0:n], func=AF.Sigmoid),
                             r=[tpg], w=[t_sig])
                        if j < 4:
                            k.op(dve, lambda c=c, pa=pa: nc.vector.tensor_tensor(
                                out=ustore[:, c, 30 + t0:30 + t0 + n], in0=pa[:, 0:n], in1=sig[:, 0:n], op=ALU.mult),
                                r=[tpa, t_sig], w=[t_ust[j]])
                            if j == 3:
                                k.op(dve, lambda c=c, pa=pa: nc.vector.tensor_tensor(
                                    out=uroll[:, c, 512:542], in0=pa[:, 482:512], in1=sig[:, 482:512], op=ALU.mult),
                                    r=[tpa, t_sig], w=[t_ur])
                        else:
                            k.op(dve, lambda c=c, pa=pa: nc.vector.tensor_tensor(
                                out=us[:, c, :, 30:34], in0=pa[:, 0:16].rearrange("p (b t) -> p b t", b=4),
                                in1=sig[:, 0:16].rearrange("p (b t) -> p b t", b=4), op=ALU.mult),
                                r=[tpa, t_sig], w=[t_us])
                    for c in range(4 if j == 4 else 0):
                        def ext(kk, c=c):
                            return uroll[:, c, kk:kk + n] if j < 4 else us[:, c, :, kk:kk + 4]

                        def accv(c=c):
                            return acc[:, c, 0:n] if j < 4 else acc[:, c, 0:16].rearrange("p (b t) -> p b t", b=4)
                        tsrc = t_ur if j < 4 else t_us
                        ce = dve
                        ceh = nc.vector
                        k.op(ce, lambda c=c, ext=ext, accv=accv, ceh=ceh: ceh.tensor_scalar(
                            out=accv(), in0=ext(0), scalar1=pv(PV_CW, c), scalar2=pv(PV_CB, c),
                            op0=ALU.mult, op1=ALU.add), r=[tsrc, t_c], w=[t_accs[c]])
                        for kk in range(1, 31):
                            k.op(ce, lambda c=c, kk=kk, ext=ext, accv=accv, ceh=ceh: ceh.scalar_tensor_tensor(
                                out=accv(), in0=ext(kk), scalar=pv(PV_CW, kk * 4 + c), in1=accv(),
                                op0=ALU.mult, op1=ALU.add), r=[tsrc, t_c, t_accs[c]], w=[t_accs[c]])
                    if j == 3:
                        pst, tp = ps()

                        def trc(pst=pst):
                            ins = None
                            for c in range(4):
                                ins = nc.tensor.transpose(pst[0:30, c * 128:(c + 1) * 128], uroll[:, c, 512:542], ident)
                            return ins
                        k.op(pe, trc, r=[t_ur, t_c], w=[tp])
                        k.op(act, lambda pst=pst: nc.scalar.copy(out=cvo[:], in_=pst[0:30, :]), r=[tp], w=[t_cvo])
                        odma(o_convp, cvo[:], [t_cvo])
                    if j == 4:
                        for b in range(4):
                            pst, tp = ps()

                            def trs(pst=pst, b=b):
                                ins = None
                                for c in range(4):
                                    ins = nc.tensor.transpose(pst[0:4, c * 128:(c + 1) * 128], us[:, c, b, 30:34], ident)
                                return ins
                            k.op(pe, trs, r=[t_us, t_c], w=[tp])
                            k.op(act, lambda pst=pst, b=b: nc.scalar.copy(out=cvs[:, b, :], in_=pst[0:4, :]),
                                 r=[tp], w=[t_cvs])
                        odma(o_convs[:, 26:30, :].rearrange("b t f -> t b f"), cvs[:], [t_cvs])
                        odma(o_convs[:, 0:26, :], sconv.rearrange("(b s) f -> b s f", b=4)[:, 4:30, :], [])
                    if j == 4:
                        for c in range(4):
                            k.op(act, lambda c=c: nc.scalar.copy(out=sqb[:, c, 0:n], in_=acc[:, c, 0:n]), r=[t_accs[c]], w=[t_sqb])
                            k.op(act, lambda c=c: nc.scalar.activation(out=sqb[:, 4 + c, 0:n], in_=acc[:, c, 0:n],
                                                                       func=AF.Square), r=[t_accs[c]], w=[t_sqb])
                        pm, tpm = ps()
                        pq, tpq = ps()

                        def mmst(pm=pm, pq=pq):
                            ins = None
                            for c in range(4):
                                nc.tensor.matmul(pm[:, 0:n], o512[:], sqb[:, c, 0:n], start=(c == 0), stop=(c == 3))
                            for c in range(4):
                                ins = nc.tensor.matmul(pq[:, 0:n], o512[:], sqb[:, 4 + c, 0:n], start=(c == 0), stop=(c == 3))
                            return ins
                        k.op(pe, mmst, r=[t_sqb, t_c], w=[tpm, tpq])
                        k.op(act, lambda pm=pm: nc.scalar.copy(out=mean_sb[:, 0:n], in_=pm[:, 0:n]), r=[tpm], w=[t_ln])
                        k.op(dve, lambda: nc.vector.tensor_tensor(out=var_sb[:, 0:n], in0=mean_sb[:, 0:n], in1=mean_sb[:, 0:n],
                                                                  op=ALU.mult), r=[t_ln], w=[t_ln])
                        k.op(dve, lambda pq=pq: nc.vector.tensor_tensor(out=var_sb[:, 0:n], in0=pq[:, 0:n], in1=var_sb[:, 0:n],
                                                                        op=ALU.subtract), r=[tpq, t_ln], w=[t_ln])
                        rsqrt_to(rstd_c[:, 0:n], var_sb[:, 0:n], 1.0, [t_ln], t_ln)
                        for c in range(4):
                            k.op(dve, lambda c=c: nc.vector.tensor_tensor(out=tt1[:, 0:n], in0=acc[:, c, 0:n],
                                                                          in1=mean_sb[:, 0:n], op=ALU.subtract),
                                 r=[t_accs[c], t_ln], w=[t_tt])
                            k.op(dve, lambda: nc.vector.tensor_tensor(out=tt2[:, 0:n], in0=tt1[:, 0:n], in1=rstd_c[:, 0:n],
                                                                      op=ALU.mult), r=[t_tt, t_ln], w=[t_tt])
                            k.op(act, lambda c=c: nc.scalar.activation(out=mixc_s[:, c, 0:n], in_=tt2[:, 0:n], func=AF.Silu,
                                                                       scale=pv(PV_LG, c), bias=pv(PV_LB, c)),
                                 r=[t_tt, t_c], w=[t_mixc[j]])
                    for c in range(4):
                        pq_, tq_ = proj(1024 + c * 128, 128)
                        k.op(act, lambda c=c, pq_=pq_: nc.scalar.copy(out=qdn[:, c, 0:n], in_=pq_[:, 0:n]), r=[tq_], w=[t_qdn])
                    rstd_from([qdn[:, c, 0:n] for c in range(4)], n, o512, sqb, t_sqb, rstd_q, t_rq, [t_qdn])
                    for c in range(4):
                        k.op(dve, lambda c=c: nc.vector.scalar_tensor_tensor(
                            out=qn[:, c, 0:n], in0=qdn[:, c, 0:n], scalar=pv(PV_QN, c), in1=rstd_q[:, 0:n],
                            op0=ALU.mult, op1=ALU.mult), r=[t_qdn, t_rq, t_c], w=[t_qn])
                    k.dma(sp, d_rope, rc_t[64:96, 0:n], ropec_d[:, t0:t0 + n], w=[t_rope])
                    k.dma(sp, d_rope, rs_t[64:96, 0:n], ropes_d[:, t0:t0 + n], w=[t_rope])
                    for h in range(8):
                        pa_, ta_ = ps()
                        pb_, tb_ = ps()

                        def mmq(h=h, pa_=pa_, pb_=pb_):
                            ins = None
                            for c in range(4):
                                nc.tensor.matmul(pa_[0:96, 0:n], wuqb[:, c, h * 96:(h + 1) * 96], qn[:, c, 0:n],
                                                 start=(c == 0), stop=(c == 3))
                            for c in range(4):
                                ins = nc.tensor.matmul(pb_[0:96, 0:n], wuqs[:, c, h, :], qn[:, c, 0:n],
                                                       start=(c == 0), stop=(c == 3))
                            return ins
                        k.op(pe, mmq, r=[t_qn, t_w], w=[ta_, tb_])
                        k.op(act, lambda h=h, pa_=pa_: nc.scalar.copy(out=qall[0:64, h, t0:t0 + n], in_=pa_[0:64, 0:n]),
                             r=[ta_], w=[t_qall[j]])
                        k.op(dve, lambda pa_=pa_: nc.vector.tensor_tensor(out=tt1[64:96, 0:n], in0=pa_[64:96, 0:n],
                                                                          in1=rc_t[64:96, 0:n], op=ALU.mult),
                             r=[ta_, t_rope], w=[t_tt])
                        k.op(dve, lambda pb_=pb_: nc.vector.tensor_tensor(out=tt2[64:96, 0:n], in0=pb_[64:96, 0:n],
                                                                          in1=rs_t[64:96, 0:n], op=ALU.mult),
                             r=[tb_, t_rope], w=[t_tt])
                        k.op(dve, lambda h=h: nc.vector.tensor_tensor(out=qall[64:96, h, t0:t0 + n], in0=tt1[64:96, 0:n],
                                                                      in1=tt2[64:96, 0:n], op=ALU.add),
                             r=[t_tt], w=[t_qall[j]])
                    for c in range(2):
                        pk_, tk_ = proj(1536 + c * 128, 128)
                        k.op(act, lambda c=c, pk_=pk_: nc.scalar.copy(out=kvf[:, c, 0:n], in_=pk_[:, 0:n]), r=[tk_], w=[t_kvf])
                    rstd_from([kvf[:, c, 0:n] for c in range(2)], n, o256, sqb, t_sqb, rstd_k, t_rk, [t_kvf])
                    for c in range(2):
                        k.op(dve, lambda c=c: nc.vector.scalar_tensor_tensor(
                            out=kvf[:, c, 0:n], in0=kvf[:, c, 0:n], scalar=pv(PV_KVN, c), in1=rstd_k[:, 0:n],
                            op0=ALU.mult, op1=ALU.mult), r=[t_kvf, t_rk, t_c], w=[t_kvf])
                        k.op(act, lambda c=c: nc.scalar.copy(out=ckvT[:, c, t0:t0 + n], in_=kvf[:, c, 0:n]),
                             r=[t_kvf], w=[t_ckvT[j]])
                    if j < 4:
                        for s in range(4):
                            pst, tp = ps()

                            def trk(pst=pst, s=s):
                                ins = None
                                for c in range(2):
                                    ins = nc.tensor.transpose(pst[:, c * 128:(c + 1) * 128], kvf[:, c, s * 128:(s + 1) * 128], ident)
                                return ins
                            k.op(pe, trk, r=[t_kvf, t_c], w=[tp])
                            k.op(act, lambda pst=pst, s=s: nc.scalar.copy(out=ckvo[:, s, :], in_=pst[:, 0:256]), r=[tp], w=[t_ckvo])
                        odma(o_ckvp[t0:t0 + 512, :].rearrange("(s p) f -> p s f", p=128), ckvo[:], [t_ckvo])
                    else:
                        pst, tp = ps()

                        def trk2(pst=pst):
                            ins = None
                            for c in range(2):
                                ins = nc.tensor.transpose(pst[0:16, c * 128:(c + 1) * 128], kvf[:, c, 0:16], ident)
                            return ins
                        k.op(pe, trk2, r=[t_kvf, t_c], w=[tp])
                        k.op(act, lambda pst=pst: nc.scalar.copy(out=ckvo[0:16, 0, :], in_=pst[0:16, 0:256]), r=[tp], w=[t_ckvo])
                        odma(o_ckvs, ckvo[0:16, 0, :], [t_ckvo])
                        k.op(dve, lambda: nc.vector.memset(newkv[:], 1.0), w=[t_newkv])
                        for b in range(4):
                            pst, tp = ps()

                            def trk3(pst=pst, b=b):
                                ins = None
                                for c in range(2):
                                    ins = nc.tensor.transpose(pst[0:4, c * 128:(c + 1) * 128], kvf[:, c, 4 * b:4 * b + 4], ident)
                                return ins
                            k.op(pe, trk3, r=[t_kvf, t_c], w=[tp])
                            k.op(act, lambda pst=pst, b=b: nc.scalar.copy(out=newkv[:, b, 0:256], in_=pst[0:4, 0:256]),
                                 r=[tp], w=[t_newkv])
                    pka, tka = proj(0, 96, wkpe, 0)
                    pkb, tkb = proj(0, 96, wkpe, 1)
                    k.op(dve, lambda pka=pka: nc.vector.tensor_tensor(out=tt1[64:96, 0:n], in0=pka[64:96, 0:n],
                                                                      in1=rc_t[64:96, 0:n], op=ALU.mult),
                         r=[tka, t_rope], w=[t_tt])
                    k.op(dve, lambda pkb=pkb: nc.vector.tensor_tensor(out=tt2[64:96, 0:n], in0=pkb[64:96, 0:n],
                                                                      in1=rs_t[64:96, 0:n], op=ALU.mult),
                         r=[tkb, t_rope], w=[t_tt])
                    k.op(dve, lambda: nc.vector.tensor_tensor(out=kpef[64:96, 0:n], in0=tt1[64:96, 0:n],
                                                              in1=tt2[64:96, 0:n], op=ALU.add), r=[t_tt], w=[t_kpef])
                    k.op(act, lambda: nc.scalar.copy(out=kper[64:96, t0:t0 + n], in_=kpef[64:96, 0:n]),
                         r=[t_kpef], w=[t_kper[j]])
                    if j < 4:
                        pst, tp = ps()

                        def trp(pst=pst):
                            ins = None
                            for s in range(4):
                                ins = nc.tensor.transpose(pst[:, s * 32:(s + 1) * 32], kpef[64:96, s * 128:(s + 1) * 128],
                                                          ident[64:96, 64:96])
                            return ins
                        k.op(pe, trp, r=[t_kpef, t_c], w=[tp])
                        k.op(act, lambda pst=pst: nc.scalar.copy(out=kpeo[:].rearrange("p s f -> p (s f)"), in_=pst[:, 0:128]),
                             r=[tp], w=[t_kpeo])
                        odma(o_kpep[t0:t0 + 512, :].rearrange("(s p) f -> p s f", p=128), kpeo[:], [t_kpeo])
                    else:
                        pst, tp = ps()
                        k.op(pe, lambda pst=pst: nc.tensor.transpose(pst[0:16, 0:32], kpef[64:96, 0:16], ident[64:96, 64:96]),
                             r=[t_kpef, t_c], w=[tp])
                        k.op(act, lambda pst=pst: nc.scalar.copy(out=kpeo[0:16, 0, :], in_=pst[0:16, 0:32]), r=[tp], w=[t_kpeo])
                        odma(o_kpes, kpeo[0:16, 0, :], [t_kpeo])
                        k.op(dve, lambda: nc.vector.tensor_copy(out=kpes0[:, :], in_=kpef[64:96, 0:16]), r=[t_kpef], w=[t_kpes0])
                k.barrier()
            if stage <= 1:
                _finish(nc, k, out_ds)
                return nc

            AT = ExitStack()
            with AT:
                mixa = sb(AT, [128, 4, T], BF16, "mixa")
                t_mixa = Tok()
                kh = sb(AT, [96, NP_], BF16, "kh")
                t_kh = Tok()
                VV = [sb(AT, [128, 16, 128], BF16, "VA"), sb(AT, [128, 16, 128], BF16, "VB")]
                t_V = [Tok(), Tok()]
                mixc = sb(AT, [128, 4, T], BF16, "mixc")
                blk = sb(AT, [128, 4096], F32, "blk")
                acc2 = blk[:, 0:2048].rearrange("p (c t) -> p c t", c=4)
                t_acc2 = [Tok() for _ in range(4)]
                sqb2 = blk[:, 2048:4096].bitcast(BF16).rearrange("p (c t) -> p c t", c=8)
                t_sqb2 = Tok()
                mean2 = sb(AT, [128, 512], F32, "mean2")
                var2 = sb(AT, [128, 512], F32, "var2")
                rstdc2 = sb(AT, [128, 512], F32, "rstdc2")
                t_ln2 = Tok()
                tq1 = sb(AT, [128, 512], F32, "tq1")
                tq2 = sb(AT, [128, 512], F32, "tq2")
                t_tq = Tok()

                def conv_gen():
                    for j in range(4):
                        t0 = j * 512
                        n = 512
                        ru = [t_ust[j]] + ([t_ust[j - 1]] if j > 0 else [])
                        for c in range(4):
                            k.op(dve, lambda c=c, t0=t0: nc.vector.tensor_scalar(
                                out=acc2[:, c, :], in0=ustore[:, c, t0:t0 + 512], scalar1=pv(PV_CW, c), scalar2=pv(PV_CB, c),
                                op0=ALU.mult, op1=ALU.add), r=ru + [t_c], w=[t_acc2[c]])
                            for kk in range(1, 31):
                                k.op(dve, lambda c=c, kk=kk, t0=t0: nc.vector.scalar_tensor_tensor(
                                    out=acc2[:, c, :], in0=ustore[:, c, t0 + kk:t0 + kk + 512], scalar=pv(PV_CW, kk * 4 + c),
                                    in1=acc2[:, c, :], op0=ALU.mult, op1=ALU.add), r=ru + [t_c, t_acc2[c]], w=[t_acc2[c]])
                                if kk % 8 == 0:
                                    yield
                            yield
                        for c in range(4):
                            k.op(act, lambda c=c: nc.scalar.copy(out=sqb2[:, c, :], in_=acc2[:, c, :]), r=[t_acc2[c]], w=[t_sqb2])
                            k.op(act, lambda c=c: nc.scalar.activation(out=sqb2[:, 4 + c, :], in_=acc2[:, c, :], func=AF.Square),
                                 r=[t_acc2[c]], w=[t_sqb2])
                        pm, tpm = ps()
                        pq, tpq = ps()

                        def mmst2(pm=pm, pq=pq):
                            ins = None
                            for c in range(4):
                                nc.tensor.matmul(pm[:, 0:n], o512[:], sqb2[:, c, :], start=(c == 0), stop=(c == 3))
                            for c in range(4):
                                ins = nc.tensor.matmul(pq[:, 0:n], o512[:], sqb2[:, 4 + c, :], start=(c == 0), stop=(c == 3))
                            return ins
                        k.op(pe, mmst2, r=[t_sqb2, t_c], w=[tpm, tpq])
                        k.op(act, lambda pm=pm: nc.scalar.copy(out=mean2[:, :], in_=pm[:, 0:n]), r=[tpm], w=[t_ln2])
                        k.op(dve, lambda: nc.vector.tensor_tensor(out=var2[:, :], in0=mean2[:, :], in1=mean2[:, :], op=ALU.mult),
                             r=[t_ln2], w=[t_ln2])
                        k.op(dve, lambda pq=pq: nc.vector.tensor_tensor(out=var2[:, :], in0=pq[:, 0:n], in1=var2[:, :], op=ALU.subtract),
                             r=[tpq, t_ln2], w=[t_ln2])
                        rsqrt_to(rstdc2[:, :], var2[:, :], 1.0, [t_ln2], t_ln2)
                        for c in range(4):
                            k.op(dve, lambda c=c: nc.vector.tensor_tensor(out=tq1[:, :], in0=acc2[:, c, :], in1=mean2[:, :],
                                                                          op=ALU.subtract), r=[t_acc2[c], t_ln2], w=[t_tq])
                            k.op(dve, lambda: nc.vector.tensor_tensor(out=tq2[:, :], in0=tq1[:, :], in1=rstdc2[:, :], op=ALU.mult),
                                 r=[t_tq, t_ln2], w=[t_tq])
                            k.op(act, lambda c=c, t0=t0: nc.scalar.activation(out=mixc[:, c, t0:t0 + 512], in_=tq2[:, :], func=AF.Silu,
                                                                              scale=pv(PV_LG, c), bias=pv(PV_LB, c)),
                                 r=[t_tq, t_c], w=[t_mixc[j]])
                        yield
                pts = [sb(AT, [128, 512], BF16, "pt") for _ in range(3)]
                t_pts = [Tok() for _ in range(3)]
                rd = sb(AT, [128, 512], F32, "rd")
                t_rd = Tok()
                k.op(dve, lambda: nc.vector.memset(VV[0][:], 1.0), w=[t_V[0]])
                k.op(dve, lambda: nc.vector.memset(VV[1][:], 1.0), w=[t_V[1]])
                k.op(dve, lambda: nc.vector.tensor_copy(out=kh[64:96, :], in_=kper[64:96, 0:NP_]), r=t_kper, w=[t_kh])
                SA = AT
                if True:
                    wkf = sb(SA, [128, 2, 1024], F32, "wkf")
                    t_wkf = Tok()
                    wukvT = sb(SA, [128, 8, 256], BF16, "wukvT")
                    t_wT = Tok()
                    ptb = sb(SA, [128, 512], I32, "ptb")
                    idxf = sb(SA, [128, 512], F32, "idxf")
                    idx = sb(SA, [128, 512], I32, "idx")
                    t_idx = Tok()
                    d_sa = k.dsem()
                    NPG = 8
                    pgf = [sb(SA, [128, 288], F32, "pgf") for _ in range(NPG)]
                    t_pgf = [Tok() for _ in range(NPG)]
                    d_pg = [k.dsem() for _ in range(NPG)]
                    CT = [sb(SA, [128, 3, 128], BF16, "CT") for _ in range(3)]
                    t_CT = [Tok(), Tok(), Tok()]
                    pgb = [sb(SA, [128, 257], BF16, "pgb") for _ in range(16)]
                    t_pgb = [Tok() for _ in range(16)]
                    QL = sb(SA, [128, 3, 32], BF16, "QL")
                    t_QL = Tok()
                    PT = sb(SA, [128, 512], BF16, "PT")
                    t_PT = Tok()
                    pn = sb(SA, [4, 32], BF16, "pn")
                    t_pn = Tok()
                    rds = sb(SA, [32, 1], F32, "rds")
                    ol = sb(SA, [32, 256], F32, "ol")
                    t_ol = Tok()
                    olT = sb(SA, [128, 2, 32], BF16, "olT")
                    t_olT = Tok()
                    if stage >= 3:
                        k.dma(sp, d_sa, wkf[:], w_ukv.rearrange("(c p) n -> p c n", p=128), w=[t_wkf])
                        k.dma(sp, d_sa, ptb[:], ptab, w=[t_idx])
                        for h in range(8):
                            pst, tp = ps()

                            def trw(pst=pst, h=h):
                                ins = None
                                for c in range(2):
                                    ins = nc.tensor.transpose(pst[:, c * 128:(c + 1) * 128], wkf[:, c, h * 128:(h + 1) * 128], ident)
                                return ins
                            k.op(pe, trw, r=[t_wkf, t_c], w=[tp])
                            k.op(act, lambda pst=pst, h=h: nc.scalar.copy(out=wukvT[:, h, :], in_=pst[:, 0:256]), r=[tp], w=[t_wT])
                        k.op(dve, lambda: nc.vector.tensor_copy(out=idxf[:], in_=ptb[:]), r=[t_idx], w=[t_idx])
                        k.op(dve, lambda: nc.vector.tensor_scalar(out=idxf[:], in0=idxf[:], scalar1=128.0,
                                                                  scalar2=cst[:, C_IOTA:C_IOTA + 1], op0=ALU.mult, op1=ALU.add),
                             r=[t_idx, t_c], w=[t_idx])
                        k.op(dve, lambda: nc.vector.tensor_copy(out=idx[:], in_=idxf[:]), r=[t_idx], w=[t_idx])
                        for i in range(16):
                            k.op(dve, lambda i=i: nc.vector.memset(pgb[i][:], 1.0), w=[t_pgb[i]])
                        npages = 128 if not SMALLW else 2
                    def sample_gen():
                        for b in range(4):
                            c0 = NP_ + 4 * b
                            pst, tp = ps()

                            def mmql(pst=pst, c0=c0):
                                ins = None
                                for rc in range(2):
                                    for h in range(8):
                                        ins = nc.tensor.matmul(pst[:, rc * 32 + h * 4:rc * 32 + h * 4 + 4],
                                                               wukvT[0:64, h, rc * 128:(rc + 1) * 128],
                                                               qall[0:64, h, c0:c0 + 4], start=True, stop=True)
                                return ins
                            k.op(pe, mmql, r=[t_wT, t_qall[4]], w=[tp])
                            k.op(act, lambda pst=pst: nc.scalar.copy(out=QL[:, 0:2, :],
                                                                     in_=pst[:, 0:64].rearrange("p (r q) -> p r q", r=2)),
                                 r=[tp], w=[t_QL])
                            k.op(dve, lambda c0=c0: nc.vector.tensor_copy(
                                out=QL[0:32, 2, :].rearrange("p (h t) -> p h t", h=8), in_=qall[64:96, :, c0:c0 + 4]),
                                r=[t_qall[4]], w=[t_QL])
                            pso, tpso = banks[7]
                            first = [True]
                            for g0 in range(0, npages, 16):
                                gn = min(16, npages - g0)
                                psS, tpS = banks[4]
                                pend_s = None
                                for s_ in range(gn):
                                    pg = g0 + s_
                                    col = b * 128 + pg
                                    bi = pg % NPG
                                    pb = pg % 16
                                    k.idma(d_pg[bi], pgf[bi][:, 0:288], ccat, idx[:, col:col + 1], r=[t_idx], w=[t_pgf[bi]])
                                    pst, tp = ps()

                                    def trp2(pst=pst, bi=bi):
                                        nc.tensor.transpose(pst[:, 0:128], pgf[bi][:, 0:128], ident)
                                        nc.tensor.transpose(pst[:, 128:256], pgf[bi][:, 128:256], ident)
                                        return nc.tensor.transpose(pst[0:32, 256:384], pgf[bi][:, 256:288], ident)
                                    k.op(pe, trp2, r=[t_pgf[bi], t_c], w=[tp])
                                    ct = CT[pg % 3]
                                    tct = t_CT[pg % 3]
                                    k.op(act, lambda pst=pst, ct=ct: nc.scalar.copy(
                                        out=ct[:, 0:2, :], in_=pst[:, 0:256].rearrange("p (r q) -> p r q", r=2)), r=[tp], w=[tct])
                                    k.op(dve, lambda pst=pst, ct=ct: nc.vector.tensor_copy(out=ct[0:32, 2, :], in_=pst[0:32, 256:384]),
                                         r=[tp], w=[tct])
                                    k.op(dve, lambda bi=bi, pb=pb: nc.vector.tensor_copy(out=pgb[pb][:, 0:256], in_=pgf[bi][:, 0:256]),
                                         r=[t_pgf[bi]], w=[t_pgb[pb]])

                                    def mms(psS=psS, ct=ct, s_=s_):
                                        nc.tensor.matmul(psS[:, s_ * 32:(s_ + 1) * 32], ct[:, 0, :], QL[:, 0, :], start=True, stop=False)
                                        nc.tensor.matmul(psS[:, s_ * 32:(s_ + 1) * 32], ct[:, 1, :], QL[:, 1, :], start=False, stop=False)
                                        return nc.tensor.matmul(psS[:, s_ * 32:(s_ + 1) * 32], ct[0:32, 2, :], QL[0:32, 2, :],
                                                                start=False, stop=True)
                                    if pend_s is not None:
                                        k.op(pe, pend_s[0], r=[pend_s[1], t_QL], w=[tpS])
                                    pend_s = (mms, tct)
                                k.op(pe, pend_s[0], r=[pend_s[1], t_QL], w=[tpS])
                                pend_s = None
                                k.op(act, lambda psS=psS, gn=gn: nc.scalar.activation(out=PT[:, 0:gn * 32], in_=psS[:, 0:gn * 32],
                                                                                     func=AF.Exp, scale=SCALE), r=[tpS], w=[t_PT])
                                for s_ in range(gn):
                                    pb = (g0 + s_) % 16
                                    k.op(pe, lambda pso=pso, s_=s_, pb=pb, st=first[0]: nc.tensor.matmul(
                                        pso[0:32, 0:257], PT[:, s_ * 32:(s_ + 1) * 32], pgb[pb][:, :], start=st, stop=False,
                                        skip_group_check=True), r=[t_PT, t_pgb[pb]], w=[tpso])
                                    first[0] = False
                                yield
                            psn, tpn = ps()

                            def mmn(psn=psn, c0=c0, b=b):
                                nc.tensor.matmul(psn[0:4, 0:32], ckvT[:, 0, c0:c0 + 4], QL[:, 0, :], start=True, stop=False)
                                nc.tensor.matmul(psn[0:4, 0:32], ckvT[:, 1, c0:c0 + 4], QL[:, 1, :], start=False, stop=False)
                                return nc.tensor.matmul(psn[0:4, 0:32], kpes0[:, 4 * b:4 * b + 4], QL[0:32, 2, :], start=False, stop=True)
                            k.op(pe, mmn, r=[t_ckvT[4], t_QL, t_kpes0], w=[tpn])
                            k.op(act, lambda psn=psn: nc.scalar.activation(out=pn[:, :], in_=psn[0:4, 0:32], func=AF.Exp, scale=SCALE),
                                 r=[tpn], w=[t_pn])
                            k.op(dve, lambda: nc.vector.tensor_tensor(out=pn[:, :], in0=pn[:, :], in1=mnew[:, :], op=ALU.mult),
                                 r=[t_pn, t_c], w=[t_pn])
                            k.op(pe, lambda pso=pso, b=b, st=first[0]: nc.tensor.matmul(
                                pso[0:32, 0:257], pn[:, :], newkv[:, b, :], start=st, stop=True, skip_group_check=True),
                                r=[t_pn, t_newkv], w=[tpso])
                            k.op(dve, lambda pso=pso: nc.vector.reciprocal(out=rds[:, :], in_=pso[0:32, 256:257]), r=[tpso], w=[t_ol])
                            k.op(dve, lambda pso=pso: nc.vector.tensor_scalar(out=ol[:, :], in0=pso[0:32, 0:256], scalar1=rds[:, 0:1],
                                                                              scalar2=1.0, op0=ALU.mult, op1=ALU.mult),
                                 r=[tpso, t_ol], w=[t_ol])
                            pst, tp = ps()

                            def tro(pst=pst):
                                nc.tensor.transpose(pst[:, 0:32], ol[:, 0:128], ident[0:32, 0:32])
                                return nc.tensor.transpose(pst[:, 32:64], ol[:, 128:256], ident[0:32, 0:32])
                            k.op(pe, tro, r=[t_ol, t_c], w=[tp])
                            k.op(act, lambda pst=pst: nc.scalar.copy(out=olT[:, :, :], in_=pst[:, 0:64].rearrange("p (r q) -> p r q", r=2)),
                                 r=[tp], w=[t_olT])
                            pst, tp = ps()

                            def mmo(pst=pst):
                                ins = None
                                for h in range(8):
                                    for rc in range(2):
                                        ins = nc.tensor.matmul(pst[0:64, h * 4:h * 4 + 4], wukv[:, rc, h * 128 + 64:h * 128 + 128],
                                                               olT[:, rc, h * 4:h * 4 + 4], start=(rc == 0), stop=(rc == 1))
                                return ins
                            k.op(pe, mmo, r=[t_olT, t_wukv], w=[tp])
                            pv_ = pst[0:64, 0:32].rearrange("p (g e t) -> p g e t", g=4, e=2)
                            k.op(act, lambda pv_=pv_, c0=c0: nc.scalar.copy(out=mixa[0:64, :, c0:c0 + 4], in_=pv_[:, :, 0, :]),
                                 r=[tp], w=[t_mixa])
                            k.op(dve, lambda pv_=pv_, c0=c0: nc.vector.tensor_copy(out=mixa[64:128, :, c0:c0 + 4], in_=pv_[:, :, 1, :]),
                                 r=[tp], w=[t_mixa])
                            yield

                pti = [0]
                nrot[0] = 4
                sgen = sample_gen() if stage >= 3 else iter(())
                cgen = conv_gen()
                for h in range(8 if stage >= 2 else 0):
                    V = VV[h % 2]
                    tV = t_V[h % 2]
                    noff = 0 if h % 2 == 0 else 64
                    doff = 64 - noff
                    for j in range(4):
                        t0 = j * 512
                        pst, tp = ps()

                        def mmk(pst=pst, t0=t0, h=h):
                            ins = None
                            for c in range(2):
                                ins = nc.tensor.matmul(pst[0:64, 0:512], wukv[:, c, h * 128:h * 128 + 64],
                                                       ckvT[:, c, t0:t0 + 512], start=(c == 0), stop=(c == 1))
                            return ins
                        k.op(pe, mmk, r=[t_wukv, t_ckvT[j]], w=[tp])
                        k.op(act, lambda pst=pst, t0=t0: nc.scalar.copy(out=kh[0:64, t0:t0 + 512], in_=pst[0:64, 0:512]),
                             r=[tp], w=[t_kh])
                    for half in range(2):
                        pst, tp = ps()

                        def mmv(pst=pst, half=half, h=h):
                            ins = None
                            for kt in range(8):
                                for c in range(2):
                                    ins = nc.tensor.matmul(pst[:, kt * 64:(kt + 1) * 64],
                                                           ckvT[:, c, (half * 8 + kt) * 128:(half * 8 + kt + 1) * 128],
                                                           wukv[:, c, h * 128 + 64:h * 128 + 128], start=(c == 0), stop=(c == 1))
                            return ins
                        k.op(pe, mmv, r=[t_wukv] + t_ckvT[0:4], w=[tp])
                        k.op(act, lambda pst=pst, half=half, V=V, noff=noff: nc.scalar.copy(
                            out=V[:, half * 8:(half + 1) * 8, noff:noff + 64],
                            in_=pst[:, 0:512].rearrange("p (k d) -> p k d", k=8)), r=[tp], w=[tV])
                    for j in range(4):
                        po, tpo = psacc()
                        nkt = 4 * j + 4
                        pend = None

                        def emit_pv(pt, tpt, kt, qlo, po=po, tpo=tpo, nkt=nkt, V=V, tV=tV):
                            k.op(pe, lambda: nc.tensor.matmul(
                                po[:, qlo:512], V[:, kt, :], pt[:, qlo:512], start=(kt == 0), stop=(kt == nkt - 1),
                                skip_group_check=True), r=[tpt, tV], w=[tpo])
                        for kt in range(nkt):
                            qlo = max(0, kt * 128 - j * 512)
                            pss, tps = ps()
                            k.op(pe, lambda pss=pss, kt=kt, qlo=qlo, j=j, h=h: nc.tensor.matmul(
                                pss[:, qlo:512], kh[:, kt * 128:(kt + 1) * 128], qall[:, h, j * 512 + qlo:(j + 1) * 512],
                                start=True, stop=True), r=[t_kh, t_qall[j]], w=[tps])
                            pt = pts[pti[0] % 3]
                            tpt = t_pts[pti[0] % 3]
                            pti[0] += 1
                            k.op(act, lambda pss=pss, pt=pt, qlo=qlo: nc.scalar.activation(
                                out=pt[:, qlo:512], in_=pss[:, qlo:512], func=AF.Exp, scale=SCALE), r=[tps], w=[tpt])
                            if kt >= 4 * j:
                                k.op(dve, lambda pt=pt, qlo=qlo: nc.vector.tensor_tensor(
                                    out=pt[:, qlo:qlo + 128], in0=pt[:, qlo:qlo + 128], in1=tri[:], op=ALU.mult),
                                    r=[tpt, t_c], w=[tpt])
                            if pend is not None:
                                emit_pv(*pend)
                            pend = (pt, tpt, kt, qlo)
                        emit_pv(*pend)
                        k.op(dve, lambda po=po, noff=noff, doff=doff: nc.vector.reciprocal(
                            out=rd[noff:noff + 64, :], in_=po[doff:doff + 64, :]), r=[tpo], w=[t_rd])
                        k.op(dve, lambda po=po, noff=noff, h=h, j=j: nc.vector.tensor_tensor(
                            out=mixa[noff:noff + 64, h // 2, j * 512:(j + 1) * 512], in0=po[noff:noff + 64, :],
                            in1=rd[noff:noff + 64, :], op=ALU.mult), r=[tpo, t_rd], w=[t_mixa])
                        next(sgen, None)
                        next(cgen, None)
                        next(cgen, None)
                        next(cgen, None)

                for _ in sgen:
                    pass
                for _ in cgen:
                    pass
                k.barrier()
                nrot[0] = 6
                OP = ExitStack()
                with OP:
                    woutb = sb(OP, [128, 8, 1024], BF16, "woutb")
                    t_wo = Tok()
                    wload(d_w0, woutb[:], w_out.rearrange("(c p) n -> p c n", p=128), [t_wo])
                    xin2 = [wkf[:, 0, :], wkf[:, 1, :]]
                    t_xin2 = [Tok(), Tok()]
                    d_xin2 = [k.dsem(), k.dsem()]
                    xt = blk[:, :].rearrange("p (c t) -> p c t", c=8)
                    t_xt = Tok()
                    d_xs = k.dsem()
                    if stage >= 4:
                        for j, (t0, n) in enumerate(TILES):
                            nsub = (n + 127) // 128
                            for s in range(nsub):
                                rows = min(128, n - s * 128)
                                bi = (j * 4 + s) % 2
                                src = xp[t0 + s * 128:t0 + s * 128 + rows, :] if j < 4 else xs[:, :]
                                k.dma(sp, d_xin2[bi], xin2[bi][0:rows, :], src, w=[t_xin2[bi]])
                                for half in range(2):
                                    pst, tp = ps()

                                    def tr(bi=bi, rows=rows, half=half, pst=pst):
                                        ins = None
                                        for cc in range(4):
                                            c = half * 4 + cc
                                            ins = nc.tensor.transpose(pst[:, cc * 128:cc * 128 + rows],
                                                                      xin2[bi][0:rows, c * 128:(c + 1) * 128], ident[0:rows, 0:rows])
                                        return ins
                                    k.op(pe, tr, r=[t_xin2[bi], t_c], w=[tp])
                                    k.op(act, lambda half=half, s=s, rows=rows, pst=pst: nc.scalar.copy(
                                        out=xt[:, half * 4:half * 4 + 4, s * 128:s * 128 + rows],
                                        in_=pst[:, 0:512].rearrange("p (c t) -> p c t", c=4)[:, :, 0:rows]), r=[tp], w=[t_xt])
                            for oc in range(8):
                                pst, tp = ps()

                                def mmo2(pst=pst, oc=oc, t0=t0, n=n, j=j):
                                    ins = None
                                    for c in range(8):
                                        rhs = (mixc[:, c, t0:t0 + n] if j < 4 else mixc_s[:, c, 0:n]) if c < 4 else mixa[:, c - 4, t0:t0 + n]
                                        ins = nc.tensor.matmul(pst[:, 0:n], woutb[:, c, oc * 128:(oc + 1) * 128], rhs,
                                                               start=(c == 0), stop=(c == 7))
                                    return ins
                                k.op(pe, mmo2, r=[t_wo, t_mixc[j], t_mixa], w=[tp])
                                k.op(dve, lambda pst=pst, oc=oc, n=n: nc.vector.tensor_tensor(
                                    out=xt[:, oc, 0:n], in0=pst[:, 0:n], in1=xt[:, oc, 0:n], op=ALU.add), r=[tp, t_xt], w=[t_xt])
                            k.dma(sp, d_xs, xscr[:, :, t0:t0 + n], xt[:, :, 0:n], r=[t_xt])
                    k.barrier()


        xfm = sb(es, [128, 8, T], F32, "xfm")
        t_x = [Tok() for _ in TILES]
        d_x = k.dsem()
        for j, (t0, n) in enumerate(TILES):
            k.dma(sp, d_x, xfm[:, :, t0:t0 + n], xscr[:, :, t0:t0 + n], w=[t_x[j]])
        for j in range(len(TILES)):
            t_x[j].w = (d_x, d_x.cnt)
        sq2 = sb(es, [128, 8, 512], BF16, "sq2")
        t_sq2 = Tok()
        rstd2 = sb(es, [128, 512], F32, "rstd2")
        t_r2 = Tok()

        def fm_rstd(j):
            t0, n = TILES[j]
            rstd_from([xfm[:, c, t0:t0 + n] for c in range(8)], n, o1024, sq2, t_sq2, rstd2, t_r2, [t_x[j]])

        d_wgu = [k.dsem(), k.dsem()]
        d_wd = [k.dsem(), k.dsem()]
        hb = t_hb = wgu = t_wgu = wdb = t_wdb = hid = t_hid = sgt = t_sg = tmpm = t_tm = gbc = t_gbc = None
        gi = [0]

        def alloc_ff(FF):
            nonlocal hb, t_hb, wgu, t_wgu, wdb, t_wdb, hid, t_hid, sgt, t_sg, tmpm, t_tm, gbc, t_gbc
            hb = sb(FF, [128, 8, T], BF16, "hb")
            t_hb = [Tok() for _ in TILES]
            wgu = [sb(FF, [128, 2, 8, 512], BF16, "wgu") for _ in range(2)]
            t_wgu = [Tok(), Tok()]
            wdb = [sb(FF, [128, 4, 1024], BF16, "wdb") for _ in range(2)]
            t_wdb = [Tok(), Tok()]
            hid = sb(FF, [128, 4, T], BF16, "hid")
            t_hid = [Tok() for _ in TILES]
            sgt = sb(FF, [128, 512], F32, "sgt")
            t_sg = Tok()
            tmpm = sb(FF, [128, 512], F32, "tmpm")
            t_tm = Tok()
            gbc = sb(FF, [128, T], F32, "gbc")
            t_gbc = Tok()
        if True:

            def gated_ffn(Wg, Wu, Wd, dff, use_gate):
                nch = dff // 128
                for g0 in range(0, nch, 4):
                    gc = min(4, nch - g0)
                    bi = gi[0] % 2
                    gi[0] += 1
                    f0 = g0 * 128
                    wload(d_wgu[bi], wgu[bi][:, 0, :, 0:gc * 128], Wg[:, f0:f0 + gc * 128].rearrange("(c p) n -> p c n", p=128), [t_wgu[bi]])
                    wload(d_wgu[bi], wgu[bi][:, 1, :, 0:gc * 128], Wu[:, f0:f0 + gc * 128].rearrange("(c p) n -> p c n", p=128), [t_wgu[bi]])
                    wload(d_wd[bi], wdb[bi][:, 0:gc, :], Wd[f0:f0 + gc * 128, :].rearrange("(c p) n -> p c n", p=128), [t_wdb[bi]])
                    pendD = None

                    def emit_down(j, t0, n, bi, gc):
                        for oc in range(8):
                            pd_, td_ = ps()

                            def mmd(pd_=pd_, oc=oc):
                                ins = None
                                for fc in range(gc):
                                    ins = nc.tensor.matmul(pd_[:, 0:n], wdb[bi][:, fc, oc * 128:(oc + 1) * 128], hid[:, fc, t0:t0 + n],
                                                           start=(fc == 0), stop=(fc == gc - 1))
                                return ins
                            k.op(pe, mmd, r=[t_wdb[bi], t_hid[j]], w=[td_])
                            k.op(dve, lambda pd_=pd_, oc=oc: nc.vector.tensor_tensor(
                                out=xfm[:, oc, t0:t0 + n], in0=pd_[:, 0:n], in1=xfm[:, oc, t0:t0 + n], op=ALU.add),
                                r=[td_, t_x[j]], w=[t_x[j]])
                    for j, (t0, n) in enumerate(TILES):
                        for fc in range(gc):
                            pg_, tg_ = ps()
                            pu_, tu_ = ps()

                            def mmgu(pg_=pg_, pu_=pu_, fc=fc, t0=t0, n=n, bi=bi):
                                ins = None
                                for c in range(8):
                                    nc.tensor.matmul(pg_[:, 0:n], wgu[bi][:, 0, c, fc * 128:(fc + 1) * 128], hb[:, c, t0:t0 + n],
                                                     start=(c == 0), stop=(c == 7))
                                for c in range(8):
                                    ins = nc.tensor.matmul(pu_[:, 0:n], wgu[bi][:, 1, c, fc * 128:(fc + 1) * 128], hb[:, c, t0:t0 + n],
                                                           start=(c == 0), stop=(c == 7))
                                return ins
                            k.op(pe, mmgu, r=[t_wgu[bi], t_hb[j]], w=[tg_, tu_])
                            k.op(act, lambda pg_=pg_, n=n: nc.scalar.activation(out=sgt[:, 0:n], in_=pg_[:, 0:n], func=AF.Silu),
                                 r=[tg_], w=[t_sg])
                            if use_gate:
                                k.op(dve, lambda pu_=pu_, n=n: nc.vector.tensor_tensor(out=tmpm[:, 0:n], in0=pu_[:, 0:n], in1=sgt[:, 0:n],
                                                                                       op=ALU.mult), r=[tu_, t_sg], w=[t_tm])
                                k.op(dve, lambda fc=fc, t0=t0, n=n: nc.vector.tensor_tensor(
                                    out=hid[:, fc, t0:t0 + n], in0=tmpm[:, 0:n], in1=gbc[:, t0:t0 + n], op=ALU.mult),
                                    r=[t_tm, t_gbc], w=[t_hid[j]])
                            else:
                                k.op(dve, lambda pu_=pu_, fc=fc, t0=t0, n=n: nc.vector.tensor_tensor(
                                    out=hid[:, fc, t0:t0 + n], in0=pu_[:, 0:n], in1=sgt[:, 0:n], op=ALU.mult),
                                    r=[tu_, t_sg], w=[t_hid[j]])
                        if pendD is not None:
                            emit_down(*pendD)
                        pendD = (j, t0, n, bi, gc)
                    emit_down(*pendD)
                    pendD = None

            def norm_to_hb(pvcol):
                for j, (t0, n) in enumerate(TILES):
                    fm_rstd(j)
                    for c in range(8):
                        k.op(dve, lambda c=c, t0=t0, n=n: nc.vector.scalar_tensor_tensor(
                            out=hb[:, c, t0:t0 + n], in0=xfm[:, c, t0:t0 + n], scalar=pv(pvcol, c), in1=rstd2[:, 0:n],
                            op0=ALU.mult, op1=ALU.mult), r=[t_x[j], t_r2, t_c], w=[t_hb[j]])

            if stage >= 5:
                FF1 = ExitStack()
                with FF1:
                    alloc_ff(FF1)
                    norm_to_hb(PV_NF0)
                    gated_ffn(ffn_g, ffn_u, ffn_d, DFF, False)
                    k.barrier()

            if stage >= 6:
                PM = ExitStack()
                with PM:
                    tmpm = sb(PM, [128, 512], F32, "tmpm2")
                    t_tm = Tok()
                    pwb = sb(PM, [128, 4, 2, 256], BF16, "pwb")
                    t_pw = Tok()
                    wload(d_w0, pwb[:], pool_w.rearrange("g (c p) n -> p g c n", p=128), [t_pw])
                    L = 15 + 512
                    hf = sb(PM, [128, 8, L], F32, "hf")
                    t_hf = Tok()
                    sA = sb(PM, [128, 2, L], F32, "sA")
                    sB = sb(PM, [128, 2, L], F32, "sB")
                    t_s = Tok()
                    dif = sb(PM, [128, 8, 512], BF16, "dif")
                    t_dif = Tok()
                    hs = sb(PM, [128, 8, 4, 19], F32, "hs")
                    t_hs = Tok()
                    sAs = sb(PM, [128, 2, 4, 19], F32, "sAs")
                    sBs = sb(PM, [128, 2, 4, 19], F32, "sBs")
                    spl = sb(PM, [60, 1024], F32, "spl")
                    t_spl = Tok()
                    pout = sb(PM, [15, 1024], F32, "pout")
                    t_pout = Tok()
                    pouts = sb(PM, [4, 4, 1024], F32, "pouts")
                    t_pouts = Tok()
                    d_pm = k.dsem()
                    k.dma(sp, d_pm, spl[:], spool, w=[t_spl])
                    for c in range(8):
                        pst, tp = ps()
                        k.op(pe, lambda pst=pst, c=c: nc.tensor.transpose(pst[:, 0:60], spl[:, c * 128:(c + 1) * 128], ident[0:60, 0:60]),
                             r=[t_spl, t_c], w=[tp])
                        k.op(act, lambda pst=pst, c=c: nc.scalar.copy(out=hs[:, c, :, 0:15],
                                                                    in_=pst[:, 0:60].rearrange("p (b s) -> p b s", b=4)), r=[tp], w=[t_hs])
                    k.op(dve, lambda: nc.vector.memset(hf[:, :, 0:15], 0.0), w=[t_hf])
                    for j, (t0, n) in enumerate(TILES):
                        fm_rstd(j)
                        samp = (j == 4)
                        for c in range(8):
                            if not samp:
                                dst = hf[:, c, 15:15 + n]
                                xin_ = xfm[:, c, t0:t0 + n]
                                rin = rstd2[:, 0:n]
                            else:
                                dst = hs[:, c, :, 15:19]
                                xin_ = xfm[:, c, t0:t0 + 16].rearrange("p (b t) -> p b t", b=4)
                                rin = rstd2[:, 0:16].rearrange("p (b t) -> p b t", b=4)
                            k.op(dve, lambda c=c, dst=dst, xin_=xin_, rin=rin: nc.vector.scalar_tensor_tensor(
                                out=dst, in0=xin_, scalar=pv(PV_NM1, c), in1=rin, op0=ALU.mult, op1=ALU.mult),
                                r=[t_x[j], t_r2, t_c], w=[t_hs if samp else t_hf])
                        th = t_hs if samp else t_hf
                        if j == 3:
                            for half in range(2):
                                pst, tp = ps()

                                def trh(pst=pst, half=half):
                                    ins = None
                                    for cc in range(4):
                                        ins = nc.tensor.transpose(pst[0:15, cc * 128:(cc + 1) * 128], hf[:, half * 4 + cc, 512:527], ident)
                                    return ins
                                k.op(pe, trh, r=[t_hf, t_c], w=[tp])
                                k.op(act, lambda pst=pst, half=half: nc.scalar.copy(out=pout[:, half * 512:(half + 1) * 512], in_=pst[0:15, :]),
                                     r=[tp], w=[t_pout])
                            odma(o_poolp, pout[:], [t_pout])
                        if samp:
                            for b in range(4):
                                for half in range(2):
                                    pst, tp = ps()

                                    def trh2(pst=pst, half=half, b=b):
                                        ins = None
                                        for cc in range(4):
                                            ins = nc.tensor.transpose(pst[0:4, cc * 128:(cc + 1) * 128], hs[:, half * 4 + cc, b, 15:19], ident)
                                        return ins
                                    k.op(pe, trh2, r=[t_hs, t_c], w=[tp])
                                    k.op(act, lambda pst=pst, half=half, b=b: nc.scalar.copy(
                                        out=pouts[:, b, half * 512:(half + 1) * 512], in_=pst[0:4, :]), r=[tp], w=[t_pouts])
                            odma(o_pools[:, 11:15, :].rearrange("b t f -> t b f"), pouts[:], [t_pouts])
                            odma(o_pools[:, 0:11, :], spool.rearrange("(b s) f -> b s f", b=4)[:, 4:15, :], [])
                        for g in range(4):
                            w_ = 2 << g
                            if not samp:
                                cur = hf[:, 2 * g:2 * g + 2, :]
                                A, B = sA[:, :, :], sB[:, :, :]
                                LL = 15 + n
                            else:
                                cur = hs[:, 2 * g:2 * g + 2, :, :]
                                A, B = sAs[:, :, :, :], sBs[:, :, :, :]
                                LL = 19
                            sh = 1
                            src = cur
                            tsrc = th
                            flip = 0
                            while sh < w_:
                                dst = A if flip == 0 else B
                                if not samp:
                                    o_, a_, b_ = dst[:, :, sh:LL], src[:, :, sh:LL], src[:, :, 0:LL - sh]
                                else:
                                    o_, a_, b_ = dst[:, :, :, sh:LL], src[:, :, :, sh:LL], src[:, :, :, 0:LL - sh]
                                k.op(dve, lambda o_=o_, a_=a_, b_=b_: nc.vector.tensor_tensor(out=o_, in0=a_, in1=b_, op=ALU.add),
                                     r=[tsrc, t_s], w=[t_s])
                                src = dst
                                tsrc = t_s
                                flip ^= 1
                                sh *= 2
                            if not samp:
                                sw = src[:, :, 15:15 + n]
                                hh = hf[:, 2 * g:2 * g + 2, 15:15 + n]
                                dd = dif[:, 2 * g:2 * g + 2, 0:n]
                            else:
                                sw = src[:, :, :, 15:19]
                                hh = hs[:, 2 * g:2 * g + 2, :, 15:19]
                                dd = dif[:, 2 * g:2 * g + 2, 0:16].rearrange("p c (b t) -> p c b t", b=4)
                            k.op(dve, lambda sw=sw, hh=hh, dd=dd, w_=w_: nc.vector.scalar_tensor_tensor(
                                out=dd, in0=sw, scalar=1.0 / w_, in1=hh, op0=ALU.mult, op1=ALU.subtract), r=[t_s, th], w=[t_dif])
                            if j == 0:
                                for cc in range(2):
                                    k.op(dve, lambda cc=cc, g=g, src=src: nc.vector.tensor_tensor(
                                        out=tmpm[:, 0:16], in0=src[:, cc, 15:31], in1=cst[:, C_INV + g * 16:C_INV + g * 16 + 16],
                                        op=ALU.mult), r=[t_s, t_c], w=[t_tm])
                                    k.op(dve, lambda cc=cc, g=g: nc.vector.tensor_tensor(
                                        out=dif[:, 2 * g + cc, 0:16], in0=tmpm[:, 0:16], in1=hf[:, 2 * g + cc, 15:31], op=ALU.subtract),
                                        r=[t_tm, t_hf], w=[t_dif])
                        for g in range(4):
                            for oc in range(2):
                                pst, tp = ps()

                                def mmp(pst=pst, g=g, oc=oc, n=n):
                                    ins = None
                                    for c in range(2):
                                        ins = nc.tensor.matmul(pst[:, 0:n], pwb[:, g, c, oc * 128:(oc + 1) * 128], dif[:, 2 * g + c, 0:n],
                                                               start=(c == 0), stop=(c == 1))
                                    return ins
                                k.op(pe, mmp, r=[t_pw, t_dif], w=[tp])
                                ch = 2 * g + oc
                                k.op(dve, lambda pst=pst, ch=ch, t0=t0, n=n: nc.vector.scalar_tensor_tensor(
                                    out=xfm[:, ch, t0:t0 + n], in0=pst[:, 0:n], scalar=pv(PV_PS, ch), in1=xfm[:, ch, t0:t0 + n],
                                    op0=ALU.mult, op1=ALU.add), r=[tp, t_x[j], t_c], w=[t_x[j]])
                        if j < 3:
                            k.op(dve, lambda: nc.vector.tensor_copy(out=hf[:, :, 0:15], in_=hf[:, :, 512:527]), r=[t_dif, t_s], w=[t_hf])
                    k.barrier()

            if stage >= 7:
                MO = ExitStack()
                with MO:
                    alloc_ff(MO)
                    rw = sb(MO, [128, 8, 8], F32, "rw")
                    t_rw = Tok()
                    d_mo = k.dsem()
                    k.dma(sp, d_mo, rw[:], router_w.rearrange("(c p) e -> p c e", p=128), w=[t_rw])
                    hn = sb(MO, [128, 2, 512], F32, "hn")
                    t_hn = [Tok(), Tok()]
                    lgT = sb(MO, [8, 512], F32, "lgT")
                    t_lgT = Tok()
                    lg = sb(MO, [128, 17, 8], F32, "lg")
                    t_lg = Tok()
                    gT = sb(MO, [8, T], F32, "gT")
                    t_gT = Tok()
                    m1 = sb(MO, [128, 17], F32, "m1")
                    m2 = sb(MO, [128, 17], F32, "m2")
                    wk = sb(MO, [128, 17, 8], F32, "wk")
                    eq1 = sb(MO, [128, 17, 8], F32, "eq1")
                    eq2 = sb(MO, [128, 17, 8], F32, "eq2")
                    g1 = sb(MO, [128, 17], F32, "g1")
                    g2 = sb(MO, [128, 17], F32, "g2")
                    sel = sb(MO, [8, 8, 128], F32, "sel")
                    t_sel = Tok()
                    k.op(dve, lambda: nc.vector.memset(lg[:], 0.0), w=[t_lg])
                    for e in range(8):
                        k.op(dve, lambda e=e: nc.vector.tensor_copy(out=sel[:, e, :], in_=cst[0:8, C_ID + e:C_ID + e + 1].broadcast_to([8, 128])),
                             r=[t_c], w=[t_sel])
                    for j, (t0, n) in enumerate(TILES):
                        fm_rstd(j)
                        nsub = (n + 127) // 128
                        pst, tp = ps()
                        for c in range(8):
                            hb_ = c % 2
                            k.op(dve, lambda c=c, t0=t0, n=n, hb_=hb_: nc.vector.scalar_tensor_tensor(
                                out=hn[:, hb_, 0:n], in0=xfm[:, c, t0:t0 + n], scalar=pv(PV_NF1, c), in1=rstd2[:, 0:n],
                                op0=ALU.mult, op1=ALU.mult), r=[t_x[j], t_r2, t_c], w=[t_hn[hb_]])
                            k.op(act, lambda c=c, t0=t0, n=n, hb_=hb_: nc.scalar.copy(out=hb[:, c, t0:t0 + n], in_=hn[:, hb_, 0:n]),
                                 r=[t_hn[hb_]], w=[t_hb[j]])

                            k.op(pe, lambda pst=pst, n=n, c=c, hb_=hb_: nc.tensor.matmul(
                                pst[0:8, 0:n], rw[:, c, :], hn[:, hb_, 0:n], start=(c == 0), stop=(c == 7)),
                                r=[t_hn[hb_], t_rw], w=[tp])
                        k.op(act, lambda pst=pst, n=n: nc.scalar.copy(out=lgT[:, 0:n], in_=pst[0:8, 0:n]), r=[tp], w=[t_lgT])
                        pst2, tp2 = ps()

                        def trl(pst2=pst2, nsub=nsub, n=n):
                            ins = None
                            for s_ in range(nsub):
                                rows = min(128, n - s_ * 128)
                                ins = nc.tensor.transpose(pst2[0:rows, s_ * 8:s_ * 8 + 8], lgT[0:8, s_ * 128:s_ * 128 + rows], ident[0:8, 0:8])
                            return ins
                        k.op(pe, trl, r=[t_lgT, t_c], w=[tp2])
                        rows_all = min(128, n)
                        k.op(act, lambda pst2=pst2, j=j, nsub=nsub, rows_all=rows_all: nc.scalar.copy(
                            out=lg[0:rows_all, j * 4:j * 4 + nsub, :], in_=pst2[0:rows_all, 0:nsub * 8].rearrange("p (s e) -> p s e", e=8)),
                            r=[tp2], w=[t_lg])
                    X = mybir.AxisListType.X
                    k.op(dve, lambda: nc.vector.tensor_reduce(out=m1[:], in_=lg[:], axis=X, op=ALU.max), r=[t_lg], w=[t_lg])
                    k.op(dve, lambda: nc.vector.tensor_tensor(out=eq1[:], in0=lg[:], in1=m1[:].unsqueeze(2).broadcast_to([128, 17, 8]),
                                                              op=ALU.is_equal), r=[t_lg], w=[t_lg])
                    k.op(dve, lambda: nc.vector.scalar_tensor_tensor(out=wk[:], in0=eq1[:], scalar=-1e30, in1=lg[:], op0=ALU.mult,
                                                                     op1=ALU.add), r=[t_lg], w=[t_lg])
                    k.op(dve, lambda: nc.vector.tensor_reduce(out=m2[:], in_=wk[:], axis=X, op=ALU.max), r=[t_lg], w=[t_lg])
                    k.op(dve, lambda: nc.vector.tensor_tensor(out=eq2[:], in0=wk[:], in1=m2[:].unsqueeze(2).broadcast_to([128, 17, 8]),
                                                              op=ALU.is_equal), r=[t_lg], w=[t_lg])
                    k.op(dve, lambda: nc.vector.tensor_tensor(out=g1[:], in0=m1[:], in1=m2[:], op=ALU.subtract), r=[t_lg], w=[t_lg])
                    k.op(act, lambda: nc.scalar.activation(out=g1[:], in_=g1[:], func=AF.Sigmoid), r=[t_lg], w=[t_lg])
                    k.op(dve, lambda: nc.vector.tensor_scalar(out=g2[:], in0=g1[:], scalar1=-1.0, scalar2=1.0, op0=ALU.mult, op1=ALU.add),
                         r=[t_lg], w=[t_lg])
                    k.op(dve, lambda: nc.vector.tensor_tensor(out=eq1[:], in0=eq1[:], in1=g1[:].unsqueeze(2).broadcast_to([128, 17, 8]),
                                                              op=ALU.mult), r=[t_lg], w=[t_lg])
                    k.op(dve, lambda: nc.vector.tensor_tensor(out=eq2[:], in0=eq2[:], in1=g2[:].unsqueeze(2).broadcast_to([128, 17, 8]),
                                                              op=ALU.mult), r=[t_lg], w=[t_lg])
                    k.op(dve, lambda: nc.vector.tensor_tensor(out=wk[:], in0=eq1[:], in1=eq2[:], op=ALU.add), r=[t_lg], w=[t_lg])
                    for st in range(17):
                        rows = 128 if st < 16 else 16
                        pst, tp = ps()
                        k.op(pe, lambda pst=pst, st=st, rows=rows: nc.tensor.transpose(pst[0:8, 0:rows], wk[0:rows, st, :], ident[0:rows, 0:rows]),
                             r=[t_lg, t_c], w=[tp])
                        k.op(act, lambda pst=pst, st=st, rows=rows: nc.scalar.copy(out=gT[:, st * 128:st * 128 + rows], in_=pst[0:8, 0:rows]),
                             r=[tp], w=[t_gT])
                    for e in range(NE if not SMALLW else 1):
                        for j, (t0, n) in enumerate(TILES):
                            pst, tp = ps()
                            k.op(pe, lambda pst=pst, e=e, t0=t0, n=n: nc.tensor.matmul(pst[:, 0:n], sel[:, e, :], gT[:, t0:t0 + n],
                                                                                     start=True, stop=True), r=[t_sel, t_gT], w=[tp])
                            k.op(act, lambda pst=pst, t0=t0, n=n: nc.scalar.copy(out=gbc[:, t0:t0 + n], in_=pst[:, 0:n]), r=[tp], w=[t_gbc])
                        gated_ffn(moe_g[e], moe_u[e], moe_d[e], DFE, True)
                    k.barrier()

        if stage >= 8:
            FN = ExitStack()
            with FN:
                yt = sb(FN, [128, 8, 512], F32, "yt")
                t_yt = Tok()
                yo = [sb(FN, [128, 1024], F32, "yo") for _ in range(2)]
                t_yo = [Tok(), Tok()]
                for j, (t0, n) in enumerate(TILES):
                    fm_rstd(j)
                    for c in range(8):
                        k.op(dve, lambda c=c, t0=t0, n=n: nc.vector.scalar_tensor_tensor(
                            out=yt[:, c, 0:n], in0=xfm[:, c, t0:t0 + n], scalar=pv(PV_NFIN, c), in1=rstd2[:, 0:n],
                            op0=ALU.mult, op1=ALU.mult), r=[t_x[j], t_r2, t_c], w=[t_yt])
                    nsub = (n + 127) // 128
                    for s_ in range(nsub):
                        rows = min(128, n - s_ * 128)
                        bi = (j * 4 + s_) % 2
                        for half in range(2):
                            pst, tp = ps()

                            def try_(pst=pst, half=half, s_=s_, rows=rows):
                                ins = None
                                for cc in range(4):
                                    ins = nc.tensor.transpose(pst[0:rows, cc * 128:(cc + 1) * 128], yt[:, half * 4 + cc, s_ * 128:s_ * 128 + rows], ident)
                                return ins
                            k.op(pe, try_, r=[t_yt, t_c], w=[tp])
                            k.op(act, lambda pst=pst, half=half, rows=rows, bi=bi: nc.scalar.copy(
                                out=yo[bi][0:rows, half * 512:(half + 1) * 512], in_=pst[0:rows, :]), r=[tp], w=[t_yo[bi]])
                        dst = o_yp[t0 + s_ * 128:t0 + s_ * 128 + rows, :] if j < 4 else o_ys[:, :]
                        odma(dst, yo[bi][0:rows, :], [t_yo[bi]])
        _finish(nc, k, out_ds)
    return nc


def _finish(nc, k, out_ds):
    print("K ops:", k.n, {e.name: e.cnt for e in [k.pe, k.act, k.dve, k.pool, k.sp]})
    for d in out_ds:
        if d.cnt:
            k.sp.h.wait_ge(d.sem, d.cnt)


def _host_consts():
    cst = np.zeros((128, C_W), np.float32)
    cst[:, C_ID:C_ID + 128] = np.eye(128, dtype=np.float32)
    kk = np.arange(128)[:, None]
    qq = np.arange(128)[None, :]
    cst[:, C_TRI:C_TRI + 128] = (kk <= qq).astype(np.float32)
    for kx in range(4):
        for h in range(8):
            for t in range(4):
                cst[kx, C_MN + h * 4 + t] = 1.0 if kx <= t else 0.0
    for g, w in enumerate((2, 4, 8, 16)):
        for p in range(16):
            cst[:, C_INV + g * 16 + p] = 1.0 / min(p + 1, w)
    cst[:, C_IOTA] = np.arange(128, dtype=np.float32)
    half = 16
    inv_freq = np.power(np.float32(10000.0), -np.arange(half, dtype=np.float32) / np.float32(half)).astype(np.float32)
    pos = np.concatenate([np.arange(NP_, dtype=np.float32),
                          np.tile(16384 + np.arange(4, dtype=np.float32), 4)]).astype(np.float32)
    ang = (pos[None, :] * inv_freq[:, None]).astype(np.float32)
    cos = np.cos(ang.astype(np.float64)).astype(np.float32)
    sin = np.sin(ang.astype(np.float64)).astype(np.float32)
    ropec = np.concatenate([cos, cos], 0)
    ropes = np.concatenate([-sin, sin], 0)
    return cst, np.ascontiguousarray(ropec), np.ascontiguousarray(ropes)


def _fm(v):
    return np.ascontiguousarray(np.asarray(v, np.float32).reshape(-1, 128).T)


def kernel(stage=99, limit=10 ** 9, **inp):
    f = lambda a: np.ascontiguousarray(np.asarray(a, dtype=np.float32))
    cst, ropec, ropes = _host_consts()
    pvec = np.zeros((128, PV_W), np.float32)
    pvec[:, PV_NM0:PV_NM0 + 8] = _fm(inp["norm_mix"][0])
    pvec[:, PV_NF0:PV_NF0 + 8] = _fm(inp["norm_ffn"][0])
    pvec[:, PV_NM1:PV_NM1 + 8] = _fm(inp["norm_mix"][1])
    pvec[:, PV_NF1:PV_NF1 + 8] = _fm(inp["norm_ffn"][1])
    pvec[:, PV_NFIN:PV_NFIN + 8] = _fm(inp["norm_final"])
    pvec[:, PV_CB:PV_CB + 4] = _fm(inp["conv_b"][0])
    pvec[:, PV_LG:PV_LG + 4] = _fm(inp["conv_ln_g"][0])
    pvec[:, PV_LB:PV_LB + 4] = _fm(inp["conv_ln_b"][0])
    pvec[:, PV_QN:PV_QN + 4] = _fm(inp["q_norm"][0])
    pvec[:, PV_KVN:PV_KVN + 2] = _fm(inp["kv_norm"][0])
    pvec[:, PV_PS:PV_PS + 8] = _fm(inp["pool_scale"][0])
    cw = np.asarray(inp["conv_w"][0], np.float32)
    pvec[:, PV_CW:PV_CW + 124] = cw.reshape(31, 4, 128).transpose(2, 0, 1).reshape(128, 124)
    shared = {
        "ccat": np.concatenate([f(inp["cache_ckv"][0][:CACHE_PAGES]).reshape(CACHE_PAGES * 128, 256),
                                f(inp["cache_kpe"][0][:CACHE_PAGES]).reshape(CACHE_PAGES * 128, 32)], axis=1),
        "pvec": pvec, "cst": cst, "ropec": ropec, "ropes": ropes,
        "w_in": f(inp["w_in"][0]), "w_uq": f(inp["w_uq"][0]), "w_ukv": f(inp["w_ukv"][0]), "w_out": f(inp["w_out"][0]),
        "ffn_g": f(inp["ffn_w_gate"][0]), "ffn_u": f(inp["ffn_w_up"][0]), "ffn_d": f(inp["ffn_w_down"][0]),
        "pool_w": f(inp["pool_w"][0]), "router_w": f(inp["router_w"][0]),
        "moe_g": f(inp["moe_w_gate"][0][:(1 if SMALLW else NE)]), "moe_u": f(inp["moe_w_up"][0][:(1 if SMALLW else NE)]),
        "moe_d": f(inp["moe_w_down"][0][:(1 if SMALLW else NE)]),
    }
    xpr = f(inp["x_prompt"])
    xsa = f(inp["x_sample"])
    pt = np.ascontiguousarray(np.asarray(inp["page_table"], dtype=np.int32))
    sc = f(inp["state_conv"][0])
    spl = f(inp["state_pool"][0])
    in_maps = []
    for c in range(8):
        m = dict(shared)
        m["xp"] = xpr[c]
        m["xs"] = np.ascontiguousarray(xsa[4 * c:4 * c + 4].reshape(16, 1024))
        m["ptab"] = np.ascontiguousarray(np.broadcast_to(pt[4 * c:4 * c + 4].reshape(1, 512), (128, 512)))
        m["sconv"] = np.ascontiguousarray(sc[4 * c:4 * c + 4].reshape(120, 512))
        m["spool"] = np.ascontiguousarray(spl[4 * c:4 * c + 4].reshape(60, 1024))
        in_maps.append(m)
    nc = build(stage, limit)
    res = run_bass_kernel_spmd(nc, in_maps, core_ids=list(range(8)))
    R = res.results
    cat = lambda key: np.stack([np.asarray(R[c][key], np.float32) for c in range(8)], 0)
    y_p = cat("o_yp")
    y_s = cat("o_ys").reshape(32, 4, 1024)
    ckv_p = cat("o_ckvp")[None]
    kpe_p = cat("o_kpep")[None]
    conv_p = cat("o_convp")[None]
    pool_p = cat("o_poolp")[None]
    ckv_s = cat("o_ckvs").reshape(1, 32, 4, 256)
    kpe_s = cat("o_kpes").reshape(1, 32, 4, 32)
    conv_s = cat("o_convs").reshape(1, 32, 30, 512)
    pool_s = cat("o_pools").reshape(1, 32, 15, 1024)
    return (y_p, y_s, ckv_p, kpe_p, conv_p, pool_p, ckv_s, kpe_s, conv_s, pool_s)
```

```python
from contextlib import ExitStack
import numpy as np
import concourse.bass as bass
import concourse.mybir as mybir
from concourse.bass_utils import run_bass_kernel_spmd

F32 = mybir.dt.float32
BF16 = mybir.dt.bfloat16
I32 = mybir.dt.int32
AF = mybir.ActivationFunctionType
ALU = mybir.AluOpType

NP_ = 2048
NS_ = 16
T = NP_ + NS_
TILES = [(0, 512), (512, 512), (1024, 512), (1536, 512), (2048, 16)]
EPS = 1e-6
SCALE = 96.0 ** -0.5
DFF = 2816
DFE = 3584
NE = 8
CACHE_PAGES = 5120
SMALLW = False
PV_NM0, PV_NF0, PV_NM1, PV_NF1, PV_NFIN = 0, 8, 16, 24, 32
PV_CB, PV_LG, PV_LB, PV_QN, PV_KVN, PV_PS, PV_CW = 40, 44, 48, 52, 56, 58, 66
PV_W = 66 + 124
C_ID, C_TRI, C_MN, C_INV, C_IOTA = 0, 128, 256, 288, 352
C_W = 353


class Eng:
    def __init__(self, name, h, sem):
        self.name, self.h, self.sem, self.cnt, self.seen = name, h, sem, 0, {}


class Tok:
    __slots__ = ("w", "r")

    def __init__(self):
        self.w = None
        self.r = {}


class K:
    def __init__(self, nc, es):
        self.nc = nc
        self.es = es
        mk = lambda n, h: Eng(n, h, es.enter_context(nc.semaphore("s_" + n)))
        self.pe = mk("pe", nc.tensor)
        self.act = mk("act", nc.scalar)
        self.dve = mk("dve", nc.vector)
        self.pool = mk("pool", nc.gpsimd)
        self.sp = mk("sp", nc.sync)
        self.dsems = []
        self.nds = 0
        self.n = 0
        self.limit = 10 ** 9
        self.log = []

    def dsem(self):
        self.nds += 1
        d = Eng("d%d" % self.nds, None, self.es.enter_context(self.nc.semaphore("s_d%d" % self.nds)))
        self.dsems.append(d)
        return d

    def _waits(self, eng, r, w):
        need = {}

        def add(p):
            if p is None:
                return
            e, c = p
            if e is eng and eng is self.pe:
                return
            if need.get(e, 0) < c:
                need[e] = c

        for t in r:
            add(t.w)
        for t in w:
            add(t.w)
            for e, c in t.r.items():
                add((e, c))
        for e, c in need.items():
            if eng.seen.get(e, 0) >= c:
                continue
            eng.h.wait_ge(e.sem, c)
            eng.seen[e] = c

    def op(self, eng, fn, r=(), w=()):
        self.n += 1
        if self.n > self.limit:
            return None
        self._waits(eng, r, w)
        ins = fn()
        eng.cnt += 1
        ins.then_inc(eng.sem, 1)
        for t in r:
            t.r[eng] = eng.cnt
        for t in w:
            t.w = (eng, eng.cnt)
            t.r = {}
        return ins

    def dma(self, q, ds, out, in_, r=(), w=(), **kw):
        self.n += 1
        if self.n > self.limit:
            return None
        self._waits(q, r, w)
        ins = q.h.dma_start(out=out, in_=in_, **kw)
        ds.cnt += 16
        ins.then_inc(ds.sem, 16)
        for t in r:
            t.r[ds] = ds.cnt
        for t in w:
            t.w = (ds, ds.cnt)
            t.r = {}

    def idma(self, ds, out, in_, idx_ap, r=(), w=()):
        q = self.pool
        self.n += 1
        if self.n > self.limit:
            return None
        self._waits(q, r, w)
        ins = q.h.indirect_dma_start(out=out, out_offset=None, in_=in_,
                                     in_offset=bass.IndirectOffsetOnAxis(ap=idx_ap, axis=0))
        ds.cnt += 16
        ins.then_inc(ds.sem, 16)
        for t in r:
            t.r[ds] = ds.cnt
        for t in w:
            t.w = (ds, ds.cnt)
            t.r = {}

    def barrier(self):
        engs = [self.pe, self.act, self.dve, self.pool, self.sp]
        for e in engs:
            for o in engs + self.dsems:
                if o is e or o.cnt == 0:
                    continue
                if e.seen.get(o, 0) < o.cnt:
                    e.h.wait_ge(o.sem, o.cnt)
                    e.seen[o] = o.cnt


def build(stage=99, limit=10 ** 9):
    nc = bass.Bass("TRN2", target_bir_lowering=False)

    def din(name, shape, dt=F32):
        return nc.dram_tensor(name, list(shape), dt, kind="ExternalInput").ap()

    def dout(name, shape):
        return nc.dram_tensor(name, list(shape), F32, kind="ExternalOutput").ap()

    xp = din("xp", [NP_, 1024])
    xs = din("xs", [NS_, 1024])
    ccat = din("ccat", [CACHE_PAGES * 128, 288])
    ptab = din("ptab", [128, 512], I32)
    sconv = din("sconv", [120, 512])
    spool = din("spool", [60, 1024])
    pvec_d = din("pvec", [128, PV_W])
    cst_d = din("cst", [128, C_W])
    ropec_d = din("ropec", [32, T])
    ropes_d = din("ropes", [32, T])
    w_in = din("w_in", [1024, 1824])
    w_uq = din("w_uq", [512, 768])
    w_ukv = din("w_ukv", [256, 1024])
    w_out = din("w_out", [1024, 1024])
    ffn_g = din("ffn_g", [1024, DFF])
    ffn_u = din("ffn_u", [1024, DFF])
    ffn_d = din("ffn_d", [DFF, 1024])
    pool_w = din("pool_w", [4, 256, 256])
    router_w = din("router_w", [1024, 8])
    ne_ = 1 if SMALLW else NE
    moe_g = din("moe_g", [ne_, 1024, DFE])
    moe_u = din("moe_u", [ne_, 1024, DFE])
    moe_d = din("moe_d", [ne_, DFE, 1024])

    o_yp = dout("o_yp", [NP_, 1024])
    o_ys = dout("o_ys", [NS_, 1024])
    o_ckvp = dout("o_ckvp", [NP_, 256])
    o_kpep = dout("o_kpep", [NP_, 32])
    o_convp = dout("o_convp", [30, 512])
    o_poolp = dout("o_poolp", [15, 1024])
    o_ckvs = dout("o_ckvs", [NS_, 256])
    o_kpes = dout("o_kpes", [NS_, 32])
    o_convs = dout("o_convs", [4, 30, 512])
    o_pools = dout("o_pools", [4, 15, 1024])
    xscr = nc.dram_tensor("xscr", [128, 8, T], F32, kind="Internal").ap()

    es = ExitStack()
    with es:
        k = K(nc, es)
        k.limit = limit
        pe, act, dve, pool, sp = k.pe, k.act, k.dve, k.pool, k.sp
        ctr = [0]

        def sb(stack, shape, dt=F32, name=None):
            ctr[0] += 1
            return stack.enter_context(nc.sbuf_tensor("%s_%d" % (name or "t", ctr[0]), list(shape), dt))

        banks = []
        for i in range(8):
            banks.append((es.enter_context(nc.psum_tensor("ps%d" % i, [128, 512], F32)), Tok()))
        pctr = [0]

        nrot = [6]

        def ps():
            b = banks[pctr[0] % nrot[0]]
            pctr[0] += 1
            return b
        actr = [0]

        def psacc():
            b = banks[5 + actr[0] % 2]
            actr[0] += 1
            return b

        out_ds = []
        octr = [0]

        odm = {}

        def odma(out, in_, r):
            key = id(r[0]) if r else 0
            if key not in odm:
                odm[key] = k.dsem()
                out_ds.append(odm[key])
            k.dma(sp, odm[key], out, in_, r=r)

        cst = sb(es, [128, C_W], name="cst")
        pvec = sb(es, [128, PV_W], name="pvec")
        t_c = Tok()
        d_c = k.dsem()
        k.dma(sp, d_c, cst[:], cst_d, w=[t_c])
        k.dma(sp, d_c, pvec[:], pvec_d, w=[t_c])
        ident = cst[:, C_ID:C_ID + 128]
        o1024 = sb(es, [128, 128], BF16, "o1024")
        o512 = sb(es, [128, 128], BF16, "o512")
        o256 = sb(es, [128, 128], BF16, "o256")
        tri = sb(es, [128, 128], BF16, "tri")
        mnew = sb(es, [4, 32], BF16, "mnew")
        k.op(dve, lambda: nc.vector.memset(o1024[:], 1.0 / 1024), w=[t_c])
        k.op(dve, lambda: nc.vector.memset(o512[:], 1.0 / 512), w=[t_c])
        k.op(dve, lambda: nc.vector.memset(o256[:], 1.0 / 256), w=[t_c])
        k.op(dve, lambda: nc.vector.tensor_copy(out=tri[:], in_=cst[:, C_TRI:C_TRI + 128]), r=[t_c], w=[t_c])
        k.op(dve, lambda: nc.vector.tensor_copy(out=mnew[:], in_=cst[0:4, C_MN:C_MN + 32]), r=[t_c], w=[t_c])

        def pv(col, c=0):
            return pvec[:, col + c:col + c + 1]

        def wload(ds, dst, src, w):
            k.dma(pool, ds, dst, src, w=w)
            if ds.cnt >= 48 and pool.seen.get(ds, 0) < ds.cnt - 32:
                pool.h.wait_ge(ds.sem, ds.cnt - 32)
                pool.seen[ds] = ds.cnt - 32

        epsb = sb(es, [128, 1], F32, "epsb")
        k.op(dve, lambda: nc.vector.memset(epsb[:], EPS), w=[t_c])

        def rsqrt_to(out_ap, in_ap, scale, r, t_out):
            np_ = out_ap.shape[0]
            k.op(act, lambda: nc.scalar.activation(out=out_ap, in_=in_ap, func=AF.Sqrt, scale=scale,
                                                   bias=epsb[0:np_, 0:1]), r=list(r) + [t_c], w=[t_out])
            k.op(dve, lambda: nc.vector.reciprocal(out=out_ap, in_=out_ap), r=[t_out], w=[t_out])

        def rstd_from(srcs, n, ones_t, sqbuf, t_sq, rstd_ap, t_rstd, r):
            for i, s in enumerate(srcs):
                k.op(act, (lambda s=s, i=i: nc.scalar.activation(out=sqbuf[:, i, 0:n], in_=s, func=AF.Square)),
                     r=r, w=[t_sq])
            pst, tp = ps()

            def mm():
                ins = None
                for i in range(len(srcs)):
                    ins = nc.tensor.matmul(pst[:, 0:n], ones_t[:], sqbuf[:, i, 0:n], start=(i == 0),
                                           stop=(i == len(srcs) - 1))
                return ins
            k.op(pe, mm, r=[t_sq, t_c], w=[tp])
            rsqrt_to(rstd_ap[:, 0:n], pst[:, 0:n], 1.0, [tp], t_rstd)

        L0 = ExitStack()
        with L0:
            ustore = sb(L0, [128, 4, 30 + NP_], BF16, "ustore")
            t_ust = [Tok() for _ in range(4)]
            mixc_s = sb(L0, [128, 4, NS_], BF16, "mixc_s")
            t_mixc = [Tok() for _ in TILES]
            k.op(dve, lambda: nc.vector.memset(ustore[:], 0.0), w=t_ust)
            ckvT = sb(L0, [128, 2, T], BF16, "ckvT")
            t_ckvT = [Tok() for _ in TILES]
            kper = sb(L0, [128, T], BF16, "kper")
            t_kper = [Tok() for _ in TILES]
            qall = sb(L0, [96, 8, T], BF16, "qall")
            t_qall = [Tok() for _ in TILES]
            kpes0 = sb(L0, [32, NS_], BF16, "kpes0")
            t_kpes0 = Tok()
            newkv = sb(L0, [4, 4, 257], BF16, "newkv")
            t_newkv = Tok()
            wukv = sb(L0, [128, 2, 1024], BF16, "wukv")
            t_wukv = Tok()
            d_w0 = k.dsem()
            wload(d_w0, wukv[:], w_ukv.rearrange("(c p) n -> p c n", p=128), [t_wukv])

            PA = ExitStack()
            with PA:
                winb = sb(PA, [128, 8, 1824], BF16, "winb")
                wkpe = sb(PA, [128, 8, 2, 96], BF16, "wkpe")
                wuqb = sb(PA, [128, 4, 768], BF16, "wuqb")
                wuqs = sb(PA, [128, 4, 8, 96], BF16, "wuqs")
                t_w = Tok()
                k.op(pool, lambda: nc.gpsimd.memset(wkpe[:], 0.0), w=[t_w])
                k.op(pool, lambda: nc.gpsimd.memset(wuqs[:], 0.0), w=[t_w])
                win_v = w_in.rearrange("(c p) n -> p c n", p=128)
                for c in range(8):
                    wload(d_w0, winb[:, c, :], w_in[c * 128:(c + 1) * 128, :], [t_w])
                wload(d_w0, wkpe[:, :, 0, 64:96], win_v[:, :, 1792:1824], [t_w])
                wload(d_w0, wkpe[:, :, 1, 64:80], win_v[:, :, 1808:1824], [t_w])
                wload(d_w0, wkpe[:, :, 1, 80:96], win_v[:, :, 1792:1808], [t_w])
                wuq_v = w_uq.rearrange("(c p) n -> p c n", p=128)
                t_w2 = Tok()
                t_w2.w = t_w.w
                d_w1 = k.dsem()
                wload(d_w1, wuqb[:], wuq_v, [t_w2])
                wuq_v4 = w_uq.rearrange("(c p) (h d) -> p c h d", p=128, d=96)
                for c in range(4):
                    wload(d_w1, wuqs[:, c, :, 64:80], wuq_v4[:, c, :, 80:96], [t_w2])
                    wload(d_w1, wuqs[:, c, :, 80:96], wuq_v4[:, c, :, 64:80], [t_w2])

                xin = [sb(PA, [128, 1024], F32, "xin") for _ in range(2)]
                t_xin = [Tok(), Tok()]
                d_xin = [k.dsem(), k.dsem()]
                junk = sb(PA, [128, 1024], BF16, "junk")
                t_junk = Tok()
                ssq = sb(PA, [128, 4], F32, "ssq")
                h0 = sb(PA, [128, 8, 512], BF16, "h0")
                t_h0 = Tok()
                sqb = sb(PA, [128, 8, 512], BF16, "sqb")
                t_sqb = Tok()
                sig = sb(PA, [128, 512], F32, "sig")
                t_sig = Tok()
                uroll = sb(PA, [128, 4, 542], F32, "uroll")
                t_ur = Tok()
                us = sb(PA, [128, 4, 4, 34], F32, "us")
                t_us = Tok()
                acc = sb(PA, [128, 4, 512], F32, "acc")
                t_accs = [Tok() for _ in range(4)]
                mean_sb = sb(PA, [128, 512], F32, "mean")
                var_sb = sb(PA, [128, 512], F32, "var")
                rstd_c = sb(PA, [128, 512], F32, "rstdc")
                t_ln = Tok()
                tt1 = sb(PA, [128, 512], F32, "tt1")
                tt2 = sb(PA, [128, 512], F32, "tt2")
                t_tt = Tok()
                qdn = sb(PA, [128, 4, 512], F32, "qdn")
                t_qdn = Tok()
                rstd_q = sb(PA, [128, 512], F32, "rstdq")
                t_rq = Tok()
                qn = sb(PA, [128, 4, 512], BF16, "qn")
                t_qn = Tok()
                kvf = sb(PA, [128, 2, 512], F32, "kvf")
                t_kvf = Tok()
                rstd_k = sb(PA, [128, 512], F32, "rstdk")
                t_rk = Tok()
                rc_t = sb(PA, [128, 512], F32, "ropec")
                rs_t = sb(PA, [128, 512], F32, "ropes")
                t_rope = Tok()
                d_rope = k.dsem()
                kpef = sb(PA, [128, 512], F32, "kpef")
                t_kpef = Tok()
                ckvo = sb(PA, [128, 4, 256], F32, "ckvo")
                t_ckvo = Tok()
                kpeo = sb(PA, [128, 4, 32], F32, "kpeo")
                t_kpeo = Tok()
                cvo = sig[0:30, :]
                t_cvo = t_sig
                cvs = qdn[0:4, :, :]
                t_cvs = t_qdn
                scv = kpef[0:120, :]
                t_scv = t_kpef
                d_misc = k.dsem()

                k.op(dve, lambda: nc.vector.memset(uroll[:, :, 0:30], 0.0), w=[t_ur])
                k.dma(sp, d_misc, scv[:], sconv, w=[t_scv])
                for c in range(4):
                    pst, tp = ps()
                    k.op(pe, lambda c=c, pst=pst: nc.tensor.transpose(pst[:, 0:120], scv[:, c * 128:(c + 1) * 128],
                                                                      ident[0:120, 0:120]), r=[t_scv, t_c], w=[tp])
                    k.op(act, lambda c=c, pst=pst: nc.scalar.copy(
                        out=us[:, c, :, 0:30], in_=pst[:, 0:120].rearrange("p (b s) -> p b s", b=4)), r=[tp], w=[t_us])

                for j, (t0, n) in enumerate(TILES):
                    nsub = (n + 127) // 128
                    for s in range(nsub):
                        rows = min(128, n - s * 128)
                        bi = (j * 4 + s) % 2
                        src = xp[t0 + s * 128:t0 + s * 128 + rows, :] if j < 4 else xs[:, :]
                        k.dma(sp, d_xin[bi], xin[bi][0:rows, :], src, w=[t_xin[bi]])
                        k.op(act, lambda bi=bi, rows=rows, s=s: nc.scalar.activation(
                            out=junk[0:rows, :], in_=xin[bi][0:rows, :], func=AF.Square,
                            accum_out=ssq[0:rows, s:s + 1]), r=[t_xin[bi]], w=[t_junk])
                        rsqrt_to(ssq[0:rows, s:s + 1], ssq[0:rows, s:s + 1], 1.0 / 1024, [t_junk], t_junk)
                        k.op(dve, lambda bi=bi, rows=rows, s=s: nc.vector.tensor_scalar(
                            out=xin[bi][0:rows, :], in0=xin[bi][0:rows, :], scalar1=ssq[0:rows, s:s + 1], scalar2=1.0,
                            op0=ALU.mult, op1=ALU.mult), r=[t_junk, t_xin[bi]], w=[t_xin[bi]])
                        for half in range(2):
                            pst, tp = ps()

                            def tr(bi=bi, rows=rows, half=half, pst=pst):
                                ins = None
                                for cc in range(4):
                                    c = half * 4 + cc
                                    ins = nc.tensor.transpose(pst[:, cc * 128:cc * 128 + rows],
                                                              xin[bi][0:rows, c * 128:(c + 1) * 128],
                                                              ident[0:rows, 0:rows])
                                return ins
                            k.op(pe, tr, r=[t_xin[bi], t_c], w=[tp])
                            for cc in range(4):
                                c = half * 4 + cc
                                k.op(act, lambda c=c, cc=cc, s=s, rows=rows, pst=pst: nc.scalar.activation(
                                    out=h0[:, c, s * 128:s * 128 + rows], in_=pst[:, cc * 128:cc * 128 + rows],
                                    func=AF.Copy, scale=pv(PV_NM0, c)), r=[tp, t_c], w=[t_h0])

                    def proj(col0, m, wt=None, wsel=None):
                        pst, tp = ps()

                        def mm():
                            ins = None
                            for c in range(8):
                                l = winb[:, c, col0:col0 + m] if wt is None else wt[:, c, wsel, 0:m]
                                ins = nc.tensor.matmul(pst[0:m, 0:n], l, h0[:, c, 0:n], start=(c == 0), stop=(c == 7))
                            return ins
                        k.op(pe, mm, r=[t_h0, t_w], w=[tp])
                        return pst, tp

                    for c in range(4):
                        pa, tpa = proj(c * 128, 128)
                        pg, tpg = proj(512 + c * 128, 128)
                        k.op(act, lambda pg=pg: nc.scalar.activation(out=sig[:, 0:n], in_=pg[:, 0:n], func=AF.Sigmoid),
                             r=[tpg], w=[t_sig])
                        if j < 4:
                            k.op(dve, lambda c=c, pa=pa: nc.vector.tensor_tensor(
                                out=ustore[:, c, 30 + t0:30 + t0 + n], in0=pa[:, 0:n], in1=sig[:, 0:n], op=ALU.mult),
                                r=[tpa, t_sig], w=[t_ust[j]])
                            if j == 3:
                                k.op(dve, lambda c=c, pa=pa: nc.vector.tensor_tensor(
                                    out=uroll[:, c, 512:542], in0=pa[:, 482:512], in1=sig[:, 482:512], op=ALU.mult),
                                    r=[tpa, t_sig], w=[t_ur])
                        else:
                            k.op(dve, lambda c=c, pa=pa: nc.vector.tensor_tensor(
                                out=us[:, c, :, 30:34], in0=pa[:, 0:16].rearrange("p (b t) -> p b t", b=4),
                                in1=sig[:, 0:16].rearrange("p (b t) -> p b t", b=4), op=ALU.mult),
                                r=[tpa, t_sig], w=[t_us])
                    for c in range(4 if j == 4 else 0):
                        def ext(kk, c=c):
                            return uroll[:, c, kk:kk + n] if j < 4 else us[:, c, :, kk:kk + 4]

                        def accv(c=c):
                            return acc[:, c, 0:n] if j < 4 else acc[:, c, 0:16].rearrange("p (b t) -> p b t", b=4)
                        tsrc = t_ur if j < 4 else t_us
                        ce = dve
                        ceh = nc.vector
                        k.op(ce, lambda c=c, ext=ext, accv=accv, ceh=ceh: ceh.tensor_scalar(
                            out=accv(), in0=ext(0), scalar1=pv(PV_CW, c), scalar2=pv(PV_CB, c),
                            op0=ALU.mult, op1=ALU.add), r=[tsrc, t_c], w=[t_accs[c]])
                        for kk in range(1, 31):
                            k.op(ce, lambda c=c, kk=kk, ext=ext, accv=accv, ceh=ceh: ceh.scalar_tensor_tensor(
                                out=accv(), in0=ext(kk), scalar=pv(PV_CW, kk * 4 + c), in1=accv(),
                                op0=ALU.mult, op1=ALU.add), r=[tsrc, t_c, t_accs[c]], w=[t_accs[c]])
                    if j == 3:
                        pst, tp = ps()

                        def trc(pst=pst):
                            ins = None
                            for c in range(4):
                                ins = nc.tensor.transpose(pst[0:30, c * 128:(c + 1) * 128], uroll[:, c, 512:542], ident)
                            return ins
                        k.op(pe, trc, r=[t_ur, t_c], w=[tp])
                        k.op(act, lambda pst=pst: nc.scalar.copy(out=cvo[:], in_=pst[0:30, :]), r=[tp], w=[t_cvo])
                        odma(o_convp, cvo[:], [t_cvo])
                    if j == 4:
                        for b in range(4):
                            pst, tp = ps()

                            def trs(pst=pst, b=b):
                                ins = None
                                for c in range(4):
                                    ins = nc.tensor.transpose(pst[0:4, c * 128:(c + 1) * 128], us[:, c, b, 30:34], ident)
                                return ins
                            k.op(pe, trs, r=[t_us, t_c], w=[tp])
                            k.op(act, lambda pst=pst, b=b: nc.scalar.copy(out=cvs[:, b, :], in_=pst[0:4, :]),
                                 r=[tp], w=[t_cvs])
                        odma(o_convs[:, 26:30, :].rearrange("b t f -> t b f"), cvs[:], [t_cvs])
                        odma(o_convs[:, 0:26, :], sconv.rearrange("(b s) f -> b s f", b=4)[:, 4:30, :], [])
                    if j == 4:
                        for c in range(4):
                            k.op(act, lambda c=c: nc.scalar.copy(out=sqb[:, c, 0:n], in_=acc[:, c, 0:n]), r=[t_accs[c]], w=[t_sqb])
                            k.op(act, lambda c=c: nc.scalar.activation(out=sqb[:, 4 + c, 0:n], in_=acc[:, c, 0:n],
                                                                       func=AF.Square), r=[t_accs[c]], w=[t_sqb])
                        pm, tpm = ps()
                        pq, tpq = ps()

                        def mmst(pm=pm, pq=pq):
                            ins = None
                            for c in range(4):
                                nc.tensor.matmul(pm[:, 0:n], o512[:], sqb[:, c, 0:n], start=(c == 0), stop=(c == 3))
                            for c in range(4):
                                ins = nc.tensor.matmul(pq[:, 0:n], o512[:], sqb[:, 4 + c, 0:n], start=(c == 0), stop=(c == 3))
                            return ins
                        k.op(pe, mmst, r=[t_sqb, t_c], w=[tpm, tpq])
                        k.op(act, lambda pm=pm: nc.scalar.copy(out=mean_sb[:, 0:n], in_=pm[:, 0:n]), r=[tpm], w=[t_ln])
                        k.op(dve, lambda: nc.vector.tensor_tensor(out=var_sb[:, 0:n], in0=mean_sb[:, 0:n], in1=mean_sb[:, 0:n],
                                                                  op=ALU.mult), r=[t_ln], w=[t_ln])
                        k.op(dve, lambda pq=pq: nc.vector.tensor_tensor(out=var_sb[:, 0:n], in0=pq[:, 0:n], in1=var_sb[:, 0:n],
                                                                        op=ALU.subtract), r=[tpq, t_ln], w=[t_ln])
                        rsqrt_to(rstd_c[:, 0:n], var_sb[:, 0:n], 1.0, [t_ln], t_ln)
                        for c in range(4):
                            k.op(dve, lambda c=c: nc.vector.tensor_tensor(out=tt1[:, 0:n], in0=acc[:, c, 0:n],
                                                                          in1=mean_sb[:, 0:n], op=ALU.subtract),
                                 r=[t_accs[c], t_ln], w=[t_tt])
                            k.op(dve, lambda: nc.vector.tensor_tensor(out=tt2[:, 0:n], in0=tt1[:, 0:n], in1=rstd_c[:, 0:n],
                                                                      op=ALU.mult), r=[t_tt, t_ln], w=[t_tt])
                            k.op(act, lambda c=c: nc.scalar.activation(out=mixc_s[:, c, 0:n], in_=tt2[:, 0:n], func=AF.Silu,
                                                                       scale=pv(PV_LG, c), bias=pv(PV_LB, c)),
                                 r=[t_tt, t_c], w=[t_mixc[j]])
                    for c in range(4):
                        pq_, tq_ = proj(1024 + c * 128, 128)
                        k.op(act, lambda c=c, pq_=pq_: nc.scalar.copy(out=qdn[:, c, 0:n], in_=pq_[:, 0:n]), r=[tq_], w=[t_qdn])
                    rstd_from([qdn[:, c, 0:n] for c in range(4)], n, o512, sqb, t_sqb, rstd_q, t_rq, [t_qdn])
                    for c in range(4):
                        k.op(dve, lambda c=c: nc.vector.scalar_tensor_tensor(
                            out=qn[:, c, 0:n], in0=qdn[:, c, 0:n], scalar=pv(PV_QN, c), in1=rstd_q[:, 0:n],
                            op0=ALU.mult, op1=ALU.mult), r=[t_qdn, t_rq, t_c], w=[t_qn])
                    k.dma(sp, d_rope, rc_t[64:96, 0:n], ropec_d[:, t0:t0 + n], w=[t_rope])
                    k.dma(sp, d_rope, rs_t[64:96, 0:n], ropes_d[:, t0:t0 + n], w=[t_rope])
                    for h in range(8):
                        pa_, ta_ = ps()
                        pb_, tb_ = ps()

                        def mmq(h=h, pa_=pa_, pb_=pb_):
                            ins = None
                            for c in range(4):
                                nc.tensor.matmul(pa_[0:96, 0:n], wuqb[:, c, h * 96:(h + 1) * 96], qn[:, c, 0:n],
                                                 start=(c == 0), stop=(c == 3))
                            for c in range(4):
                                ins = nc.tensor.matmul(pb_[0:96, 0:n], wuqs[:, c, h, :], qn[:, c, 0:n],
                                                       start=(c == 0), stop=(c == 3))
                            return ins
                        k.op(pe, mmq, r=[t_qn, t_w2], w=[ta_, tb_])
                        k.op(act, lambda h=h, pa_=pa_: nc.scalar.copy(out=qall[0:64, h, t0:t0 + n], in_=pa_[0:64, 0:n]),
                             r=[ta_], w=[t_qall[j]])
                        k.op(dve, lambda pa_=pa_: nc.vector.tensor_tensor(out=tt1[64:96, 0:n], in0=pa_[64:96, 0:n],
                                                                          in1=rc_t[64:96, 0:n], op=ALU.mult),
                             r=[ta_, t_rope], w=[t_tt])
                        k.op(dve, lambda pb_=pb_: nc.vector.tensor_tensor(out=tt2[64:96, 0:n], in0=pb_[64:96, 0:n],
                                                                          in1=rs_t[64:96, 0:n], op=ALU.mult),
                             r=[tb_, t_rope], w=[t_tt])
                        k.op(dve, lambda h=h: nc.vector.tensor_tensor(out=qall[64:96, h, t0:t0 + n], in0=tt1[64:96, 0:n],
                                                                      in1=tt2[64:96, 0:n], op=ALU.add),
                             r=[t_tt], w=[t_qall[j]])
                    for c in range(2):
                        pk_, tk_ = proj(1536 + c * 128, 128)
                        k.op(act, lambda c=c, pk_=pk_: nc.scalar.copy(out=kvf[:, c, 0:n], in_=pk_[:, 0:n]), r=[tk_], w=[t_kvf])
                    rstd_from([kvf[:, c, 0:n] for c in range(2)], n, o256, sqb, t_sqb, rstd_k, t_rk, [t_kvf])
                    for c in range(2):
                        k.op(dve, lambda c=c: nc.vector.scalar_tensor_tensor(
                            out=kvf[:, c, 0:n], in0=kvf[:, c, 0:n], scalar=pv(PV_KVN, c), in1=rstd_k[:, 0:n],
                            op0=ALU.mult, op1=ALU.mult), r=[t_kvf, t_rk, t_c], w=[t_kvf])
                        k.op(act, lambda c=c: nc.scalar.copy(out=ckvT[:, c, t0:t0 + n], in_=kvf[:, c, 0:n]),
                             r=[t_kvf], w=[t_ckvT[j]])
                    if j < 4:
                        for s in range(4):
                            pst, tp = ps()

                            def trk(pst=pst, s=s):
                                ins = None
                                for c in range(2):
                                    ins = nc.tensor.transpose(pst[:, c * 128:(c + 1) * 128], kvf[:, c, s * 128:(s + 1) * 128], ident)
                                return ins
                            k.op(pe, trk, r=[t_kvf, t_c], w=[tp])
                            k.op(act, lambda pst=pst, s=s: nc.scalar.copy(out=ckvo[:, s, :], in_=pst[:, 0:256]), r=[tp], w=[t_ckvo])
                        odma(o_ckvp[t0:t0 + 512, :].rearrange("(s p) f -> p s f", p=128), ckvo[:], [t_ckvo])
                    else:
                        pst, tp = ps()

                        def trk2(pst=pst):
                            ins = None
                            for c in range(2):
                                ins = nc.tensor.transpose(pst[0:16, c * 128:(c + 1) * 128], kvf[:, c, 0:16], ident)
                            return ins
                        k.op(pe, trk2, r=[t_kvf, t_c], w=[tp])
                        k.op(act, lambda pst=pst: nc.scalar.copy(out=ckvo[0:16, 0, :], in_=pst[0:16, 0:256]), r=[tp], w=[t_ckvo])
                        odma(o_ckvs, ckvo[0:16, 0, :], [t_ckvo])
                        k.op(dve, lambda: nc.vector.memset(newkv[:], 1.0), w=[t_newkv])
                        for b in range(4):
                            pst, tp = ps()

                            def trk3(pst=pst, b=b):
                                ins = None
                                for c in range(2):
                                    ins = nc.tensor.transpose(pst[0:4, c * 128:(c + 1) * 128], kvf[:, c, 4 * b:4 * b + 4], ident)
                                return ins
                            k.op(pe, trk3, r=[t_kvf, t_c], w=[tp])
                            k.op(act, lambda pst=pst, b=b: nc.scalar.copy(out=newkv[:, b, 0:256], in_=pst[0:4, 0:256]),
                                 r=[tp], w=[t_newkv])
                    pka, tka = proj(0, 96, wkpe, 0)
                    pkb, tkb = proj(0, 96, wkpe, 1)
                    k.op(dve, lambda pka=pka: nc.vector.tensor_tensor(out=tt1[64:96, 0:n], in0=pka[64:96, 0:n],
                                                                      in1=rc_t[64:96, 0:n], op=ALU.mult),
                         r=[tka, t_rope], w=[t_tt])
                    k.op(dve, lambda pkb=pkb: nc.vector.tensor_tensor(out=tt2[64:96, 0:n], in0=pkb[64:96, 0:n],
                                                                      in1=rs_t[64:96, 0:n], op=ALU.mult),
                         r=[tkb, t_rope], w=[t_tt])
                    k.op(dve, lambda: nc.vector.tensor_tensor(out=kpef[64:96, 0:n], in0=tt1[64:96, 0:n],
                                                              in1=tt2[64:96, 0:n], op=ALU.add), r=[t_tt], w=[t_kpef])
                    k.op(act, lambda: nc.scalar.copy(out=kper[64:96, t0:t0 + n], in_=kpef[64:96, 0:n]),
                         r=[t_kpef], w=[t_kper[j]])
                    if j < 4:
                        pst, tp = ps()

                        def trp(pst=pst):
                            ins = None
                            for s in range(4):
                                ins = nc.tensor.transpose(pst[:, s * 32:(s + 1) * 32], kpef[64:96, s * 128:(s + 1) * 128],
                                                          ident[64:96, 64:96])
                            return ins
                        k.op(pe, trp, r=[t_kpef, t_c], w=[tp])
                        k.op(act, lambda pst=pst: nc.scalar.copy(out=kpeo[:].rearrange("p s f -> p (s f)"), in_=pst[:, 0:128]),
                             r=[tp], w=[t_kpeo])
                        odma(o_kpep[t0:t0 + 512, :].rearrange("(s p) f -> p s f", p=128), kpeo[:], [t_kpeo])
                    else:
                        pst, tp = ps()
                        k.op(pe, lambda pst=pst: nc.tensor.transpose(pst[0:16, 0:32], kpef[64:96, 0:16], ident[64:96, 64:96]),
                             r=[t_kpef, t_c], w=[tp])
                        k.op(act, lambda pst=pst: nc.scalar.copy(out=kpeo[0:16, 0, :], in_=pst[0:16, 0:32]), r=[tp], w=[t_kpeo])
                        odma(o_kpes, kpeo[0:16, 0, :], [t_kpeo])
                        k.op(dve, lambda: nc.vector.tensor_copy(out=kpes0[:, :], in_=kpef[64:96, 0:16]), r=[t_kpef], w=[t_kpes0])
                k.barrier()
            if stage <= 1:
                _finish(nc, k, out_ds)
                return nc

            AT = ExitStack()
            with AT:
                mixa = sb(AT, [128, 4, T], BF16, "mixa")
                t_mixa = Tok()
                kh = sb(AT, [96, NP_], BF16, "kh")
                t_kh = Tok()
                VV = [sb(AT, [128, 16, 128], BF16, "VA"), sb(AT, [128, 16, 128], BF16, "VB")]
                t_V = [Tok(), Tok()]
                mixc = sb(AT, [128, 4, T], BF16, "mixc")
                blk = sb(AT, [128, 4096], F32, "blk")
                acc2 = blk[:, 0:2048].rearrange("p (c t) -> p c t", c=4)
                t_acc2 = [Tok() for _ in range(4)]
                sqb2 = blk[:, 2048:4096].bitcast(BF16).rearrange("p (c t) -> p c t", c=8)
                t_sqb2 = Tok()
                mean2 = sb(AT, [128, 512], F32, "mean2")
                var2 = sb(AT, [128, 512], F32, "var2")
                rstdc2 = sb(AT, [128, 512], F32, "rstdc2")
                t_ln2 = Tok()
                tq1 = sb(AT, [128, 512], F32, "tq1")
                tq2 = sb(AT, [128, 512], F32, "tq2")
                t_tq = Tok()

                def conv_gen():
                    for j in range(4):
                        t0 = j * 512
                        n = 512
                        ru = [t_ust[j]] + ([t_ust[j - 1]] if j > 0 else [])
                        for c in range(4):
                            k.op(dve, lambda c=c, t0=t0: nc.vector.tensor_scalar(
                                out=acc2[:, c, :], in0=ustore[:, c, t0:t0 + 512], scalar1=pv(PV_CW, c), scalar2=pv(PV_CB, c),
                                op0=ALU.mult, op1=ALU.add), r=ru + [t_c], w=[t_acc2[c]])
                            for kk in range(1, 31):
                                k.op(dve, lambda c=c, kk=kk, t0=t0: nc.vector.scalar_tensor_tensor(
                                    out=acc2[:, c, :], in0=ustore[:, c, t0 + kk:t0 + kk + 512], scalar=pv(PV_CW, kk * 4 + c),
                                    in1=acc2[:, c, :], op0=ALU.mult, op1=ALU.add), r=ru + [t_c, t_acc2[c]], w=[t_acc2[c]])
                                if kk % 4 == 0:
                                    yield
                            yield
                        for c in range(4):
                            k.op(act, lambda c=c: nc.scalar.copy(out=sqb2[:, c, :], in_=acc2[:, c, :]), r=[t_acc2[c]], w=[t_sqb2])
                            k.op(act, lambda c=c: nc.scalar.activation(out=sqb2[:, 4 + c, :], in_=acc2[:, c, :], func=AF.Square),
                                 r=[t_acc2[c]], w=[t_sqb2])
                        pm, tpm = ps()
                        pq, tpq = ps()

                        def mmst2(pm=pm, pq=pq):
                            ins = None
                            for c in range(4):
                                nc.tensor.matmul(pm[:, 0:n], o512[:], sqb2[:, c, :], start=(c == 0), stop=(c == 3))
                            for c in range(4):
                                ins = nc.tensor.matmul(pq[:, 0:n], o512[:], sqb2[:, 4 + c, :], start=(c == 0), stop=(c == 3))
                            return ins
                        k.op(pe, mmst2, r=[t_sqb2, t_c], w=[tpm, tpq])
                        k.op(act, lambda pm=pm: nc.scalar.copy(out=mean2[:, :], in_=pm[:, 0:n]), r=[tpm], w=[t_ln2])
                        k.op(dve, lambda: nc.vector.tensor_tensor(out=var2[:, :], in0=mean2[:, :], in1=mean2[:, :], op=ALU.mult),
                             r=[t_ln2], w=[t_ln2])
                        k.op(dve, lambda pq=pq: nc.vector.tensor_tensor(out=var2[:, :], in0=pq[:, 0:n], in1=var2[:, :], op=ALU.subtract),
                             r=[tpq, t_ln2], w=[t_ln2])
                        rsqrt_to(rstdc2[:, :], var2[:, :], 1.0, [t_ln2], t_ln2)
                        for c in range(4):
                            k.op(dve, lambda c=c: nc.vector.tensor_tensor(out=tq1[:, :], in0=acc2[:, c, :], in1=mean2[:, :],
                                                                          op=ALU.subtract), r=[t_acc2[c], t_ln2], w=[t_tq])
                            k.op(dve, lambda: nc.vector.tensor_tensor(out=tq2[:, :], in0=tq1[:, :], in1=rstdc2[:, :], op=ALU.mult),
                                 r=[t_tq, t_ln2], w=[t_tq])
                            k.op(act, lambda c=c, t0=t0: nc.scalar.activation(out=mixc[:, c, t0:t0 + 512], in_=tq2[:, :], func=AF.Silu,
                                                                              scale=pv(PV_LG, c), bias=pv(PV_LB, c)),
                                 r=[t_tq, t_c], w=[t_mixc[j]])
                        yield
                pts = [sb(AT, [128, 512], BF16, "pt") for _ in range(3)]
                t_pts = [Tok() for _ in range(3)]
                rd = sb(AT, [128, 512], F32, "rd")
                t_rd = Tok()
                k.op(dve, lambda: nc.vector.memset(VV[0][:], 1.0), w=[t_V[0]])
                k.op(dve, lambda: nc.vector.memset(VV[1][:], 1.0), w=[t_V[1]])
                k.op(dve, lambda: nc.vector.tensor_copy(out=kh[64:96, :], in_=kper[64:96, 0:NP_]), r=t_kper, w=[t_kh])
                SA = AT
                if True:
                    wkf = sb(SA, [128, 2, 1024], F32, "wkf")
                    t_wkf = Tok()
                    wukvT = sb(SA, [128, 8, 256], BF16, "wukvT")
                    t_wT = Tok()
                    ptb = sb(SA, [128, 512], I32, "ptb")
                    idxf = sb(SA, [128, 512], F32, "idxf")
                    idx = sb(SA, [128, 512], I32, "idx")
                    t_idx = Tok()
                    d_sa = k.dsem()
                    NPG = 8
                    pgf = [sb(SA, [128, 288], F32, "pgf") for _ in range(NPG)]
                    t_pgf = [Tok() for _ in range(NPG)]
                    d_pg = [k.dsem() for _ in range(NPG)]
                    CT = [sb(SA, [128, 3, 128], BF16, "CT") for _ in range(3)]
                    t_CT = [Tok(), Tok(), Tok()]
                    pgb = [sb(SA, [128, 257], BF16, "pgb") for _ in range(16)]
                    t_pgb = [Tok() for _ in range(16)]
                    QL = sb(SA, [128, 3, 32], BF16, "QL")
                    t_QL = Tok()
                    PT = sb(SA, [128, 512], BF16, "PT")
                    t_PT = Tok()
                    pn = sb(SA, [4, 32], BF16, "pn")
                    t_pn = Tok()
                    rds = sb(SA, [32, 1], F32, "rds")
                    ol = sb(SA, [32, 256], F32, "ol")
                    t_ol = Tok()
                    olT = sb(SA, [128, 2, 32], BF16, "olT")
                    t_olT = Tok()
                    if stage >= 3:
                        k.dma(sp, d_sa, wkf[:], w_ukv.rearrange("(c p) n -> p c n", p=128), w=[t_wkf])
                        k.dma(sp, d_sa, ptb[:], ptab, w=[t_idx])
                        for h in range(8):
                            pst, tp = ps()

                            def trw(pst=pst, h=h):
                                ins = None
                                for c in range(2):
                                    ins = nc.tensor.transpose(pst[:, c * 128:(c + 1) * 128], wkf[:, c, h * 128:(h + 1) * 128], ident)
                                return ins
                            k.op(pe, trw, r=[t_wkf, t_c], w=[tp])
                            k.op(act, lambda pst=pst, h=h: nc.scalar.copy(out=wukvT[:, h, :], in_=pst[:, 0:256]), r=[tp], w=[t_wT])
                        k.op(dve, lambda: nc.vector.tensor_copy(out=idxf[:], in_=ptb[:]), r=[t_idx], w=[t_idx])
                        k.op(dve, lambda: nc.vector.tensor_scalar(out=idxf[:], in0=idxf[:], scalar1=128.0,
                                                                  scalar2=cst[:, C_IOTA:C_IOTA + 1], op0=ALU.mult, op1=ALU.add),
                             r=[t_idx, t_c], w=[t_idx])
                        k.op(dve, lambda: nc.vector.tensor_copy(out=idx[:], in_=idxf[:]), r=[t_idx], w=[t_idx])
                        for i in range(16):
                            k.op(dve, lambda i=i: nc.vector.memset(pgb[i][:], 1.0), w=[t_pgb[i]])
                        npages = 128 if not SMALLW else 2
                    def sample_gen():
                        for b in range(4):
                            c0 = NP_ + 4 * b
                            pst, tp = ps()

                            def mmql(pst=pst, c0=c0):
                                ins = None
                                for rc in range(2):
                                    for h in range(8):
                                        ins = nc.tensor.matmul(pst[:, rc * 32 + h * 4:rc * 32 + h * 4 + 4],
                                                               wukvT[0:64, h, rc * 128:(rc + 1) * 128],
                                                               qall[0:64, h, c0:c0 + 4], start=True, stop=True)
                                return ins
                            k.op(pe, mmql, r=[t_wT, t_qall[4]], w=[tp])
                            k.op(act, lambda pst=pst: nc.scalar.copy(out=QL[:, 0:2, :],
                                                                     in_=pst[:, 0:64].rearrange("p (r q) -> p r q", r=2)),
                                 r=[tp], w=[t_QL])
                            k.op(dve, lambda c0=c0: nc.vector.tensor_copy(
                                out=QL[0:32, 2, :].rearrange("p (h t) -> p h t", h=8), in_=qall[64:96, :, c0:c0 + 4]),
                                r=[t_qall[4]], w=[t_QL])
                            pso, tpso = banks[7]
                            first = [True]
                            for g0 in range(0, npages, 16):
                                gn = min(16, npages - g0)
                                psS, tpS = banks[4]
                                pend_s = None
                                for s_ in range(gn):
                                    pg = g0 + s_
                                    col = b * 128 + pg
                                    bi = pg % NPG
                                    pb = pg % 16
                                    k.idma(d_pg[bi], pgf[bi][:, 0:288], ccat, idx[:, col:col + 1], r=[t_idx], w=[t_pgf[bi]])
                                    pst, tp = ps()

                                    def trp2(pst=pst, bi=bi):
                                        nc.tensor.transpose(pst[:, 0:128], pgf[bi][:, 0:128], ident)
                                        nc.tensor.transpose(pst[:, 128:256], pgf[bi][:, 128:256], ident)
                                        return nc.tensor.transpose(pst[0:32, 256:384], pgf[bi][:, 256:288], ident)
                                    k.op(pe, trp2, r=[t_pgf[bi], t_c], w=[tp])
                                    ct = CT[pg % 3]
                                    tct = t_CT[pg % 3]
                                    k.op(act, lambda pst=pst, ct=ct: nc.scalar.copy(
                                        out=ct[:, 0:2, :], in_=pst[:, 0:256].rearrange("p (r q) -> p r q", r=2)), r=[tp], w=[tct])
                                    k.op(dve, lambda pst=pst, ct=ct: nc.vector.tensor_copy(out=ct[0:32, 2, :], in_=pst[0:32, 256:384]),
                                         r=[tp], w=[tct])
                                    k.op(act, lambda bi=bi, pb=pb: nc.scalar.copy(out=pgb[pb][:, 0:256], in_=pgf[bi][:, 0:256]),
                                         r=[t_pgf[bi]], w=[t_pgb[pb]])

                                    def mms(psS=psS, ct=ct, s_=s_):
                                        nc.tensor.matmul(psS[:, s_ * 32:(s_ + 1) * 32], ct[:, 0, :], QL[:, 0, :], start=True, stop=False)
                                        nc.tensor.matmul(psS[:, s_ * 32:(s_ + 1) * 32], ct[:, 1, :], QL[:, 1, :], start=False, stop=False)
                                        return nc.tensor.matmul(psS[:, s_ * 32:(s_ + 1) * 32], ct[0:32, 2, :], QL[0:32, 2, :],
                                                                start=False, stop=True)
                                    if pend_s is not None:
                                        k.op(pe, pend_s[0], r=[pend_s[1], t_QL], w=[tpS])
                                    pend_s = (mms, tct)
                                k.op(pe, pend_s[0], r=[pend_s[1], t_QL], w=[tpS])
                                pend_s = None
                                k.op(act, lambda psS=psS, gn=gn: nc.scalar.activation(out=PT[:, 0:gn * 32], in_=psS[:, 0:gn * 32],
                                                                                     func=AF.Exp, scale=SCALE), r=[tpS], w=[t_PT])
                                for s_ in range(gn):
                                    pb = (g0 + s_) % 16
                                    k.op(pe, lambda pso=pso, s_=s_, pb=pb, st=first[0]: nc.tensor.matmul(
                                        pso[0:32, 0:257], PT[:, s_ * 32:(s_ + 1) * 32], pgb[pb][:, :], start=st, stop=False,
                                        skip_group_check=True), r=[t_PT, t_pgb[pb]], w=[tpso])
                                    first[0] = False
                                yield
                            psn, tpn = ps()

                            def mmn(psn=psn, c0=c0, b=b):
                                nc.tensor.matmul(psn[0:4, 0:32], ckvT[:, 0, c0:c0 + 4], QL[:, 0, :], start=True, stop=False)
                                nc.tensor.matmul(psn[0:4, 0:32], ckvT[:, 1, c0:c0 + 4], QL[:, 1, :], start=False, stop=False)
                                return nc.tensor.matmul(psn[0:4, 0:32], kpes0[:, 4 * b:4 * b + 4], QL[0:32, 2, :], start=False, stop=True)
                            k.op(pe, mmn, r=[t_ckvT[4], t_QL, t_kpes0], w=[tpn])
                            k.op(act, lambda psn=psn: nc.scalar.activation(out=pn[:, :], in_=psn[0:4, 0:32], func=AF.Exp, scale=SCALE),
                                 r=[tpn], w=[t_pn])
                            k.op(dve, lambda: nc.vector.tensor_tensor(out=pn[:, :], in0=pn[:, :], in1=mnew[:, :], op=ALU.mult),
                                 r=[t_pn, t_c], w=[t_pn])
                            k.op(pe, lambda pso=pso, b=b, st=first[0]: nc.tensor.matmul(
                                pso[0:32, 0:257], pn[:, :], newkv[:, b, :], start=st, stop=True, skip_group_check=True),
                                r=[t_pn, t_newkv], w=[tpso])
                            k.op(dve, lambda pso=pso: nc.vector.reciprocal(out=rds[:, :], in_=pso[0:32, 256:257]), r=[tpso], w=[t_ol])
                            k.op(dve, lambda pso=pso: nc.vector.tensor_scalar(out=ol[:, :], in0=pso[0:32, 0:256], scalar1=rds[:, 0:1],
                                                                              scalar2=1.0, op0=ALU.mult, op1=ALU.mult),
                                 r=[tpso, t_ol], w=[t_ol])
                            pst, tp = ps()

                            def tro(pst=pst):
                                nc.tensor.transpose(pst[:, 0:32], ol[:, 0:128], ident[0:32, 0:32])
                                return nc.tensor.transpose(pst[:, 32:64], ol[:, 128:256], ident[0:32, 0:32])
                            k.op(pe, tro, r=[t_ol, t_c], w=[tp])
                            k.op(act, lambda pst=pst: nc.scalar.copy(out=olT[:, :, :], in_=pst[:, 0:64].rearrange("p (r q) -> p r q", r=2)),
                                 r=[tp], w=[t_olT])
                            pst, tp = ps()

                            def mmo(pst=pst):
                                ins = None
                                for h in range(8):
                                    for rc in range(2):
                                        ins = nc.tensor.matmul(pst[0:64, h * 4:h * 4 + 4], wukv[:, rc, h * 128 + 64:h * 128 + 128],
                                                               olT[:, rc, h * 4:h * 4 + 4], start=(rc == 0), stop=(rc == 1))
                                return ins
                            k.op(pe, mmo, r=[t_olT, t_wukv], w=[tp])
                            pv_ = pst[0:64, 0:32].rearrange("p (g e t) -> p g e t", g=4, e=2)
                            k.op(act, lambda pv_=pv_, c0=c0: nc.scalar.copy(out=mixa[0:64, :, c0:c0 + 4], in_=pv_[:, :, 0, :]),
                                 r=[tp], w=[t_mixa])
                            k.op(dve, lambda pv_=pv_, c0=c0: nc.vector.tensor_copy(out=mixa[64:128, :, c0:c0 + 4], in_=pv_[:, :, 1, :]),
                                 r=[tp], w=[t_mixa])
                            yield

                pti = [0]
                nrot[0] = 4
                sgen = sample_gen() if stage >= 3 else iter(())
                cgen = conv_gen()
                for h in range(8 if stage >= 2 else 0):
                    V = VV[h % 2]
                    tV = t_V[h % 2]
                    noff = 0 if h % 2 == 0 else 64
                    doff = 64 - noff
                    for j in range(4):
                        t0 = j * 512
                        pst, tp = ps()

                        def mmk(pst=pst, t0=t0, h=h):
                            ins = None
                            for c in range(2):
                                ins = nc.tensor.matmul(pst[0:64, 0:512], wukv[:, c, h * 128:h * 128 + 64],
                                                       ckvT[:, c, t0:t0 + 512], start=(c == 0), stop=(c == 1))
                            return ins
                        k.op(pe, mmk, r=[t_wukv, t_ckvT[j]], w=[tp])
                        k.op(act, lambda pst=pst, t0=t0: nc.scalar.copy(out=kh[0:64, t0:t0 + 512], in_=pst[0:64, 0:512]),
                             r=[tp], w=[t_kh])
                    for half in range(2):
                        pst, tp = ps()

                        def mmv(pst=pst, half=half, h=h):
                            ins = None
                            for kt in range(8):
                                for c in range(2):
                                    ins = nc.tensor.matmul(pst[:, kt * 64:(kt + 1) * 64],
                                                           ckvT[:, c, (half * 8 + kt) * 128:(half * 8 + kt + 1) * 128],
                                                           wukv[:, c, h * 128 + 64:h * 128 + 128], start=(c == 0), stop=(c == 1))
                            return ins
                        k.op(pe, mmv, r=[t_wukv] + t_ckvT[0:4], w=[tp])
                        k.op(act, lambda pst=pst, half=half, V=V, noff=noff: nc.scalar.copy(
                            out=V[:, half * 8:(half + 1) * 8, noff:noff + 64],
                            in_=pst[:, 0:512].rearrange("p (k d) -> p k d", k=8)), r=[tp], w=[tV])
                    for j in range(4):
                        po, tpo = psacc()
                        nkt = 4 * j + 4
                        pend = None

                        def emit_pv(pt, tpt, kt, qlo, po=po, tpo=tpo, nkt=nkt, V=V, tV=tV):
                            k.op(pe, lambda: nc.tensor.matmul(
                                po[:, qlo:512], V[:, kt, :], pt[:, qlo:512], start=(kt == 0), stop=(kt == nkt - 1),
                                skip_group_check=True), r=[tpt, tV], w=[tpo])
                        for kt in range(nkt):
                            qlo = max(0, kt * 128 - j * 512)
                            pss, tps = ps()
                            k.op(pe, lambda pss=pss, kt=kt, qlo=qlo, j=j, h=h: nc.tensor.matmul(
                                pss[:, qlo:512], kh[:, kt * 128:(kt + 1) * 128], qall[:, h, j * 512 + qlo:(j + 1) * 512],
                                start=True, stop=True), r=[t_kh, t_qall[j]], w=[tps])
                            pt = pts[pti[0] % 3]
                            tpt = t_pts[pti[0] % 3]
                            pti[0] += 1
                            k.op(act, lambda pss=pss, pt=pt, qlo=qlo: nc.scalar.activation(
                                out=pt[:, qlo:512], in_=pss[:, qlo:512], func=AF.Exp, scale=SCALE), r=[tps], w=[tpt])
                            if kt >= 4 * j:
                                k.op(dve, lambda pt=pt, qlo=qlo: nc.vector.tensor_tensor(
                                    out=pt[:, qlo:qlo + 128], in0=pt[:, qlo:qlo + 128], in1=tri[:], op=ALU.mult),
                                    r=[tpt, t_c], w=[tpt])
                            if pend is not None:
                                emit_pv(*pend)
                            pend = (pt, tpt, kt, qlo)
                        emit_pv(*pend)
                        k.op(dve, lambda po=po, noff=noff, doff=doff: nc.vector.reciprocal(
                            out=rd[noff:noff + 64, :], in_=po[doff:doff + 64, :]), r=[tpo], w=[t_rd])
                        k.op(dve, lambda po=po, noff=noff, h=h, j=j: nc.vector.tensor_tensor(
                            out=mixa[noff:noff + 64, h // 2, j * 512:(j + 1) * 512], in0=po[noff:noff + 64, :],
                            in1=rd[noff:noff + 64, :], op=ALU.mult), r=[tpo, t_rd], w=[t_mixa])
                        next(sgen, None)
                        next(cgen, None)
                        next(cgen, None)
                        next(cgen, None)
                        next(cgen, None)

                for _ in sgen:
                    pass
                for _ in cgen:
                    pass
                k.barrier()
                nrot[0] = 6
                OP = ExitStack()
                with OP:
                    woutb = sb(OP, [128, 8, 1024], BF16, "woutb")
                    t_wo = Tok()
                    wload(d_w0, woutb[:], w_out.rearrange("(c p) n -> p c n", p=128), [t_wo])
                    xin2 = [wkf[:, 0, :], wkf[:, 1, :]]
                    t_xin2 = [Tok(), Tok()]
                    d_xin2 = [k.dsem(), k.dsem()]
                    xt = blk[:, :].rearrange("p (c t) -> p c t", c=8)
                    t_xt = Tok()
                    d_xs = k.dsem()
                    if stage >= 4:
                        for j, (t0, n) in enumerate(TILES):
                            nsub = (n + 127) // 128
                            for s in range(nsub):
                                rows = min(128, n - s * 128)
                                bi = (j * 4 + s) % 2
                                src = xp[t0 + s * 128:t0 + s * 128 + rows, :] if j < 4 else xs[:, :]
                                k.dma(sp, d_xin2[bi], xin2[bi][0:rows, :], src, w=[t_xin2[bi]])
                                for half in range(2):
                                    pst, tp = ps()

                                    def tr(bi=bi, rows=rows, half=half, pst=pst):
                                        ins = None
                                        for cc in range(4):
                                            c = half * 4 + cc
                                            ins = nc.tensor.transpose(pst[:, cc * 128:cc * 128 + rows],
                                                                      xin2[bi][0:rows, c * 128:(c + 1) * 128], ident[0:rows, 0:rows])
                                        return ins
                                    k.op(pe, tr, r=[t_xin2[bi], t_c], w=[tp])
                                    k.op(act, lambda half=half, s=s, rows=rows, pst=pst: nc.scalar.copy(
                                        out=xt[:, half * 4:half * 4 + 4, s * 128:s * 128 + rows],
                                        in_=pst[:, 0:512].rearrange("p (c t) -> p c t", c=4)[:, :, 0:rows]), r=[tp], w=[t_xt])
                            for oc in range(8):
                                pst, tp = ps()

                                def mmo2(pst=pst, oc=oc, t0=t0, n=n, j=j):
                                    ins = None
                                    for c in range(8):
                                        rhs = (mixc[:, c, t0:t0 + n] if j < 4 else mixc_s[:, c, 0:n]) if c < 4 else mixa[:, c - 4, t0:t0 + n]
                                        ins = nc.tensor.matmul(pst[:, 0:n], woutb[:, c, oc * 128:(oc + 1) * 128], rhs,
                                                               start=(c == 0), stop=(c == 7))
                                    return ins
                                k.op(pe, mmo2, r=[t_wo, t_mixc[j], t_mixa], w=[tp])
                                k.op(dve, lambda pst=pst, oc=oc, n=n: nc.vector.tensor_tensor(
                                    out=xt[:, oc, 0:n], in0=pst[:, 0:n], in1=xt[:, oc, 0:n], op=ALU.add), r=[tp, t_xt], w=[t_xt])
                            k.dma(sp, d_xs, xscr[:, :, t0:t0 + n], xt[:, :, 0:n], r=[t_xt])
                    k.barrier()


        xfm = sb(es, [128, 8, T], F32, "xfm")
        t_x = [Tok() for _ in TILES]
        d_x = k.dsem()
        for j, (t0, n) in enumerate(TILES):
            k.dma(sp, d_x, xfm[:, :, t0:t0 + n], xscr[:, :, t0:t0 + n], w=[t_x[j]])
        for j in range(len(TILES)):
            t_x[j].w = (d_x, d_x.cnt)
        sq2 = sb(es, [128, 8, 512], BF16, "sq2")
        t_sq2 = Tok()
        rstd2 = sb(es, [128, 512], F32, "rstd2")
        t_r2 = Tok()

        def fm_rstd(j):
            t0, n = TILES[j]
            rstd_from([xfm[:, c, t0:t0 + n] for c in range(8)], n, o1024, sq2, t_sq2, rstd2, t_r2, [t_x[j]])

        d_wgu = [k.dsem(), k.dsem()]
        d_wd = [k.dsem(), k.dsem()]
        hb = t_hb = wgu = t_wgu = wdb = t_wdb = hid = t_hid = sgt = t_sg = tmpm = t_tm = gbc = t_gbc = None
        gi = [0]

        def alloc_ff(FF):
            nonlocal hb, t_hb, wgu, t_wgu, wdb, t_wdb, hid, t_hid, sgt, t_sg, tmpm, t_tm, gbc, t_gbc
            hb = sb(FF, [128, 8, T], BF16, "hb")
            t_hb = [Tok() for _ in TILES]
            wgu = [sb(FF, [128, 2, 8, 512], BF16, "wgu") for _ in range(2)]
            t_wgu = [Tok(), Tok()]
            wdb = [sb(FF, [128, 4, 1024], BF16, "wdb") for _ in range(2)]
            t_wdb = [Tok(), Tok()]
            hid = sb(FF, [128, 4, T], BF16, "hid")
            t_hid = [Tok() for _ in TILES]
            sgt = sb(FF, [128, 512], F32, "sgt")
            t_sg = Tok()
            tmpm = sb(FF, [128, 512], F32, "tmpm")
            t_tm = Tok()
            gbc = sb(FF, [128, T], F32, "gbc")
            t_gbc = Tok()
        if True:

            def gated_ffn(Wg, Wu, Wd, dff, use_gate):
                nch = dff // 128
                for g0 in range(0, nch, 4):
                    gc = min(4, nch - g0)
                    bi = gi[0] % 2
                    gi[0] += 1
                    f0 = g0 * 128
                    wload(d_wgu[bi], wgu[bi][:, 0, :, 0:gc * 128], Wg[:, f0:f0 + gc * 128].rearrange("(c p) n -> p c n", p=128), [t_wgu[bi]])
                    wload(d_wgu[bi], wgu[bi][:, 1, :, 0:gc * 128], Wu[:, f0:f0 + gc * 128].rearrange("(c p) n -> p c n", p=128), [t_wgu[bi]])
                    wload(d_wd[bi], wdb[bi][:, 0:gc, :], Wd[f0:f0 + gc * 128, :].rearrange("(c p) n -> p c n", p=128), [t_wdb[bi]])
                    pendD = None

                    def emit_down(j, t0, n, bi, gc):
                        for oc in range(8):
                            pd_, td_ = ps()

                            def mmd(pd_=pd_, oc=oc):
                                ins = None
                                for fc in range(gc):
                                    ins = nc.tensor.matmul(pd_[:, 0:n], wdb[bi][:, fc, oc * 128:(oc + 1) * 128], hid[:, fc, t0:t0 + n],
                                                           start=(fc == 0), stop=(fc == gc - 1))
                                return ins
                            k.op(pe, mmd, r=[t_wdb[bi], t_hid[j]], w=[td_])
                            k.op(dve, lambda pd_=pd_, oc=oc: nc.vector.tensor_tensor(
                                out=xfm[:, oc, t0:t0 + n], in0=pd_[:, 0:n], in1=xfm[:, oc, t0:t0 + n], op=ALU.add),
                                r=[td_, t_x[j]], w=[t_x[j]])
                    for j, (t0, n) in enumerate(TILES):
                        for fc in range(gc):
                            pg_, tg_ = ps()
                            pu_, tu_ = ps()

                            def mmgu(pg_=pg_, pu_=pu_, fc=fc, t0=t0, n=n, bi=bi):
                                ins = None
                                for c in range(8):
                                    nc.tensor.matmul(pg_[:, 0:n], wgu[bi][:, 0, c, fc * 128:(fc + 1) * 128], hb[:, c, t0:t0 + n],
                                                     start=(c == 0), stop=(c == 7))
                                for c in range(8):
                                    ins = nc.tensor.matmul(pu_[:, 0:n], wgu[bi][:, 1, c, fc * 128:(fc + 1) * 128], hb[:, c, t0:t0 + n],
                                                           start=(c == 0), stop=(c == 7))
                                return ins
                            k.op(pe, mmgu, r=[t_wgu[bi], t_hb[j]], w=[tg_, tu_])
                            k.op(act, lambda pg_=pg_, n=n: nc.scalar.activation(out=sgt[:, 0:n], in_=pg_[:, 0:n], func=AF.Silu),
                                 r=[tg_], w=[t_sg])
                            if use_gate:
                                k.op(dve, lambda pu_=pu_, n=n: nc.vector.tensor_tensor(out=tmpm[:, 0:n], in0=pu_[:, 0:n], in1=sgt[:, 0:n],
                                                                                       op=ALU.mult), r=[tu_, t_sg], w=[t_tm])
                                k.op(dve, lambda fc=fc, t0=t0, n=n: nc.vector.tensor_tensor(
                                    out=hid[:, fc, t0:t0 + n], in0=tmpm[:, 0:n], in1=gbc[:, t0:t0 + n], op=ALU.mult),
                                    r=[t_tm, t_gbc], w=[t_hid[j]])
                            else:
                                k.op(dve, lambda pu_=pu_, fc=fc, t0=t0, n=n: nc.vector.tensor_tensor(
                                    out=hid[:, fc, t0:t0 + n], in0=pu_[:, 0:n], in1=sgt[:, 0:n], op=ALU.mult),
                                    r=[tu_, t_sg], w=[t_hid[j]])
                        if pendD is not None:
                            emit_down(*pendD)
                        pendD = (j, t0, n, bi, gc)
                    emit_down(*pendD)
                    pendD = None

            def norm_to_hb(pvcol):
                for j, (t0, n) in enumerate(TILES):
                    fm_rstd(j)
                    for c in range(8):
                        k.op(dve, lambda c=c, t0=t0, n=n: nc.vector.scalar_tensor_tensor(
                            out=hb[:, c, t0:t0 + n], in0=xfm[:, c, t0:t0 + n], scalar=pv(pvcol, c), in1=rstd2[:, 0:n],
                            op0=ALU.mult, op1=ALU.mult), r=[t_x[j], t_r2, t_c], w=[t_hb[j]])

            if stage >= 5:
                FF1 = ExitStack()
                with FF1:
                    alloc_ff(FF1)
                    norm_to_hb(PV_NF0)
                    gated_ffn(ffn_g, ffn_u, ffn_d, DFF, False)
                    k.barrier()

            if stage >= 6:
                PM = ExitStack()
                with PM:
                    tmpm = sb(PM, [128, 512], F32, "tmpm2")
                    t_tm = Tok()
                    pwb = sb(PM, [128, 4, 2, 256], BF16, "pwb")
                    t_pw = Tok()
                    wload(d_w0, pwb[:], pool_w.rearrange("g (c p) n -> p g c n", p=128), [t_pw])
                    L = 15 + 512
                    hf = sb(PM, [128, 8, L], F32, "hf")
                    t_hf = Tok()
                    sA = sb(PM, [128, 2, L], F32, "sA")
                    sB = sb(PM, [128, 2, L], F32, "sB")
                    t_s = Tok()
                    dif = sb(PM, [128, 8, 512], BF16, "dif")
                    t_dif = Tok()
                    hs = sb(PM, [128, 8, 4, 19], F32, "hs")
                    t_hs = Tok()
                    sAs = sb(PM, [128, 2, 4, 19], F32, "sAs")
                    sBs = sb(PM, [128, 2, 4, 19], F32, "sBs")
                    spl = sb(PM, [60, 1024], F32, "spl")
                    t_spl = Tok()
                    pout = sb(PM, [15, 1024], F32, "pout")
                    t_pout = Tok()
                    pouts = sb(PM, [4, 4, 1024], F32, "pouts")
                    t_pouts = Tok()
                    d_pm = k.dsem()
                    k.dma(sp, d_pm, spl[:], spool, w=[t_spl])
                    for c in range(8):
                        pst, tp = ps()
                        k.op(pe, lambda pst=pst, c=c: nc.tensor.transpose(pst[:, 0:60], spl[:, c * 128:(c + 1) * 128], ident[0:60, 0:60]),
                             r=[t_spl, t_c], w=[tp])
                        k.op(act, lambda pst=pst, c=c: nc.scalar.copy(out=hs[:, c, :, 0:15],
                                                                    in_=pst[:, 0:60].rearrange("p (b s) -> p b s", b=4)), r=[tp], w=[t_hs])
                    k.op(dve, lambda: nc.vector.memset(hf[:, :, 0:15], 0.0), w=[t_hf])
                    for j, (t0, n) in enumerate(TILES):
                        fm_rstd(j)
                        samp = (j == 4)
                        for c in range(8):
                            if not samp:
                                dst = hf[:, c, 15:15 + n]
                                xin_ = xfm[:, c, t0:t0 + n]
                                rin = rstd2[:, 0:n]
                            else:
                                dst = hs[:, c, :, 15:19]
                                xin_ = xfm[:, c, t0:t0 + 16].rearrange("p (b t) -> p b t", b=4)
                                rin = rstd2[:, 0:16].rearrange("p (b t) -> p b t", b=4)
                            k.op(dve, lambda c=c, dst=dst, xin_=xin_, rin=rin: nc.vector.scalar_tensor_tensor(
                                out=dst, in0=xin_, scalar=pv(PV_NM1, c), in1=rin, op0=ALU.mult, op1=ALU.mult),
                                r=[t_x[j], t_r2, t_c], w=[t_hs if samp else t_hf])
                        th = t_hs if samp else t_hf
                        if j == 3:
                            for half in range(2):
                                pst, tp = ps()

                                def trh(pst=pst, half=half):
                                    ins = None
                                    for cc in range(4):
                                        ins = nc.tensor.transpose(pst[0:15, cc * 128:(cc + 1) * 128], hf[:, half * 4 + cc, 512:527], ident)
                                    return ins
                                k.op(pe, trh, r=[t_hf, t_c], w=[tp])
                                k.op(act, lambda pst=pst, half=half: nc.scalar.copy(out=pout[:, half * 512:(half + 1) * 512], in_=pst[0:15, :]),
                                     r=[tp], w=[t_pout])
                            odma(o_poolp, pout[:], [t_pout])
                        if samp:
                            for b in range(4):
                                for half in range(2):
                                    pst, tp = ps()

                                    def trh2(pst=pst, half=half, b=b):
                                        ins = None
                                        for cc in range(4):
                                            ins = nc.tensor.transpose(pst[0:4, cc * 128:(cc + 1) * 128], hs[:, half * 4 + cc, b, 15:19], ident)
                                        return ins
                                    k.op(pe, trh2, r=[t_hs, t_c], w=[tp])
                                    k.op(act, lambda pst=pst, half=half, b=b: nc.scalar.copy(
                                        out=pouts[:, b, half * 512:(half + 1) * 512], in_=pst[0:4, :]), r=[tp], w=[t_pouts])
                            odma(o_pools[:, 11:15, :].rearrange("b t f -> t b f"), pouts[:], [t_pouts])
                            odma(o_pools[:, 0:11, :], spool.rearrange("(b s) f -> b s f", b=4)[:, 4:15, :], [])
                        for g in range(4):
                            w_ = 2 << g
                            if not samp:
                                cur = hf[:, 2 * g:2 * g + 2, :]
                                A, B = sA[:, :, :], sB[:, :, :]
                                LL = 15 + n
                            else:
                                cur = hs[:, 2 * g:2 * g + 2, :, :]
                                A, B = sAs[:, :, :, :], sBs[:, :, :, :]
                                LL = 19
                            sh = 1
                            src = cur
                            tsrc = th
                            flip = 0
                            while sh < w_:
                                dst = A if flip == 0 else B
                                if not samp:
                                    o_, a_, b_ = dst[:, :, sh:LL], src[:, :, sh:LL], src[:, :, 0:LL - sh]
                                else:
                                    o_, a_, b_ = dst[:, :, :, sh:LL], src[:, :, :, sh:LL], src[:, :, :, 0:LL - sh]
                                k.op(dve, lambda o_=o_, a_=a_, b_=b_: nc.vector.tensor_tensor(out=o_, in0=a_, in1=b_, op=ALU.add),
                                     r=[tsrc, t_s], w=[t_s])
                                src = dst
                                tsrc = t_s
                                flip ^= 1
                                sh *= 2
                            if not samp:
                                sw = src[:, :, 15:15 + n]
                                hh = hf[:, 2 * g:2 * g + 2, 15:15 + n]
                                dd = dif[:, 2 * g:2 * g + 2, 0:n]
                            else:
                                sw = src[:, :, :, 15:19]
                                hh = hs[:, 2 * g:2 * g + 2, :, 15:19]
                                dd = dif[:, 2 * g:2 * g + 2, 0:16].rearrange("p c (b t) -> p c b t", b=4)
                            k.op(dve, lambda sw=sw, hh=hh, dd=dd, w_=w_: nc.vector.scalar_tensor_tensor(
                                out=dd, in0=sw, scalar=1.0 / w_, in1=hh, op0=ALU.mult, op1=ALU.subtract), r=[t_s, th], w=[t_dif])
                            if j == 0:
                                for cc in range(2):
                                    k.op(dve, lambda cc=cc, g=g, src=src: nc.vector.tensor_tensor(
                                        out=tmpm[:, 0:16], in0=src[:, cc, 15:31], in1=cst[:, C_INV + g * 16:C_INV + g * 16 + 16],
                                        op=ALU.mult), r=[t_s, t_c], w=[t_tm])
                                    k.op(dve, lambda cc=cc, g=g: nc.vector.tensor_tensor(
                                        out=dif[:, 2 * g + cc, 0:16], in0=tmpm[:, 0:16], in1=hf[:, 2 * g + cc, 15:31], op=ALU.subtract),
                                        r=[t_tm, t_hf], w=[t_dif])
                        for g in range(4):
                            for oc in range(2):
                                pst, tp = ps()

                                def mmp(pst=pst, g=g, oc=oc, n=n):
                                    ins = None
                                    for c in range(2):
                                        ins = nc.tensor.matmul(pst[:, 0:n], pwb[:, g, c, oc * 128:(oc + 1) * 128], dif[:, 2 * g + c, 0:n],
                                                               start=(c == 0), stop=(c == 1))
                                    return ins
                                k.op(pe, mmp, r=[t_pw, t_dif], w=[tp])
                                ch = 2 * g + oc
                                k.op(dve, lambda pst=pst, ch=ch, t0=t0, n=n: nc.vector.scalar_tensor_tensor(
                                    out=xfm[:, ch, t0:t0 + n], in0=pst[:, 0:n], scalar=pv(PV_PS, ch), in1=xfm[:, ch, t0:t0 + n],
                                    op0=ALU.mult, op1=ALU.add), r=[tp, t_x[j], t_c], w=[t_x[j]])
                        if j < 3:
                            k.op(dve, lambda: nc.vector.tensor_copy(out=hf[:, :, 0:15], in_=hf[:, :, 512:527]), r=[t_dif, t_s], w=[t_hf])
                    k.barrier()

            if stage >= 7:
                MO = ExitStack()
                with MO:
                    alloc_ff(MO)
                    rw = sb(MO, [128, 8, 8], F32, "rw")
                    t_rw = Tok()
                    d_mo = k.dsem()
                    k.dma(sp, d_mo, rw[:], router_w.rearrange("(c p) e -> p c e", p=128), w=[t_rw])
                    hn = sb(MO, [128, 2, 512], F32, "hn")
                    t_hn = [Tok(), Tok()]
                    lgT = sb(MO, [8, 512], F32, "lgT")
                    t_lgT = Tok()
                    lg = sb(MO, [128, 17, 8], F32, "lg")
                    t_lg = Tok()
                    gT = sb(MO, [8, T], F32, "gT")
                    t_gT = Tok()
                    m1 = sb(MO, [128, 17], F32, "m1")
                    m2 = sb(MO, [128, 17], F32, "m2")
                    wk = sb(MO, [128, 17, 8], F32, "wk")
                    eq1 = sb(MO, [128, 17, 8], F32, "eq1")
                    eq2 = sb(MO, [128, 17, 8], F32, "eq2")
                    g1 = sb(MO, [128, 17], F32, "g1")
                    g2 = sb(MO, [128, 17], F32, "g2")
                    sel = sb(MO, [8, 8, 128], F32, "sel")
                    t_sel = Tok()
                    k.op(dve, lambda: nc.vector.memset(lg[:], 0.0), w=[t_lg])
                    for e in range(8):
                        k.op(dve, lambda e=e: nc.vector.tensor_copy(out=sel[:, e, :], in_=cst[0:8, C_ID + e:C_ID + e + 1].broadcast_to([8, 128])),
                             r=[t_c], w=[t_sel])
                    for j, (t0, n) in enumerate(TILES):
                        fm_rstd(j)
                        nsub = (n + 127) // 128
                        pst, tp = ps()
                        for c in range(8):
                            hb_ = c % 2
                            k.op(dve, lambda c=c, t0=t0, n=n, hb_=hb_: nc.vector.scalar_tensor_tensor(
                                out=hn[:, hb_, 0:n], in0=xfm[:, c, t0:t0 + n], scalar=pv(PV_NF1, c), in1=rstd2[:, 0:n],
                                op0=ALU.mult, op1=ALU.mult), r=[t_x[j], t_r2, t_c], w=[t_hn[hb_]])
                            k.op(act, lambda c=c, t0=t0, n=n, hb_=hb_: nc.scalar.copy(out=hb[:, c, t0:t0 + n], in_=hn[:, hb_, 0:n]),
                                 r=[t_hn[hb_]], w=[t_hb[j]])

                            k.op(pe, lambda pst=pst, n=n, c=c, hb_=hb_: nc.tensor.matmul(
                                pst[0:8, 0:n], rw[:, c, :], hn[:, hb_, 0:n], start=(c == 0), stop=(c == 7)),
                                r=[t_hn[hb_], t_rw], w=[tp])
                        k.op(act, lambda pst=pst, n=n: nc.scalar.copy(out=lgT[:, 0:n], in_=pst[0:8, 0:n]), r=[tp], w=[t_lgT])
                        pst2, tp2 = ps()

                        def trl(pst2=pst2, nsub=nsub, n=n):
                            ins = None
                            for s_ in range(nsub):
                                rows = min(128, n - s_ * 128)
                                ins = nc.tensor.transpose(pst2[0:rows, s_ * 8:s_ * 8 + 8], lgT[0:8, s_ * 128:s_ * 128 + rows], ident[0:8, 0:8])
                            return ins
                        k.op(pe, trl, r=[t_lgT, t_c], w=[tp2])
                        rows_all = min(128, n)
                        k.op(act, lambda pst2=pst2, j=j, nsub=nsub, rows_all=rows_all: nc.scalar.copy(
                            out=lg[0:rows_all, j * 4:j * 4 + nsub, :], in_=pst2[0:rows_all, 0:nsub * 8].rearrange("p (s e) -> p s e", e=8)),
                            r=[tp2], w=[t_lg])
                    X = mybir.AxisListType.X
                    k.op(dve, lambda: nc.vector.tensor_reduce(out=m1[:], in_=lg[:], axis=X, op=ALU.max), r=[t_lg], w=[t_lg])
                    k.op(dve, lambda: nc.vector.tensor_tensor(out=eq1[:], in0=lg[:], in1=m1[:].unsqueeze(2).broadcast_to([128, 17, 8]),
                                                              op=ALU.is_equal), r=[t_lg], w=[t_lg])
                    k.op(dve, lambda: nc.vector.scalar_tensor_tensor(out=wk[:], in0=eq1[:], scalar=-1e30, in1=lg[:], op0=ALU.mult,
                                                                     op1=ALU.add), r=[t_lg], w=[t_lg])
                    k.op(dve, lambda: nc.vector.tensor_reduce(out=m2[:], in_=wk[:], axis=X, op=ALU.max), r=[t_lg], w=[t_lg])
                    k.op(dve, lambda: nc.vector.tensor_tensor(out=eq2[:], in0=wk[:], in1=m2[:].unsqueeze(2).broadcast_to([128, 17, 8]),
                                                              op=ALU.is_equal), r=[t_lg], w=[t_lg])
                    k.op(dve, lambda: nc.vector.tensor_tensor(out=g1[:], in0=m1[:], in1=m2[:], op=ALU.subtract), r=[t_lg], w=[t_lg])
                    k.op(act, lambda: nc.scalar.activation(out=g1[:], in_=g1[:], func=AF.Sigmoid), r=[t_lg], w=[t_lg])
                    k.op(dve, lambda: nc.vector.tensor_scalar(out=g2[:], in0=g1[:], scalar1=-1.0, scalar2=1.0, op0=ALU.mult, op1=ALU.add),
                         r=[t_lg], w=[t_lg])
                    k.op(dve, lambda: nc.vector.tensor_tensor(out=eq1[:], in0=eq1[:], in1=g1[:].unsqueeze(2).broadcast_to([128, 17, 8]),
                                                              op=ALU.mult), r=[t_lg], w=[t_lg])
                    k.op(dve, lambda: nc.vector.tensor_tensor(out=eq2[:], in0=eq2[:], in1=g2[:].unsqueeze(2).broadcast_to([128, 17, 8]),
                                                              op=ALU.mult), r=[t_lg], w=[t_lg])
                    k.op(dve, lambda: nc.vector.tensor_tensor(out=wk[:], in0=eq1[:], in1=eq2[:], op=ALU.add), r=[t_lg], w=[t_lg])
                    for st in range(17):
                        rows = 128 if st < 16 else 16
                        pst, tp = ps()
                        k.op(pe, lambda pst=pst, st=st, rows=rows: nc.tensor.transpose(pst[0:8, 0:rows], wk[0:rows, st, :], ident[0:rows, 0:rows]),
                             r=[t_lg, t_c], w=[tp])
                        k.op(act, lambda pst=pst, st=st, rows=rows: nc.scalar.copy(out=gT[:, st * 128:st * 128 + rows], in_=pst[0:8, 0:rows]),
                             r=[tp], w=[t_gT])
                    for e in range(NE if not SMALLW else 1):
                        for j, (t0, n) in enumerate(TILES):
                            pst, tp = ps()
                            k.op(pe, lambda pst=pst, e=e, t0=t0, n=n: nc.tensor.matmul(pst[:, 0:n], sel[:, e, :], gT[:, t0:t0 + n],
                                                                                     start=True, stop=True), r=[t_sel, t_gT], w=[tp])
                            k.op(act, lambda pst=pst, t0=t0, n=n: nc.scalar.copy(out=gbc[:, t0:t0 + n], in_=pst[:, 0:n]), r=[tp], w=[t_gbc])
                        gated_ffn(moe_g[e], moe_u[e], moe_d[e], DFE, True)
                    k.barrier()

        if stage >= 8:
            FN = ExitStack()
            with FN:
                yt = sb(FN, [128, 8, 512], F32, "yt")
                t_yt = Tok()
                yo = [sb(FN, [128, 1024], F32, "yo") for _ in range(2)]
                t_yo = [Tok(), Tok()]
                for j, (t0, n) in enumerate(TILES):
                    fm_rstd(j)
                    for c in range(8):
                        k.op(dve, lambda c=c, t0=t0, n=n: nc.vector.scalar_tensor_tensor(
                            out=yt[:, c, 0:n], in0=xfm[:, c, t0:t0 + n], scalar=pv(PV_NFIN, c), in1=rstd2[:, 0:n],
                            op0=ALU.mult, op1=ALU.mult), r=[t_x[j], t_r2, t_c], w=[t_yt])
                    nsub = (n + 127) // 128
                    for s_ in range(nsub):
                        rows = min(128, n - s_ * 128)
                        bi = (j * 4 + s_) % 2
                        for half in range(2):
                            pst, tp = ps()

                            def try_(pst=pst, half=half, s_=s_, rows=rows):
                                ins = None
                                for cc in range(4):
                                    ins = nc.tensor.transpose(pst[0:rows, cc * 128:(cc + 1) * 128], yt[:, half * 4 + cc, s_ * 128:s_ * 128 + rows], ident)
                                return ins
                            k.op(pe, try_, r=[t_yt, t_c], w=[tp])
                            k.op(act, lambda pst=pst, half=half, rows=rows, bi=bi: nc.scalar.copy(
                                out=yo[bi][0:rows, half * 512:(half + 1) * 512], in_=pst[0:rows, :]), r=[tp], w=[t_yo[bi]])
                        dst = o_yp[t0 + s_ * 128:t0 + s_ * 128 + rows, :] if j < 4 else o_ys[:, :]
                        odma(dst, yo[bi][0:rows, :], [t_yo[bi]])
        _finish(nc, k, out_ds)
    return nc


def _finish(nc, k, out_ds):
    print("K ops:", k.n, {e.name: e.cnt for e in [k.pe, k.act, k.dve, k.pool, k.sp]})
    for d in out_ds:
        if d.cnt:
            k.sp.h.wait_ge(d.sem, d.cnt)


def _host_consts():
    cst = np.zeros((128, C_W), np.float32)
    cst[:, C_ID:C_ID + 128] = np.eye(128, dtype=np.float32)
    kk = np.arange(128)[:, None]
    qq = np.arange(128)[None, :]
    cst[:, C_TRI:C_TRI + 128] = (kk <= qq).astype(np.float32)
    for kx in range(4):
        for h in range(8):
            for t in range(4):
                cst[kx, C_MN + h * 4 + t] = 1.0 if kx <= t else 0.0
    for g, w in enumerate((2, 4, 8, 16)):
        for p in range(16):
            cst[:, C_INV + g * 16 + p] = 1.0 / min(p + 1, w)
    cst[:, C_IOTA] = np.arange(128, dtype=np.float32)
    half = 16
    inv_freq = np.power(np.float32(10000.0), -np.arange(half, dtype=np.float32) / np.float32(half)).astype(np.float32)
    pos = np.concatenate([np.arange(NP_, dtype=np.float32),
                          np.tile(16384 + np.arange(4, dtype=np.float32), 4)]).astype(np.float32)
    ang = (pos[None, :] * inv_freq[:, None]).astype(np.float32)
    cos = np.cos(ang.astype(np.float64)).astype(np.float32)
    sin = np.sin(ang.astype(np.float64)).astype(np.float32)
    ropec = np.concatenate([cos, cos], 0)
    ropes = np.concatenate([-sin, sin], 0)
    return cst, np.ascontiguousarray(ropec), np.ascontiguousarray(ropes)


def _fm(v):
    return np.ascontiguousarray(np.asarray(v, np.float32).reshape(-1, 128).T)


def kernel(stage=99, limit=10 ** 9, **inp):
    f = lambda a: np.ascontiguousarray(np.asarray(a, dtype=np.float32))
    cst, ropec, ropes = _host_consts()
    pvec = np.zeros((128, PV_W), np.float32)
    pvec[:, PV_NM0:PV_NM0 + 8] = _fm(inp["norm_mix"][0])
    pvec[:, PV_NF0:PV_NF0 + 8] = _fm(inp["norm_ffn"][0])
    pvec[:, PV_NM1:PV_NM1 + 8] = _fm(inp["norm_mix"][1])
    pvec[:, PV_NF1:PV_NF1 + 8] = _fm(inp["norm_ffn"][1])
    pvec[:, PV_NFIN:PV_NFIN + 8] = _fm(inp["norm_final"])
    pvec[:, PV_CB:PV_CB + 4] = _fm(inp["conv_b"][0])
    pvec[:, PV_LG:PV_LG + 4] = _fm(inp["conv_ln_g"][0])
    pvec[:, PV_LB:PV_LB + 4] = _fm(inp["conv_ln_b"][0])
    pvec[:, PV_QN:PV_QN + 4] = _fm(inp["q_norm"][0])
    pvec[:, PV_KVN:PV_KVN + 2] = _fm(inp["kv_norm"][0])
    pvec[:, PV_PS:PV_PS + 8] = _fm(inp["pool_scale"][0])
    cw = np.asarray(inp["conv_w"][0], np.float32)
    pvec[:, PV_CW:PV_CW + 124] = cw.reshape(31, 4, 128).transpose(2, 0, 1).reshape(128, 124)
    shared = {
        "ccat": np.concatenate([f(inp["cache_ckv"][0][:CACHE_PAGES]).reshape(CACHE_PAGES * 128, 256),
                                f(inp["cache_kpe"][0][:CACHE_PAGES]).reshape(CACHE_PAGES * 128, 32)], axis=1),
        "pvec": pvec, "cst": cst, "ropec": ropec, "ropes": ropes,
        "w_in": f(inp["w_in"][0]), "w_uq": f(inp["w_uq"][0]), "w_ukv": f(inp["w_ukv"][0]), "w_out": f(inp["w_out"][0]),
        "ffn_g": f(inp["ffn_w_gate"][0]), "ffn_u": f(inp["ffn_w_up"][0]), "ffn_d": f(inp["ffn_w_down"][0]),
        "pool_w": f(inp["pool_w"][0]), "router_w": f(inp["router_w"][0]),
        "moe_g": f(inp["moe_w_gate"][0][:(1 if SMALLW else NE)]), "moe_u": f(inp["moe_w_up"][0][:(1 if SMALLW else NE)]),
        "moe_d": f(inp["moe_w_down"][0][:(1 if SMALLW else NE)]),
    }
    xpr = f(inp["x_prompt"])
    xsa = f(inp["x_sample"])
    pt = np.ascontiguousarray(np.asarray(inp["page_table"], dtype=np.int32))
    sc = f(inp["state_conv"][0])
    spl = f(inp["state_pool"][0])
    in_maps = []
    for c in range(8):
        m = dict(shared)
        m["xp"] = xpr[c]
        m["xs"] = np.ascontiguousarray(xsa[4 * c:4 * c + 4].reshape(16, 1024))
        m["ptab"] = np.ascontiguousarray(np.broadcast_to(pt[4 * c:4 * c + 4].reshape(1, 512), (128, 512)))
        m["sconv"] = np.ascontiguousarray(sc[4 * c:4 * c + 4].reshape(120, 512))
        m["spool"] = np.ascontiguousarray(spl[4 * c:4 * c + 4].reshape(60, 1024))
        in_maps.append(m)
    nc = build(stage, limit)
    res = run_bass_kernel_spmd(nc, in_maps, core_ids=list(range(8)))
    R = res.results
    cat = lambda key: np.stack([np.asarray(R[c][key], np.float32) for c in range(8)], 0)
    y_p = cat("o_yp")
    y_s = cat("o_ys").reshape(32, 4, 1024)
    ckv_p = cat("o_ckvp")[None]
    kpe_p = cat("o_kpep")[None]
    conv_p = cat("o_convp")[None]
    pool_p = cat("o_poolp")[None]
    ckv_s = cat("o_ckvs").reshape(1, 32, 4, 256)
    kpe_s = cat("o_kpes").reshape(1, 32, 4, 32)
    conv_s = cat("o_convs").reshape(1, 32, 30, 512)
    pool_s = cat("o_pools").reshape(1, 32, 15, 1024)
    return (y_p, y_s, ckv_p, kpe_p, conv_p, pool_p, ckv_s, kpe_s, conv_s, pool_s)
```
